# Optimizing a Trainium2 kernel written in Bass

```python
import math
import jax, jax.numpy as jnp
from jax import lax
import numpy as np

D_MODEL = 1024
BATCH = 4
SEQ = 4096
DEPTH = 2

CHUNK = 64
Q_BLOCK = 128
HEAD_DIM = 64
FOX_HEADS = 8
FOX_DIM = FOX_HEADS * HEAD_DIM
GLA_HEADS = 4
GLA_DK = 64
GLA_DV = 128
GLA_QK = GLA_HEADS * GLA_DK
GLA_V = GLA_HEADS * GLA_DV
GLA_RANK = 16
GLA_TAU = 16.0
MIX_WIDTH = FOX_DIM + GLA_V
IN_COLS = 3 * FOX_DIM + FOX_HEADS + 2 * GLA_QK + 2 * GLA_V + GLA_RANK
S5_GROUP = 16
S5_GROUPS = D_MODEL // S5_GROUP
S5_STATE = 64
D_FF = 2816
N_EXPERTS = 8
TOP_K = 2
EXPERT_FF = 2816
N_EVEN = (DEPTH + 1) // 2
N_ODD = DEPTH // 2
EPS = 1e-6

kernel_name = "fox_gla_s5_moe_adaln_hybrid"


def rmsnorm(x, w):
    xf = x.astype(jnp.float32)
    y = xf * lax.rsqrt(jnp.mean(xf * xf, axis=-1, keepdims=True) + EPS)
    return y * w.astype(jnp.float32)


def adaln(c, w, b):
    m = jax.nn.silu(c.astype(jnp.float32)) @ w + b
    shift, scale, gate = jnp.split(m, 3, axis=-1)
    return shift[:, None, :], scale[:, None, :], gate[:, None, :]


def fox_attention(q, k, v, logf):
    B, H, S, dh = q.shape
    cum = jnp.cumsum(logf, axis=-1)
    nb = S // Q_BLOCK
    qb = q.reshape(B, H, nb, Q_BLOCK, dh).transpose(2, 0, 1, 3, 4)
    cb = cum.reshape(B, H, nb, Q_BLOCK).transpose(2, 0, 1, 3)
    kpos = jnp.arange(S)
    scale = dh ** -0.5

    def block(args):
        i, qi, ci = args
        s = jnp.einsum('bhqd,bhkd->bhqk', qi, k) * scale + ci[..., None] - cum[:, :, None, :]
        qpos = i * Q_BLOCK + jnp.arange(Q_BLOCK)
        s = jnp.where(kpos[None, :] <= qpos[:, None], s, -jnp.inf)
        p = jax.nn.softmax(s, axis=-1)
        return jnp.einsum('bhqk,bhkd->bhqd', p, v)

    out = lax.map(block, (jnp.arange(nb), qb, cb))
    return out.transpose(1, 2, 0, 3, 4).reshape(B, H, S, dh)


def gla_attention(q, k, v, log_a):
    B, H, S, dk = q.shape
    dv = v.shape[-1]
    nc = S // CHUNK

    def to_chunks(t):
        return t.reshape(B, H, nc, CHUNK, t.shape[-1]).transpose(2, 0, 1, 3, 4)

    qc, kc, vc, ac = (to_chunks(q * dk ** -0.5), to_chunks(k), to_chunks(v), to_chunks(log_a))
    bc = jnp.cumsum(ac, axis=3)
    causal = jnp.tril(jnp.ones((CHUNK, CHUNK), dtype=bool))

    def step(state, inp):
        qi, ki, vi, bi = inp
        diff = bi[:, :, :, None, :] - bi[:, :, None, :, :]
        decay = jnp.exp(jnp.where(causal[None, None, :, :, None], diff, -jnp.inf))
        attn = jnp.einsum('bhtd,bhsd,bhtsd->bhts', qi, ki, decay)
        o = (jnp.einsum('bhts,bhsv->bhtv', attn, vi)
             + jnp.einsum('bhtd,bhdv->bhtv', qi * jnp.exp(bi), state))
        blast = bi[:, :, -1:, :]
        state = (jnp.exp(blast[:, :, 0, :])[..., None] * state
                 + jnp.einsum('bhsd,bhsv->bhdv', ki * jnp.exp(blast - bi), vi))
        return state, o

    s0 = jnp.zeros((B, H, dk, dv), jnp.float32)
    _, o = lax.scan(step, s0, (qc, kc, vc, bc))
    return o.transpose(1, 2, 0, 3, 4).reshape(B, H, S, dv)


def even_mixer(h, w_in, fox_fb, gla_w2, gla_b2, gla_norm, w_o):
    B, S, _ = h.shape
    sizes = (FOX_DIM, FOX_DIM, FOX_DIM, FOX_HEADS, GLA_QK, GLA_QK, GLA_V, GLA_V, GLA_RANK)
    cuts = [sum(sizes[:i + 1]) for i in range(len(sizes) - 1)]
    fq, fk, fv, ff, gq, gk, gv, gg, glr = jnp.split(h @ w_in, cuts, axis=-1)

    def heads(t, n):
        return t.reshape(B, S, n, -1).transpose(0, 2, 1, 3)

    logf = jax.nn.log_sigmoid(ff + fox_fb).transpose(0, 2, 1)
    fox = fox_attention(heads(fq, FOX_HEADS), heads(fk, FOX_HEADS), heads(fv, FOX_HEADS), logf)
    fox = fox.transpose(0, 2, 1, 3).reshape(B, S, FOX_DIM)
    log_a = jax.nn.log_sigmoid(glr @ gla_w2 + gla_b2) / GLA_TAU
    gla = gla_attention(heads(gq, GLA_HEADS), heads(gk, GLA_HEADS), heads(gv, GLA_HEADS),
                        heads(log_a, GLA_HEADS))
    gla = rmsnorm(gla.transpose(0, 2, 1, 3), gla_norm).reshape(B, S, GLA_V) * jax.nn.silu(gg)
    return jnp.concatenate([fox, gla], axis=-1) @ w_o


def s5_mixer(h, w_in, lam_re, lam_im, log_dt, b_re, b_im, c_re, c_im, d_skip, w_glu, w_o):
    B, S, _ = h.shape
    f32 = jnp.float32
    lam_re, lam_im, log_dt = lam_re.astype(f32), lam_im.astype(f32), log_dt.astype(f32)
    b_re, b_im, c_re, c_im = b_re.astype(f32), b_im.astype(f32), c_re.astype(f32), c_im.astype(f32)
    u = h @ w_in
    ug = u.reshape(B, S, S5_GROUPS, S5_GROUP)
    dt = jnp.exp(log_dt)[:, None]
    mag = jnp.exp(lam_re * dt)
    a_re = mag * jnp.cos(lam_im * dt)
    a_im = mag * jnp.sin(lam_im * dt)
    den = lam_re * lam_re + lam_im * lam_im
    nr, ni = a_re - 1.0, a_im
    f_re = (nr * lam_re + ni * lam_im) / den
    f_im = (ni * lam_re - nr * lam_im) / den
    bb_re = f_re[..., None] * b_re - f_im[..., None] * b_im
    bb_im = f_re[..., None] * b_im + f_im[..., None] * b_re
    ut = ug.transpose(1, 0, 2, 3)
    bu_re = jnp.einsum('sbgk,gpk->sbgp', ut, bb_re)
    bu_im = jnp.einsum('sbgk,gpk->sbgp', ut, bb_im)
    ar_t = jnp.broadcast_to(a_re[None, None], (S, 1, S5_GROUPS, S5_STATE))
    ai_t = jnp.broadcast_to(a_im[None, None], (S, 1, S5_GROUPS, S5_STATE))

    def combine(e1, e2):
        a1r, a1i, b1r, b1i = e1
        a2r, a2i, b2r, b2i = e2
        return (a1r * a2r - a1i * a2i,
                a1r * a2i + a1i * a2r,
                a2r * b1r - a2i * b1i + b2r,
                a2r * b1i + a2i * b1r + b2i)

    _, _, xr, xi = lax.associative_scan(combine, (ar_t, ai_t, bu_re, bu_im), axis=0)
    y = jnp.einsum('sbgp,gkp->bsgk', xr, c_re) - jnp.einsum('sbgp,gkp->bsgk', xi, c_im)
    y = y.reshape(B, S, D_MODEL) + d_skip * u
    y = jax.nn.gelu(y)
    y = y * jax.nn.sigmoid(y @ w_glu)
    return y @ w_o


def swiglu(h, w_gate, w_up, w_down):
    return (jax.nn.silu(h @ w_gate) * (h @ w_up)) @ w_down


def moe_swiglu(h, w_router, w_gate, w_up, w_down):
    logits = (h @ w_router).astype(jnp.float32)
    top_val, top_idx = lax.top_k(logits, TOP_K)
    weights = jax.nn.softmax(top_val, axis=-1)
    gates = jnp.sum(jax.nn.one_hot(top_idx, N_EXPERTS, dtype=jnp.float32) * weights[..., None], axis=-2)
    out = jnp.zeros_like(h)
    for e in range(N_EXPERTS):
        out = out + gates[..., e:e + 1] * swiglu(h, w_gate[e], w_up[e], w_down[e])
    return out


def setup_inputs(seed: int = 0) -> dict:
    key = jax.random.key(seed)
    ks = iter(jax.random.split(key, 64))
    f32 = jnp.float32

    def nrm(shape, scale):
        return jax.random.normal(next(ks), shape, f32) * scale

    def gain(shape):
        return 1.0 + 0.05 * jax.random.normal(next(ks), shape, f32)

    D = D_MODEL
    NE, NO = N_EVEN, N_ODD
    G, P, K = S5_GROUPS, S5_STATE, S5_GROUP
    mod_s = 0.5 * D ** -0.5
    inp = {}
    inp['x'] = nrm((BATCH, SEQ, D), 1.0)
    inp['c'] = nrm((BATCH, D), 1.0)
    inp['e_norm_mix'] = gain((NE, D))
    inp['e_mod_mix_w'] = nrm((NE, D, 3 * D), mod_s)
    inp['e_mod_mix_b'] = nrm((NE, 3 * D), 0.02)
    inp['e_w_in'] = nrm((NE, D, IN_COLS), D ** -0.5)
    inp['e_fox_fb'] = 2.0 + nrm((NE, FOX_HEADS), 0.5)
    inp['e_gla_w2'] = nrm((NE, GLA_RANK, GLA_QK), GLA_RANK ** -0.5)
    inp['e_gla_b2'] = nrm((NE, GLA_QK), 0.02)
    inp['e_gla_norm'] = gain((NE, GLA_DV))
    inp['e_w_o'] = nrm((NE, MIX_WIDTH, D), MIX_WIDTH ** -0.5)
    inp['e_norm_ffn'] = gain((NE, D))
    inp['e_mod_ffn_w'] = nrm((NE, D, 3 * D), mod_s)
    inp['e_mod_ffn_b'] = nrm((NE, 3 * D), 0.02)
    inp['e_ffn_gate'] = nrm((NE, D, D_FF), D ** -0.5)
    inp['e_ffn_up'] = nrm((NE, D, D_FF), D ** -0.5)
    inp['e_ffn_down'] = nrm((NE, D_FF, D), D_FF ** -0.5)
    inp['o_norm_mix'] = gain((NO, D))
    inp['o_mod_mix_w'] = nrm((NO, D, 3 * D), mod_s)
    inp['o_mod_mix_b'] = nrm((NO, 3 * D), 0.02)
    inp['o_w_in'] = nrm((NO, D, D), D ** -0.5)
    inp['o_lam_re'] = -0.5 + nrm((NO, G, P), 0.01)
    inp['o_lam_im'] = math.pi * jnp.arange(P, dtype=f32)[None, None, :] + nrm((NO, G, P), 0.01)
    inp['o_log_dt'] = jax.random.uniform(next(ks), (NO, G), f32, math.log(1e-3), math.log(1e-1))
    inp['o_b_re'] = nrm((NO, G, P, K), (2.0 * K) ** -0.5)
    inp['o_b_im'] = nrm((NO, G, P, K), (2.0 * K) ** -0.5)
    inp['o_c_re'] = nrm((NO, G, K, P), (2.0 / P) ** 0.5)
    inp['o_c_im'] = nrm((NO, G, K, P), (2.0 / P) ** 0.5)
    inp['o_d_skip'] = nrm((NO, D), 1.0)
    inp['o_w_glu'] = nrm((NO, D, D), D ** -0.5)
    inp['o_w_o'] = nrm((NO, D, D), D ** -0.5)
    inp['o_norm_ffn'] = gain((NO, D))
    inp['o_mod_ffn_w'] = nrm((NO, D, 3 * D), mod_s)
    inp['o_mod_ffn_b'] = nrm((NO, 3 * D), 0.02)
    inp['o_router'] = nrm((NO, D, N_EXPERTS), D ** -0.5)
    inp['o_exp_gate'] = nrm((NO, N_EXPERTS, D, EXPERT_FF), D ** -0.5)
    inp['o_exp_up'] = nrm((NO, N_EXPERTS, D, EXPERT_FF), D ** -0.5)
    inp['o_exp_down'] = nrm((NO, N_EXPERTS, EXPERT_FF, D), EXPERT_FF ** -0.5)
    inp['final_norm'] = gain((D,))
    return inp


def reference(x, c,
              e_norm_mix, e_mod_mix_w, e_mod_mix_b, e_w_in, e_fox_fb, e_gla_w2, e_gla_b2,
              e_gla_norm, e_w_o, e_norm_ffn, e_mod_ffn_w, e_mod_ffn_b, e_ffn_gate, e_ffn_up,
              e_ffn_down,
              o_norm_mix, o_mod_mix_w, o_mod_mix_b, o_w_in, o_lam_re, o_lam_im, o_log_dt,
              o_b_re, o_b_im, o_c_re, o_c_im, o_d_skip, o_w_glu, o_w_o, o_norm_ffn,
              o_mod_ffn_w, o_mod_ffn_b, o_router, o_exp_gate, o_exp_up, o_exp_down,
              final_norm):
    for i in range(DEPTH):
        j = i // 2
        if i % 2 == 0:
            sh, sc, g = adaln(c, e_mod_mix_w[j], e_mod_mix_b[j])
            h = rmsnorm(x, e_norm_mix[j]) * (1.0 + sc) + sh
            y = even_mixer(h, e_w_in[j], e_fox_fb[j], e_gla_w2[j], e_gla_b2[j], e_gla_norm[j], e_w_o[j])
            x = x + (g * y).astype(x.dtype)
            sh, sc, g = adaln(c, e_mod_ffn_w[j], e_mod_ffn_b[j])
            h = rmsnorm(x, e_norm_ffn[j]) * (1.0 + sc) + sh
            y = swiglu(h, e_ffn_gate[j], e_ffn_up[j], e_ffn_down[j])
            x = x + (g * y).astype(x.dtype)
        else:
            sh, sc, g = adaln(c, o_mod_mix_w[j], o_mod_mix_b[j])
            h = rmsnorm(x, o_norm_mix[j]) * (1.0 + sc) + sh
            y = s5_mixer(h, o_w_in[j], o_lam_re[j], o_lam_im[j], o_log_dt[j], o_b_re[j], o_b_im[j],
                         o_c_re[j], o_c_im[j], o_d_skip[j], o_w_glu[j], o_w_o[j])
            x = x + (g * y).astype(x.dtype)
            sh, sc, g = adaln(c, o_mod_ffn_w[j], o_mod_ffn_b[j])
            h = rmsnorm(x, o_norm_ffn[j]) * (1.0 + sc) + sh
            y = moe_swiglu(h, o_router[j], o_exp_gate[j], o_exp_up[j], o_exp_down[j])
            x = x + (g * y).astype(x.dtype)
    return rmsnorm(x, final_norm).astype(x.dtype)
```

```python
import contextlib
import numpy as np
import concourse.bass as bass
import concourse.mybir as mybir
from concourse.bass_utils import run_bass_kernel_spmd

F32 = mybir.dt.float32
BF16 = mybir.dt.bfloat16
AF = mybir.ActivationFunctionType
ALU = mybir.AluOpType
AX = mybir.AxisListType

D = 1024
S = 4096
NB = 4
DFF = 2816
NE = 8
EPS = 1e-6
NCORE = 8


class Prog:
    NDSEM = 16
    ENGS = ("pe", "act", "dve", "pool", "sp")

    def __init__(self):
        self.nc = bass.Bass("TRN2", target_bir_lowering=False)
        self.ops = []
        self.uid = 0
        self.banks = [self.ps(f"bank{i}", [128, 512], F32) for i in range(8)]
        self.bank_i = 0
        self.reserved = set()
        self.rot = {}
        self.prefix = ""
        self.phase_base = None

    def inp(self, name, shape, dtype=F32):
        return self.nc.dram_tensor(name, list(shape), dtype, kind="ExternalInput").ap()

    def out(self, name, shape, dtype=F32):
        return self.nc.dram_tensor(name, list(shape), dtype, kind="ExternalOutput").ap()

    def sb(self, name, shape, dtype=F32):
        return self.nc.alloc_sbuf_tensor(self.prefix + name, list(shape), dtype)

    def dram(self, name, shape, dtype=F32):
        return self.nc.dram_tensor(name, list(shape), dtype, kind="Internal").ap()

    def begin_phase(self, prefix):
        if self.phase_base is not None:
            self.nc.sbuf_base = self.phase_base
        else:
            self.phase_base = self.nc.sbuf_base
        self.prefix = prefix
        self.rot = {}
        self.ops.append(("barrier", None, (), (), False))
        self.on_phase()

    def on_phase(self):
        pass

    def barrier(self):
        self.ops.append(("barrier", None, (), (), False))

    def allgather_pair(self, src, dst, r=(), w=()):
        groups = [[2 * i, 2 * i + 1] for i in range(NCORE // 2)]
        self.ops.append(("pool", lambda e: e.collective_compute("AllGather", ALU.bypass, replica_groups=groups, ins=[src], outs=[dst]),
                         tuple(r), tuple(w), "cc"))

    def sel_load(self, dst, tmp, pieces0, pieces1, sel, dkey, tkey, prange=(0, 128), extra_w=(), extra_r=()):
        a, b = prange
        for fn, src in pieces0:
            self.dma(fn(dst), src, r=list(extra_r), w=[dkey] + list(extra_w))
        for fn, src in pieces1:
            self.dma(fn(tmp), src, r=list(extra_r), w=[tkey])
        self.op("dve", lambda e: e.tensor_scalar(out=dst, in0=dst, scalar1=sel[a:b, 0:1], scalar2=None, op0=ALU.mult), r=[dkey, "sel_sb"], w=[dkey])
        self.op("dve", lambda e: e.scalar_tensor_tensor(out=dst, in0=tmp, scalar=sel[a:b, 1:2], in1=dst, op0=ALU.mult, op1=ALU.add),
                r=[dkey, tkey, "sel_sb"], w=[dkey])

    def ps(self, name, shape, dtype=F32):
        return self.nc.alloc_psum_tensor(name, list(shape), dtype)

    def bank(self):
        while True:
            i = self.bank_i
            self.bank_i = (i + 1) % 8
            if i not in self.reserved:
                return i

    def rbuf(self, name, shape, dtype=F32, n=2):
        if name not in self.rot:
            self.rot[name] = [[self.sb(f"{name}{i}", shape, dtype) for i in range(n)], 0]
        lst, i = self.rot[name]
        self.rot[name][1] = (i + 1) % len(lst)
        return lst[i], (self.prefix + name, i)

    def op(self, eng, fn, r=(), w=(), dma=False):
        self.ops.append((eng, fn, tuple(r), tuple(w), dma))

    def dma(self, out, in_, r=(), w=(), q="sp", **kw):
        self.op(q, lambda e: e.dma_start(out=out, in_=in_, **kw), r, w, dma=True)

    def build(self):
        nc = self.nc
        nds = self.NDSEM
        seq = {e: 0 for e in self.ENGS}
        dcount = {e: 0 for e in self.ENGS}
        last_w = {}
        last_r = {}
        known = {e: {} for e in self.ENGS}
        plan = {e: [] for e in self.ENGS}
        pending = {e: {} for e in self.ENGS}
        cur_tok = {}
        for (eng, fn, r, w, is_dma) in self.ops:
            if eng == "barrier":
                for e in self.ENGS:
                    for s_, v_ in cur_tok.items():
                        if pending[e].get(s_, 0) < v_:
                            pending[e][s_] = v_
                continue
            need = dict(pending[eng])
            pending[eng] = {}

            def add(tok):
                if tok is None:
                    return
                s, v = tok
                if need.get(s, 0) < v:
                    need[s] = v
            for k in r:
                add(last_w.get(k))
                if isinstance(k, tuple) and k[0] == "bank":
                    own = ("c", eng)
                    for sn_, t in last_r.get(k, {}).items():
                        if sn_ != own:
                            add(t)
            for k in w:
                add(last_w.get(k))
                for t in last_r.get(k, {}).values():
                    add(t)
            if is_dma == "cc":
                ncc = getattr(self, "_ncc", 0)
                self._ncc = ncc + 1
                sname = ("cc", ncc)
                tok = (sname, 1)
                inc = 1
            elif is_dma:
                d = dcount[eng]
                dcount[eng] += 1
                sname = ("d", eng, d % nds)
                tok = (sname, 16 * (d // nds + 1))
                if d >= nds:
                    add((sname, 16 * (d // nds)))
                inc = 16
            else:
                seq[eng] += 1
                sname = ("c", eng)
                tok = (sname, seq[eng])
                inc = 1
            waits = []
            for s, v in need.items():
                if eng == "pe" and s == ("c", "pe"):
                    continue
                if known[eng].get(s, 0) >= v:
                    continue
                known[eng][s] = v
                waits.append((s, v))
            plan[eng].append((fn, waits, sname, inc))
            cur_tok[tok[0]] = tok[1]
            for k in w:
                last_w[k] = tok
                last_r[k] = {}
            for k in r:
                last_r.setdefault(k, {})[sname] = tok
        final_waits = []
        for e in self.ENGS:
            if seq[e]:
                final_waits.append((("c", e), seq[e]))
            for i in range(min(nds, dcount[e])):
                cnt = (dcount[e] - 1 - i) // nds + 1
                final_waits.append((("d", e, i), 16 * cnt))
        for i in range(getattr(self, "_ncc", 0)):
            final_waits.append((("cc", i), 1))
        semnames = set()
        for e in self.ENGS:
            for (_, waits, sname, _) in plan[e]:
                semnames.add(sname)
        sems = {}
        with contextlib.ExitStack() as st:
            for s in sorted(semnames, key=str):
                sems[s] = st.enter_context(nc.semaphore("_".join(str(x) for x in s)))
            block = st.enter_context(nc.Block())

            def run(engname, final=False):
                def body(e):
                    for (fn, waits, sname, inc) in plan[engname]:
                        for (s, v) in waits:
                            e.wait_ge(sems[s], v)
                        ins = fn(e)
                        ins.then_inc(sems[sname], inc)
                    if final:
                        for (s, v) in final_waits:
                            e.wait_ge(sems[s], v)
                return body
            block.tensor(run("pe"))
            block.scalar(run("act"))
            block.vector(run("dve"))
            block.gpsimd(run("pool"))
            block.sync(run("sp", final=True))
        return nc


class K(Prog):
    SLOT = 1408
    NSLOT = 4

    def __init__(self):
        super().__init__()
        self.on_phase()

    def on_phase(self):
        self.wst = None
        self.slot_i = 0
        self.ones = self.sb("ones_f", [128, 128], F32)
        ones = self.ones
        self.op("pool", lambda e: e.memset(ones[:], 1.0), w=["ones"])

    def make_ident(self):
        self.ident = self.sb("ident_f", [128, 128], F32)
        ident, ones = self.ident, self.ones
        self.op("pool", lambda e: e.affine_select(out=ident[:], in_=ones[:], pattern=[[-1, 128]],
                                                   compare_op=ALU.is_equal, fill=0.0, base=0, channel_multiplier=1),
                r=["ones"], w=["ident"])

    def load(self, name, dram_ap, shape, dtype=F32):
        t = self.sb(name, shape, dtype)
        self.dma(t[:], dram_ap, w=[name, t.name])
        return t

    def wload(self, Wap, k0, KT, c0, cb):
        if self.wst is None:
            self.wst = [self.sb(f"wst{i}", [128, self.SLOT], F32) for i in range(self.NSLOT)]
            self.wbf = [self.sb(f"wbf{i}", [128, self.SLOT], BF16) for i in range(self.NSLOT)]
        s = self.slot_i
        self.slot_i = (s + 1) % self.NSLOT
        assert KT * cb <= self.SLOT
        st = self.wst[s][:, 0:KT * cb].rearrange("p (kt m) -> p kt m", kt=KT)
        bf = self.wbf[s][:, 0:KT * cb].rearrange("p (kt m) -> p kt m", kt=KT)
        src = Wap[k0:k0 + KT * 128, c0:c0 + cb].rearrange("(kt p) m -> p kt m", p=128)
        self.dma(st, src, w=[("wst", s)])
        self.cast_i = getattr(self, "cast_i", 0) + 1
        if self.cast_i % 2:
            self.op("act", lambda e: e.activation(out=bf, in_=st, func=AF.Copy), r=[("wst", s)], w=[("wbf", s)])
        else:
            self.op("dve", lambda e: e.tensor_copy(out=bf, in_=st), r=[("wst", s)], w=[("wbf", s)])
        return bf, ("wbf", s)

    def linear(self, Ws, k0, Kdim, M, rhs, rkeys, N, evac, cbs=128):
        KT = Kdim // 128
        cblocks = [(c0, min(cbs, M - c0)) for c0 in range(0, M, cbs)]
        PF = max(1, self.NSLOT // len(Ws) - 1)
        loaded = {}
        for bi, (c0, cb) in enumerate(cblocks):
            for bj in range(bi, min(bi + PF + 1, len(cblocks))):
                if bj not in loaded:
                    loaded[bj] = [self.wload(W, k0, KT, cblocks[bj][0], cblocks[bj][1]) for W in Ws]
            blocks = loaded.pop(bi)
            for m0 in range(0, cb, 128):
                msz = min(128, cb - m0)
                for n0 in range(0, N, 512):
                    nsz = min(512, N - n0)
                    pss = []
                    for (bf, key) in blocks:
                        b = self.bank()
                        ps = self.banks[b][0:msz, 0:nsz]

                        def mm(e, bf=bf, ps=ps, m0=m0, msz=msz, n0=n0, nsz=nsz):
                            ins = None
                            for kt in range(KT):
                                ins = e.matmul(ps, lhsT=bf[:, kt, m0:m0 + msz], rhs=rhs(kt, n0, nsz),
                                               start=(kt == 0), stop=(kt == KT - 1))
                            return ins
                        self.op("pe", mm, r=[key] + list(rkeys), w=[("bank", b)])
                        pss.append((ps, ("bank", b)))
                    evac(c0 + m0, msz, n0, nsz, pss)

    def adaln(self, tag, csil, W, bcols, pre=None):
        modc = self.sb(f"modc_{tag}", [128, 24], F32)
        key = f"modc_{tag}"
        if pre is not None:
            self.dma(modc[:], pre, w=[key])
            return modc, key

        def evac(m0, msz, n0, nsz, pss):
            ps, pk = pss[0]
            col = m0 // 128
            self.op("dve", lambda e: e.tensor_tensor(out=modc[:, col:col + 1], in0=ps, in1=bcols[:, col:col + 1], op=ALU.add),
                    r=[pk, bcols.name], w=[key])
        self.linear([W], 0, D, 3 * D, lambda kt, n0, nsz: csil[:, kt:kt + 1], [csil.name], 1, evac)
        return modc, key

    def silu_c(self, ccol_ap):
        c = self.load("c_col", ccol_ap, [128, 8])
        csil = self.sb("c_sil", [128, 8], BF16)
        self.op("act", lambda e: e.activation(out=csil[:], in_=c[:], func=AF.Silu), r=["c_col"], w=["c_sil", csil.name])
        return csil

    def wmod(self, tag, modc, mkey, normw):
        wm = self.sb(f"wm_{tag}", [128, 8], F32)
        self.op("dve", lambda e: e.scalar_tensor_tensor(out=wm[:], in0=modc[:, 8:16], scalar=1.0, in1=normw[:],
                                                         op0=ALU.add, op1=ALU.mult),
                r=[mkey, normw.name], w=[wm.name])
        return wm

    def rmsnorm(self, xget, N, wm, shc, ckeys, outs, after=None):
        epsc = self.epsc
        if getattr(self, "ones_b_phase", None) != self.prefix:
            self.ones_b = self.sb("ones_b16", [128, 128], BF16)
            ob_, on_ = self.ones_b, self.ones
            self.op("act", lambda e: e.activation(out=ob_[:], in_=on_[:], func=AF.Copy), r=["ones"], w=["ones_b16"])
            self.ones_b_phase = self.prefix
        ones = self.ones_b
        for n0 in range(0, N, 512):
            xa, xk = xget(n0)
            b = self.bank()
            ps = self.banks[b][:, 0:512]
            for kt in range(8):
                sq, sk = self.rbuf("sq", [128, 512], BF16)
                self.op("act", lambda e, sq=sq, kt=kt, xa=xa: e.activation(out=sq[:], in_=xa[:, kt, :], func=AF.Square),
                        r=[xk], w=[sk])
                self.op("pe", lambda e, sq=sq, kt=kt, ps=ps: e.matmul(ps, lhsT=ones[:], rhs=sq[:], start=(kt == 0), stop=(kt == 7)),
                        r=["ones_b16", sk], w=[("bank", b)])
            rs, rk = self.rbuf("rstd", [128, 512], F32)
            self.op("act", lambda e, rs=rs, ps=ps: e.activation(out=rs[:], in_=ps, func=AF.Sqrt, scale=1.0 / D, bias=epsc[:]),
                    r=[("bank", b), "epsc"], w=[rk])
            self.op("dve", lambda e, rs=rs: e.reciprocal(out=rs[:], in_=rs[:]), r=[rk], w=[rk])
            for kt in range(8):
                tm, tk = self.rbuf("nt", [128, 512], F32)
                self.op("dve", lambda e, tm=tm, kt=kt, xa=xa, rs=rs: e.scalar_tensor_tensor(
                    out=tm[:], in0=xa[:, kt, :], scalar=wm[:, kt:kt + 1], in1=rs[:], op0=ALU.mult, op1=ALU.mult),
                    r=[xk, wm.name, rk], w=[tk])
                for (ot, ok, oeng) in outs:
                    if callable(ot):
                        dst, ok = ot(kt, n0)
                    else:
                        dst = ot[:, kt, n0:n0 + 512]
                    if shc is None:
                        if oeng == "act":
                            self.op("act", lambda e, dst=dst, tm=tm: e.activation(out=dst, in_=tm[:], func=AF.Copy), r=[tk], w=[ok])
                        else:
                            self.op(oeng, lambda e, dst=dst, tm=tm: e.tensor_copy(out=dst, in_=tm[:]), r=[tk], w=[ok])
                    else:
                        sh = shc[:, kt:kt + 1]
                        if oeng == "act":
                            self.op("act", lambda e, dst=dst, tm=tm, sh=sh: e.activation(out=dst, in_=tm[:], func=AF.Identity, bias=sh),
                                    r=[tk] + list(ckeys), w=[ok])
                        else:
                            self.op(oeng, lambda e, dst=dst, tm=tm, sh=sh: e.tensor_scalar(out=dst, in0=tm[:], scalar1=sh, scalar2=None, op0=ALU.add),
                                    r=[tk] + list(ckeys), w=[ok])
            if after is not None:
                after(n0)

    def consts(self):
        self.epsc = self.sb("epsc", [128, 1], F32)
        epsc = self.epsc
        self.op("pool", lambda e: e.memset(epsc[:], EPS), w=["epsc"])

    def ffn(self, hb, hkey, N, Wg, Wu, Wd, xres, xkey, gcol, gkeys, hidden, gate_b=None, gbkey=None):
        HF = DFF // 2
        for half in range(2):
            f0 = half * HF

            def evac_gu(m0, msz, n0, nsz, pss, f0=f0):
                (pg, kg), (pu, ku) = pss
                mt = (m0 - f0) // 128
                ta, tak = self.rbuf("fa", [128, 512], F32)
                tb, tbk = self.rbuf("fb", [128, 512], F32)
                self.op("act", lambda e: e.activation(out=ta[0:msz, 0:nsz], in_=pg, func=AF.Silu), r=[kg], w=[tak])
                dst = hidden[0:msz, mt, n0:n0 + nsz]
                if gate_b is None:
                    self.op("dve", lambda e: e.tensor_tensor(out=dst, in0=ta[0:msz, 0:nsz], in1=pu, op=ALU.mult),
                            r=[tak, ku], w=["hidden"])
                else:
                    self.op("dve", lambda e: e.tensor_tensor(out=tb[0:msz, 0:nsz], in0=ta[0:msz, 0:nsz], in1=pu, op=ALU.mult),
                            r=[tak, ku], w=[tbk])
                    self.op("pool", lambda e: e.tensor_tensor(out=dst, in0=tb[0:msz, 0:nsz], in1=gate_b[0:msz, n0:n0 + nsz], op=ALU.mult),
                            r=[tbk, gbkey], w=["hidden"])
            WgH = Wg[:, f0:f0 + HF]
            WuH = Wu[:, f0:f0 + HF]
            self.linear([WgH, WuH], 0, D, HF, lambda kt, n0, nsz: hb[:, kt, n0:n0 + nsz], [hkey], N,
                        lambda m0, msz, n0, nsz, pss, f0=f0: evac_gu(m0 + f0, msz, n0, nsz, pss))

            def evac_d(m0, msz, n0, nsz, pss):
                ps, pk = pss[0]
                mt = m0 // 128
                dst = xres[:, mt, n0:n0 + nsz]
                self.op("dve", lambda e: e.scalar_tensor_tensor(out=dst, in0=ps, scalar=gcol[:, mt:mt + 1], in1=dst,
                                                                 op0=ALU.mult, op1=ALU.add),
                        r=[pk, xkey] + list(gkeys), w=[xkey])
            self.linear([Wd], f0, HF, D, lambda kt, n0, nsz: hidden[:, kt, n0:n0 + nsz], ["hidden"], N, evac_d)


def fm_cols(v):
    v = np.asarray(v, dtype=np.float32)
    return np.ascontiguousarray(v.reshape(-1, 128).T)


IN_COLS = 3096
NT = 2048


def io_L1(P):
    return dict(xT=P.inp("xT", [D, NT]), ccol=P.inp("ccol", [128, 8]), mod_w=P.inp("mod_w", [D, 3 * D]), mod_b=P.inp("mod_b", [128, 24]),
                norm_w=P.inp("norm_w", [128, 8]), w_in=P.inp("w_in", [D, IN_COLS]), projT=P.out("projT", [IN_COLS, NT]), modc=P.out("modc", [128, 24]))


def build_L1():
    P = K()
    phase_L1(P, io_L1(P))
    return P.build()


def phase_L1(P, io):
    P.consts()
    xT, ccol, mw, mb, nw, w_in, projT, modo = (io[k] for k in ("xT", "ccol", "mod_w", "mod_b", "norm_w", "w_in", "projT", "modc"))
    csil = P.silu_c(ccol)
    bcols = P.load("mod_b_sb", mb, [128, 24])
    normw = P.load("norm_w_sb", nw, [128, 8])
    modc, mkey = P.adaln("m0", csil, mw, bcols, io.get("pre0"))
    P.dma(modo, modc[:], r=[mkey], q="pool")
    wm = P.wmod("m0", modc, mkey, normw)
    ntok = xT.shape[1]
    ncols = w_in.shape[1]
    hb = P.sb("hb", [128, 8, ntok], BF16)
    xv = xT.rearrange("(kt p) n -> p kt n", p=128)

    def xget(n0):
        xb, xk = P.rbuf("xchunk", [128, 8, 512], F32)
        P.dma(xb[:], xv[:, :, n0:n0 + 512], w=[xk])
        return xb, xk
    P.rmsnorm(xget, ntok, wm, modc, [mkey], [(hb, "hb", "act")])

    gath = io.get("gath")
    gdone = set()

    def evac(m0, msz, n0, nsz, pss):
        ps, pk = pss[0]
        ob, okk = P.rbuf("osb", [128, 512], F32, n=3)
        P.op("dve", lambda e: e.tensor_copy(out=ob[0:msz, 0:nsz], in_=ps), r=[pk], w=[okk])
        P.dma(projT[m0:m0 + msz, n0:n0 + nsz], ob[0:msz, 0:nsz], r=[okk], w=[("projrow", m0 // 128)], q="pool")
        if gath is not None and n0 + nsz == ntok:
            for k, (b0, b1) in enumerate(gath.bounds):
                if k not in gdone and b1 <= m0 + msz:
                    gdone.add(k)
                    P.allgather_pair(gath.own[b0:b1, :], gath.dsts[k], r=[("projrow", t) for t in range(b0 // 128, (b1 - 1) // 128 + 1)])
    P.linear([w_in], 0, D, ncols, lambda kt, n0, nsz: hb[:, kt, n0:n0 + nsz], ["hb"], ntok, evac)
    assert gath is None or len(gdone) == len(gath.bounds)


NP_ = 1024


def load_cast_chunks(P, srcT, t0, N, dst, dkey):
    sv = srcT.rearrange("(kt p) n -> p kt n", p=128)
    for n0 in range(0, N, 512):
        xb, xk = P.rbuf("xchunk", [128, 8, 512], F32)
        P.dma(xb[:], sv[:, :, t0 + n0:t0 + n0 + 512], w=[xk])
        P.op("act", lambda e, xb=xb, n0=n0: e.activation(out=dst[:, :, n0:n0 + 512], in_=xb[:], func=AF.Copy), r=[xk], w=[dkey])


L3_IN = dict(x0T=[D, NT], mixT=[D, NT], modc0=[128, 24], w_o=[D, D], ccol=[128, 8], nw1=[128, 8], mw1=[D, 3 * D], mb1=[128, 24],
             Wg=[D, DFF], Wu=[D, DFF], Wd=[DFF, D], nw2=[128, 8], mw2=[D, 3 * D], mb2=[128, 24], w_in2=[D, D])
L3_OUT = dict(x2T=[D, NT], uT=[D, NT], modc2=[128, 24])


def build_L3():
    P = K()
    io = {k: P.inp(k, v) for k, v in L3_IN.items()}
    io.update({k: P.out(k, v) for k, v in L3_OUT.items()})
    phase_L3(P, io)
    return P.build()


def phase_L3(P, io):
    P.consts()
    (x0T, mixT, modc0_d, w_o, ccol, nw1, mw1, mb1, Wg, Wu, Wd, nw2, mw2, mb2, w_in2, x2T, uT, modo) = (
        io.get(k) for k in ("x0T", "mixT", "modc0", "w_o", "ccol", "nw1", "mw1", "mb1", "Wg", "Wu", "Wd", "nw2", "mw2", "mb2", "w_in2",
                            "x2T", "uT", "modc2"))
    csil = P.silu_c(ccol)
    sel3 = P.load("sel_sb", io["sel"], [128, 2]) if "sel" in io else None
    modc0 = P.load("modc0_sb", modc0_d, [128, 24])
    b1 = P.load("mb1_sb", mb1, [128, 24])
    b2 = P.load("mb2_sb", mb2, [128, 24])
    n1 = P.load("nw1_sb", nw1, [128, 8])
    n2 = P.load("nw2_sb", nw2, [128, 8])
    modc1, mk1 = P.adaln("m1", csil, mw1, b1, io.get("pre1"))
    modc2, mk2 = P.adaln("m2", csil, mw2, b2, io.get("pre2"))
    P.dma(modo, modc2[:], r=[mk2], q="pool")
    wm1 = P.wmod("m1", modc1, mk1, n1)
    wm2 = P.wmod("m2", modc2, mk2, n2)

    xres = P.sb("xres", [128, 8, NP_], F32)
    hbA = P.sb("hbA", [128, 8, NP_], BF16)
    hbB = P.sb("hbB", [128, 8, NP_], BF16)
    hidden = P.sb("hidden", [128, 11, NP_], BF16)
    x0v = x0T.rearrange("(kt p) n -> p kt n", p=128)
    x2v = x2T.rearrange("(kt p) n -> p kt n", p=128)
    for ps_ in range(NT // NP_):
        t0 = ps_ * NP_
        P.dma(xres[:], x0v[:, :, t0:t0 + NP_], w=["xres"])
        if "mix_g" in io:
            mg = io["mix_g"]
            for n0 in range(0, NP_, 512):
                mixbf = bool(io.get("mix_bf16"))
                if mixbf:
                    xb2, xk2 = P.rbuf("xchunkb", [128, 8, 512], BF16)
                else:
                    xb, xk = P.rbuf("xchunk", [128, 8, 512], F32)
                    xb2, xk2 = P.rbuf("xchunk", [128, 8, 512], F32)

                def mpieces(h):
                    c0 = h * NT + t0 + n0
                    out = []
                    for kt_, (rk, r0) in enumerate(((0, 0), (0, 128), (1, 0), (1, 128), (0, 256), (0, 384), (1, 256), (1, 384))):
                        src = mg[rk][r0:r0 + 128, c0:c0 + 512]
                        out.append(((lambda base, kt_=kt_: base[:, kt_, :]), src))
                    return out
                if mixbf:
                    P.sel_load(hbA[:, :, n0:n0 + 512], xb2[:], mpieces(0), mpieces(1), sel3, "hbA", xk2, extra_r=mg.keys())
                else:
                    P.sel_load(xb[:], xb2[:], mpieces(0), mpieces(1), sel3, xk, xk2)
                    P.op("act", lambda e, xb=xb, n0=n0: e.activation(out=hbA[:, :, n0:n0 + 512], in_=xb[:], func=AF.Copy), r=[xk], w=["hbA"])
        else:
            load_cast_chunks(P, mixT, t0, NP_, hbA, "hbA")

        def evac_o(m0, msz, n0, nsz, pss):
            ps, pk = pss[0]
            mt = m0 // 128
            dst = xres[:, mt, n0:n0 + nsz]
            P.op("dve", lambda e: e.scalar_tensor_tensor(out=dst, in0=ps, scalar=modc0[:, 16 + mt:17 + mt], in1=dst,
                                                          op0=ALU.mult, op1=ALU.add),
                 r=[pk, "xres", "modc0_sb"], w=["xres"])
        P.linear([w_o], 0, D, D, lambda kt, n0, nsz: hbA[:, kt, n0:n0 + nsz], ["hbA"], NP_, evac_o)
        P.rmsnorm(lambda n0: (xres[:, :, n0:n0 + 512], "xres"), NP_, wm1, modc1, [mk1], [(hbB, "hbB", "act")])
        P.ffn(hbB, "hbB", NP_, Wg, Wu, Wd, xres, "xres", modc1[:, 16:24], [mk1], hidden)
        P.dma(x2v[:, :, t0:t0 + NP_], xres[:], r=["xres"], q="pool")
        P.rmsnorm(lambda n0: (xres[:, :, n0:n0 + 512], "xres"), NP_, wm2, modc2, [mk2], [(hbA, "hbA", "act")])

        def evac_u(m0, msz, n0, nsz, pss, t0=t0):
            ps, pk = pss[0]
            ob, okk = P.rbuf("osb", [128, 512], F32, n=3)
            P.op("dve", lambda e: e.tensor_copy(out=ob[0:msz, 0:nsz], in_=ps), r=[pk], w=[okk])
            P.dma(uT[m0:m0 + msz, t0 + n0:t0 + n0 + nsz], ob[0:msz, 0:nsz], r=[okk], q="pool")
            if io.get("uT_bf") is not None:
                obb, obk2 = P.rbuf("osbb", [128, 512], BF16, n=3)
                P.op("act", lambda e: e.activation(out=obb[0:msz, 0:nsz], in_=ob[0:msz, 0:nsz], func=AF.Copy), r=[okk], w=[obk2])
                P.dma(io["uT_bf"][m0:m0 + msz, t0 + n0:t0 + n0 + nsz], obb[0:msz, 0:nsz], r=[obk2], q="pool")
        P.linear([w_in2], 0, D, D, lambda kt, n0, nsz: hbA[:, kt, n0:n0 + nsz], ["hbA"], NP_, evac_u)


L5_IN = dict(x2T=[D, NT], s5yT=[D, NT], uT=[D, NT], dskip=[128, 8], w_glu=[D, D], w_o2=[D, D], modc2=[128, 24], ccol=[128, 8],
             nw3=[128, 8], mw3=[D, 3 * D], mb3=[128, 24], router=[D, NE], Eg=[NE, D, DFF], Eu=[NE, D, DFF], Ed=[NE, DFF, D], fnorm=[128, 8])


def build_L5():
    P = K()
    io = {k: P.inp(k, v) for k, v in L5_IN.items()}
    io["outT"] = P.out("outT", [D, NT])
    phase_L5(P, io)
    return P.build()


def phase_L5(P, io):
    P.consts()
    P.make_ident()
    IDENT, ONES = P.ident, P.ones
    (x2T, s5yT, uT, dsk, w_glu, w_o2, modc2_d, ccol, nw3, mw3, mb3, router, Eg, Eu, Ed, fnw, outT) = (
        io.get(k) for k in ("x2T", "s5yT", "uT", "dskip", "w_glu", "w_o2", "modc2", "ccol", "nw3", "mw3", "mb3", "router", "Eg", "Eu", "Ed",
                        "fnorm", "outT"))
    sel = P.load("sel_sb", io["sel"], [128, 2]) if "sel" in io else None
    syg = io.get("s5y_g")
    csil = P.silu_c(ccol)
    modc2 = P.load("modc2_sb", modc2_d, [128, 24])
    b3 = P.load("mb3_sb", mb3, [128, 24])
    n3 = P.load("nw3_sb", nw3, [128, 8])
    fn = P.load("fnorm_sb", fnw, [128, 8])
    dcol = P.load("dskip_sb", dsk, [128, 8])
    rt = P.load("router_sb", router.rearrange("(kt p) e -> p kt e", p=128), [128, 8, NE])
    modc3, mk3 = P.adaln("m3", csil, mw3, b3, io.get("pre3"))
    wm3 = P.wmod("m3", modc3, mk3, n3)

    xres = P.sb("xres", [128, 8, NP_], F32)
    hbA = P.sb("hbA", [128, 8, NP_], BF16)
    hidden = P.sb("hidden", [128, 11, NP_], BF16)
    gate_b = P.sb("gate_b", [128, NE, NP_], BF16)
    ygb = hidden
    x2v = x2T.rearrange("(kt p) n -> p kt n", p=128)
    syv = s5yT.rearrange("(kt p) n -> p kt n", p=128) if s5yT is not None else None
    uv = uT.rearrange("(kt p) n -> p kt n", p=128)
    ov = outT.rearrange("(kt p) n -> p kt n", p=128)
    for ps_ in range(NT // NP_):
        t0 = ps_ * NP_
        if sel is None or syg is not None:
            P.dma(xres[:], x2v[:, :, t0:t0 + NP_], w=["xres"])
        else:
            for n0 in range(0, NP_, 512):
                xb, xk = P.rbuf("xchunk", [128, 8, 512], F32)
                P.dma(xres[:, :, n0:n0 + 512], x2v[:, :, t0 + n0:t0 + n0 + 512], w=["xres"])
                P.dma(xb[:], x2v[:, :, NT + t0 + n0:NT + t0 + n0 + 512], w=[xk])
                P.op("dve", lambda e, n0=n0: e.tensor_scalar(out=xres[:, :, n0:n0 + 512], in0=xres[:, :, n0:n0 + 512], scalar1=sel[:, 0:1], scalar2=None, op0=ALU.mult),
                     r=["xres", "sel_sb"], w=["xres"])
                P.op("dve", lambda e, n0=n0, xb=xb: e.scalar_tensor_tensor(out=xres[:, :, n0:n0 + 512], in0=xb[:], scalar=sel[:, 1:2], in1=xres[:, :, n0:n0 + 512],
                                                                          op0=ALU.mult, op1=ALU.add), r=["xres", xk, "sel_sb"], w=["xres"])
        for n0 in range(0, NP_, 512):
            if sel is None:
                sb_, sk = P.rbuf("xchunk", [128, 8, 512], F32)
                P.dma(sb_[:], syv[:, :, t0 + n0:t0 + n0 + 512], w=[sk])
                ub, uk = P.rbuf("xchunk", [128, 8, 512], F32)
                P.dma(ub[:], uv[:, :, t0 + n0:t0 + n0 + 512], w=[uk])
            for kt in range(8):
                ta, tak = P.rbuf("fa", [128, 512], F32)
                tb, tbk = P.rbuf("fb", [128, 512], F32)
                if syg is not None:
                    c0 = t0 + n0
                    pcs = []
                    rk_, r0_ = kt // 4, (kt % 4) * 128
                    for src in (syg[rk_][r0_:r0_ + 128, c0:c0 + 512], syg[rk_][r0_:r0_ + 128, NT + c0:NT + c0 + 512], uv[:, kt, c0:c0 + 512]):
                        pb_, pk_ = P.rbuf("pc", [128, 512], F32, n=6)
                        P.dma(pb_[:], src, w=[pk_])
                        pcs.append((pb_, pk_))
                    (s0, s0k), (s1, s1k), (u0, u0k) = pcs
                    P.op("dve", lambda e, s0=s0: e.tensor_scalar(out=s0[:], in0=s0[:], scalar1=sel[:, 0:1], scalar2=None, op0=ALU.mult), r=[s0k, "sel_sb"], w=[s0k])
                    P.op("dve", lambda e, s0=s0, s1=s1: e.scalar_tensor_tensor(out=s0[:], in0=s1[:], scalar=sel[:, 1:2], in1=s0[:], op0=ALU.mult, op1=ALU.add),
                         r=[s0k, s1k, "sel_sb"], w=[s0k])
                    P.op("dve", lambda e, ta=ta, u0=u0, s0=s0, kt=kt: e.scalar_tensor_tensor(out=ta[:], in0=u0[:], scalar=dcol[:, kt:kt + 1], in1=s0[:], op0=ALU.mult, op1=ALU.add),
                         r=[s0k, u0k, "dskip_sb"], w=[tak])
                elif sel is not None:
                    c0 = t0 + n0
                    pcs = []
                    for (src, off) in ((syv, 0), (uv, 0), (syv, NT), (uv, NT)):
                        pb_, pk_ = P.rbuf("pc", [128, 512], F32, n=8)
                        P.dma(pb_[:], src[:, kt, off + c0:off + c0 + 512], w=[pk_])
                        pcs.append((pb_, pk_))
                    (s0, s0k), (u0, u0k), (s1, s1k), (u1, u1k) = pcs
                    P.op("dve", lambda e, u0=u0, s0=s0, kt=kt: e.scalar_tensor_tensor(out=s0[:], in0=u0[:], scalar=dcol[:, kt:kt + 1], in1=s0[:], op0=ALU.mult, op1=ALU.add),
                         r=[s0k, u0k, "dskip_sb"], w=[s0k])
                    P.op("dve", lambda e, u1=u1, s1=s1, kt=kt: e.scalar_tensor_tensor(out=s1[:], in0=u1[:], scalar=dcol[:, kt:kt + 1], in1=s1[:], op0=ALU.mult, op1=ALU.add),
                         r=[s1k, u1k, "dskip_sb"], w=[s1k])
                    P.op("dve", lambda e, s0=s0: e.tensor_scalar(out=s0[:], in0=s0[:], scalar1=sel[:, 0:1], scalar2=None, op0=ALU.mult), r=[s0k, "sel_sb"], w=[s0k])
                    P.op("dve", lambda e, ta=ta, s0=s0, s1=s1: e.scalar_tensor_tensor(out=ta[:], in0=s1[:], scalar=sel[:, 1:2], in1=s0[:], op0=ALU.mult, op1=ALU.add),
                         r=[s0k, s1k, "sel_sb"], w=[tak])
                else:
                    P.op("dve", lambda e, ta=ta, ub=ub, sb_=sb_, kt=kt: e.scalar_tensor_tensor(
                        out=ta[:], in0=ub[:, kt, :], scalar=dcol[:, kt:kt + 1], in1=sb_[:, kt, :], op0=ALU.mult, op1=ALU.add),
                        r=[sk, uk, "dskip_sb"], w=[tak])
                P.op("act", lambda e, ta=ta, tb=tb: e.activation(out=tb[:], in_=ta[:], func=AF.Square), r=[tak], w=[tbk])
                P.op("dve", lambda e, tb=tb: e.tensor_scalar(out=tb[:], in0=tb[:], scalar1=0.044715, scalar2=1.0, op0=ALU.mult, op1=ALU.add),
                     r=[tbk], w=[tbk])
                P.op("dve", lambda e, ta=ta, tb=tb: e.tensor_tensor(out=tb[:], in0=tb[:], in1=ta[:], op=ALU.mult), r=[tak, tbk], w=[tbk])
                P.op("act", lambda e, tb=tb: e.activation(out=tb[:], in_=tb[:], func=AF.Sigmoid, scale=1.5957691216057308), r=[tbk], w=[tbk])
                P.op("pool", lambda e, ta=ta, tb=tb, kt=kt, n0=n0: e.tensor_tensor(out=hbA[:, kt, n0:n0 + 512], in0=ta[:], in1=tb[:], op=ALU.mult),
                     r=[tak, tbk], w=["hbA"])

        def evac_glu(m0, msz, n0, nsz, pss):
            ps, pk = pss[0]
            mt = m0 // 128
            ta, tak = P.rbuf("fa", [128, 512], F32)
            P.op("act", lambda e: e.activation(out=ta[:, 0:nsz], in_=ps, func=AF.Sigmoid), r=[pk], w=[tak])
            P.op("dve", lambda e: e.tensor_tensor(out=ygb[:, mt, n0:n0 + nsz], in0=hbA[:, mt, n0:n0 + nsz], in1=ta[:, 0:nsz], op=ALU.mult),
                 r=[tak, "hbA"], w=["hidden"])
        P.linear([w_glu], 0, D, D, lambda kt, n0, nsz: hbA[:, kt, n0:n0 + nsz], ["hbA"], NP_, evac_glu)

        def evac_o(m0, msz, n0, nsz, pss):
            ps, pk = pss[0]
            mt = m0 // 128
            dst = xres[:, mt, n0:n0 + nsz]
            P.op("dve", lambda e: e.scalar_tensor_tensor(out=dst, in0=ps, scalar=modc2[:, 16 + mt:17 + mt], in1=dst,
                                                          op0=ALU.mult, op1=ALU.add),
                 r=[pk, "xres", "modc2_sb"], w=["xres"])
        P.linear([w_o2], 0, D, D, lambda kt, n0, nsz: ygb[:, kt, n0:n0 + nsz], ["hidden"], NP_, evac_o)

        cur = {}

        def xget(n0):
            cur["hf"], cur["hfk"] = P.rbuf("xchunk", [128, 8, 512], F32)
            return xres[:, :, n0:n0 + 512], "xres"

        def hf_dst(kt, n0):
            return cur["hf"][:, kt, :], cur["hfk"]

        def router_chunk(n0):
            hf, hfk = cur["hf"], cur["hfk"]
            T = []
            for tt in range(4):
                t = dict(tt=tt)
                t["b"] = P.bank()
                t["lps"] = P.banks[t["b"]][:, 0:NE]
                t["lg"], t["lgk"] = P.rbuf("lg", [128, NE], F32, n=4)
                t["l2"], t["l2k"] = P.rbuf("lg2", [128, NE], F32, n=4)
                t["sc"], t["sck"] = P.rbuf("rsc", [128, 4], F32, n=4)
                t["dg"], t["dgk"] = P.rbuf("dg", [128, NE, 128], F32, n=4)
                T.append(t)

            def s_logits(t):
                tt, lps = t["tt"], t["lps"]

                def mm(e):
                    ins = None
                    for kt in range(8):
                        ins = e.matmul(lps, lhsT=hf[:, kt, tt * 128:(tt + 1) * 128], rhs=rt[:, kt, :], start=(kt == 0), stop=(kt == 7))
                    return ins
                P.op("pe", mm, r=[hfk, "router_sb"], w=[("bank", t["b"])])
            stages = [
                s_logits,
                lambda t: P.op("dve", lambda e: e.tensor_copy(out=t["lg"][:], in_=t["lps"]), r=[("bank", t["b"])], w=[t["lgk"]]),
                lambda t: P.op("dve", lambda e: e.tensor_reduce(out=t["sc"][:, 0:1], in_=t["lg"][:], axis=AX.X, op=ALU.max), r=[t["lgk"]], w=[t["sck"]]),
                lambda t: P.op("dve", lambda e: e.tensor_scalar(out=t["l2"][:], in0=t["lg"][:], scalar1=t["sc"][:, 0:1], scalar2=-1e30,
                                                                  op0=ALU.is_equal, op1=ALU.mult), r=[t["lgk"], t["sck"]], w=[t["l2k"]]),
                lambda t: P.op("dve", lambda e: e.tensor_tensor(out=t["l2"][:], in0=t["l2"][:], in1=t["lg"][:], op=ALU.add), r=[t["lgk"], t["l2k"]], w=[t["l2k"]]),
                lambda t: P.op("dve", lambda e: e.tensor_reduce(out=t["sc"][:, 1:2], in_=t["l2"][:], axis=AX.X, op=ALU.max), r=[t["l2k"], t["sck"]], w=[t["sck"]]),
                lambda t: P.op("dve", lambda e: e.tensor_scalar(out=t["sc"][:, 2:3], in0=t["sc"][:, 0:1], scalar1=-1.0, scalar2=None, op0=ALU.mult),
                               r=[t["sck"]], w=[t["sck"]]),
                lambda t: P.op("dve", lambda e: e.tensor_scalar(out=t["l2"][:], in0=t["lg"][:], scalar1=t["sc"][:, 1:2], scalar2=None, op0=ALU.is_ge),
                               r=[t["lgk"], t["sck"], t["l2k"]], w=[t["l2k"]]),
                lambda t: P.op("act", lambda e: e.activation(out=t["lg"][:], in_=t["lg"][:], func=AF.Exp, bias=t["sc"][:, 2:3]), r=[t["lgk"], t["sck"]], w=[t["lgk"]]),
                lambda t: P.op("dve", lambda e: e.tensor_tensor(out=t["lg"][:], in0=t["lg"][:], in1=t["l2"][:], op=ALU.mult), r=[t["lgk"], t["l2k"]], w=[t["lgk"]]),
                lambda t: P.op("dve", lambda e: e.tensor_reduce(out=t["sc"][:, 3:4], in_=t["lg"][:], axis=AX.X, op=ALU.add), r=[t["lgk"], t["sck"]], w=[t["sck"]]),
                lambda t: P.op("dve", lambda e: e.reciprocal(out=t["sc"][:, 3:4], in_=t["sc"][:, 3:4]), r=[t["sck"]], w=[t["sck"]]),
                lambda t: P.op("dve", lambda e: e.tensor_scalar(out=t["lg"][:], in0=t["lg"][:], scalar1=t["sc"][:, 3:4], scalar2=None, op0=ALU.mult),
                               r=[t["lgk"], t["sck"]], w=[t["lgk"]]),
            ]
            for st_ in stages:
                for t in T:
                    st_(t)
            for t in T:
                for ex in range(NE):
                    P.op("dve", lambda e, t=t, ex=ex: e.tensor_scalar(out=t["dg"][:, ex, :], in0=IDENT[:], scalar1=t["lg"][:, ex:ex + 1], scalar2=None, op0=ALU.mult),
                         r=["ident", t["lgk"]], w=[t["dgk"]])
            for t in T:
                tok = n0 + t["tt"] * 128
                for half in range(2):
                    b2 = P.bank()
                    gps = P.banks[b2][:, 0:512]
                    P.op("pe", lambda e, gps=gps, t=t, half=half: e.matmul(gps, lhsT=ONES[:], rhs=t["dg"][:, 4 * half:4 * half + 4, :], start=True, stop=True),
                         r=["ones", t["dgk"]], w=[("bank", b2)])
                    P.op("act", lambda e, gps=gps, half=half, tok=tok: e.activation(
                        out=gate_b[:, 4 * half:4 * half + 4, tok:tok + 128], in_=gps.rearrange("p (e n) -> p e n", e=4), func=AF.Copy),
                        r=[("bank", b2)], w=["gate_b"])
        P.rmsnorm(xget, NP_, wm3, modc3, [mk3], [(hbA, "hbA", "act"), (hf_dst, None, "pool")], after=router_chunk)

        for ex in range(NE):
            P.ffn(hbA, "hbA", NP_, Eg[ex], Eu[ex], Ed[ex], xres, "xres", modc3[:, 16:24], [mk3], hidden,
                  gate_b=gate_b[:, ex, :], gbkey="gate_b")

        def xget2(n0):
            cur["o"], cur["ok"] = P.rbuf("xchunk", [128, 8, 512], F32)
            return xres[:, :, n0:n0 + 512], "xres"

        def o_dst(kt, n0):
            return cur["o"][:, kt, :], cur["ok"]

        def store(n0, t0=t0):
            P.dma(ov[:, :, t0 + n0:t0 + n0 + 512], cur["o"][:], r=[cur["ok"]], q="pool")
        P.rmsnorm(xget2, NP_, fn, None, [], [(o_dst, None, "act")], after=store)


NJ = S // 8
JH = 256


def io_L4(P):
    io = dict(uT=P.inp("uT", [512, S]), s5yT=P.out("s5yT", [512, S]))
    for n in ("lamr", "lami", "logdt"):
        io[n] = P.inp(n + "_d", [128, 16])
    for n in ("br", "bi", "cr", "ci"):
        io[n] = P.inp(n + "_d", [128, 16, 16])
    return io


def build_L4(stage=9):
    P = K()
    phase_L4(P, io_L4(P), stage)
    return P.build()


def phase_L4(P, io, stage=9):
    P.make_ident()
    IDENT, ONES = P.ident, P.ones
    uT_d = io.get("uT")
    yT_d = io["s5yT"]
    sel4 = P.load("sel_sb", io["sel"], [128, 2]) if "sel" in io else None
    sc_in = {n: P.load(n, io[n], [128, 16]) for n in ("lamr", "lami", "logdt")}
    vin = {n: P.load(n, io[n], [128, 16, 16]) for n in ("br", "bi", "cr", "ci")}

    def act(o, i, func, **kw):
        P.op("act", lambda e: e.activation(out=o[0], in_=i[0], func=func, **kw), r=[i[1]], w=[o[1]])

    def tt(o, a, b, op, eng="dve"):
        P.op(eng, lambda e: e.tensor_tensor(out=o[0], in0=a[0], in1=b[0], op=op), r=[a[1], b[1], o[1]], w=[o[1]])

    def ts(o, a, s1, s2, op0, op1=None):
        if op1 is None:
            P.op("dve", lambda e: e.tensor_scalar(out=o[0], in0=a[0], scalar1=s1, scalar2=None, op0=op0), r=[a[1], o[1]], w=[o[1]])
        else:
            P.op("dve", lambda e: e.tensor_scalar(out=o[0], in0=a[0], scalar1=s1, scalar2=s2, op0=op0, op1=op1), r=[a[1], o[1]], w=[o[1]])

    def new(name, shape=(128, 16), dt=F32):
        t = P.sb(name, list(shape), dt)
        return (t[:], name), t

    lamr = (sc_in["lamr"][:], "lamr")
    lami = (sc_in["lami"][:], "lami")
    logdt = (sc_in["logdt"][:], "logdt")
    dt_, _ = new("dt")
    mag, _ = new("mag")
    ang, _ = new("ang")
    t1, _ = new("t1")
    t2, _ = new("t2")
    sn, _ = new("sn")
    cs, _ = new("cs")
    ar, art = new("ar")
    ai, ait = new("ai")
    PI = float(np.pi)
    act(dt_, logdt, AF.Exp)
    tt(t1, lamr, dt_, ALU.mult)
    act(mag, t1, AF.Exp)
    tt(ang, lami, dt_, ALU.mult)
    ki_t = P.sb("ki", [128, 16], mybir.dt.int32)
    t3, _ = new("t3")

    def wrap_angle(dst, src, offset):
        ts(t3, src, 1.0 / (2 * PI), offset / (2 * PI), ALU.mult, ALU.add)
        P.op("dve", lambda e: e.tensor_copy(out=ki_t[:], in_=t3[0]), r=["t3"], w=["ki"])
        P.op("dve", lambda e: e.tensor_copy(out=t3[0], in_=ki_t[:]), r=["ki"], w=["t3"])
        ts(dst, src, offset, None, ALU.add)
        P.op("dve", lambda e: e.scalar_tensor_tensor(out=dst[0], in0=t3[0], scalar=-2 * PI, in1=dst[0], op0=ALU.mult, op1=ALU.add),
             r=["t3", dst[1]], w=[dst[1]])
        ts(t3, dst, PI, None, ALU.is_gt)
        P.op("dve", lambda e: e.scalar_tensor_tensor(out=dst[0], in0=t3[0], scalar=-2 * PI, in1=dst[0], op0=ALU.mult, op1=ALU.add),
             r=["t3", dst[1]], w=[dst[1]])
        ts(dst, dst, PI, None, ALU.min)
        ts(dst, dst, -PI, None, ALU.max)
    wrap_angle(t1, ang, 0.0)
    act(sn, t1, AF.Sin)
    wrap_angle(t2, ang, 0.5 * PI)
    act(cs, t2, AF.Sin)
    tt(ar, mag, cs, ALU.mult)
    tt(ai, mag, sn, ALU.mult)
    den, _ = new("den")
    nr, _ = new("nr")
    fr, frt = new("fr")
    fi, fit = new("fi")
    tt(den, lamr, lamr, ALU.mult)
    tt(t2, lami, lami, ALU.mult)
    tt(den, den, t2, ALU.add)
    P.op("dve", lambda e: e.reciprocal(out=den[0], in_=den[0]), r=["den"], w=["den"])
    ts(nr, ar, -1.0, None, ALU.add)
    tt(fr, nr, lamr, ALU.mult)
    tt(t2, ai, lami, ALU.mult)
    tt(fr, fr, t2, ALU.add)
    tt(fr, fr, den, ALU.mult)
    tt(fi, ai, lamr, ALU.mult)
    tt(t2, nr, lami, ALU.mult)
    tt(fi, fi, t2, ALU.subtract)
    tt(fi, fi, den, ALU.mult)
    bbr, bbrt = new("bbr", (128, 16, 16))
    bbi, bbit = new("bbi", (128, 16, 16))
    tv, tvt = new("tv", (128, 16, 16))
    frb = (frt[:].unsqueeze(2).to_broadcast([128, 16, 16]), "fr")
    fib = (fit[:].unsqueeze(2).to_broadcast([128, 16, 16]), "fi")
    br = (vin["br"][:], "br")
    bi = (vin["bi"][:], "bi")
    tt(bbr, frb, br, ALU.mult)
    tt(tv, fib, bi, ALU.mult)
    tt(bbr, bbr, tv, ALU.subtract)
    tt(bbi, frb, bi, ALU.mult)
    tt(tv, fib, br, ALU.mult)
    tt(bbi, bbi, tv, ALU.add)
    pwr_t = P.sb("pwr", [128, 16, 9], F32)
    pwi_t = P.sb("pwi", [128, 16, 9], F32)
    npr_t = P.sb("npr", [128, 16, 8], F32)
    npi_t = P.sb("npi", [128, 16, 8], F32)
    rpr_t = P.sb("rpr", [128, 16, 8], F32)
    rpi_t = P.sb("rpi", [128, 16, 8], F32)
    iar, _ = new("iar")
    iai, _ = new("iai")
    tt(t1, ar, ar, ALU.mult)
    tt(t2, ai, ai, ALU.mult)
    tt(t1, t1, t2, ALU.add)
    P.op("dve", lambda e: e.reciprocal(out=t1[0], in_=t1[0]), r=["t1"], w=["t1"])
    tt(iar, ar, t1, ALU.mult)
    tt(iai, ai, t1, ALU.mult)
    ts(iai, iai, -1.0, None, ALU.mult)

    def powers(prt, pit, name_r, name_i, a_r, a_i, n):
        P.op("pool", lambda e: e.memset(prt[:, :, 0:1], 1.0), w=[name_r])
        P.op("pool", lambda e: e.memset(pit[:, :, 0:1], 0.0), w=[name_i])
        for m in range(1, n):
            pr0 = (prt[:, :, m - 1], name_r)
            pi0 = (pit[:, :, m - 1], name_i)
            pr1 = (prt[:, :, m], name_r)
            pi1 = (pit[:, :, m], name_i)
            tt(pr1, pr0, a_r, ALU.mult)
            tt(t2, pi0, a_i, ALU.mult)
            tt(pr1, pr1, t2, ALU.subtract)
            tt(pi1, pr0, a_i, ALU.mult)
            tt(t2, pi0, a_r, ALU.mult)
            tt(pi1, pi1, t2, ALU.add)
    powers(pwr_t, pwi_t, "pwr", "pwi", ar, ai, 9)
    powers(npr_t, npi_t, "npr", "npi", iar, iai, 8)
    for s_ in range(8):
        P.op("pool", lambda e, s_=s_: e.tensor_copy(out=rpr_t[:, :, s_], in_=pwr_t[:, :, 7 - s_]), r=["pwr"], w=["rpr"])
        P.op("pool", lambda e, s_=s_: e.tensor_copy(out=rpi_t[:, :, s_], in_=pwi_t[:, :, 7 - s_]), r=["pwi"], w=["rpi"])

    def cprod(outr, outi, xr, xi, pr_ap, pi_ap, pkeys, n, tmp, neg_imag=False):
        shape = [128, 16, n, 16]
        xrb = (xr[0].unsqueeze(2).to_broadcast(shape), xr[1])
        xib = (xi[0].unsqueeze(2).to_broadcast(shape), xi[1])
        prb = (pr_ap.unsqueeze(3).to_broadcast(shape), pkeys[0])
        pib = (pi_ap.unsqueeze(3).to_broadcast(shape), pkeys[1])
        tt(outr, xrb, prb, ALU.mult)
        tt(tmp, xib, pib, ALU.mult)
        tt(outr, outr, tmp, ALU.subtract)
        tt(outi, xrb, pib, ALU.mult)
        tt(tmp, xib, prb, ALU.mult)
        tt(outi, outi, tmp, ALU.add)
        if neg_imag:
            ts(outi, outi, -1.0, None, ALU.mult)

    cpr, cprt = new("cpr", (128, 16, 8, 16))
    cpi, cpit = new("cpi", (128, 16, 8, 16))
    ctmp, ctmpt = new("ctmp", (128, 16, 9, 16))
    ctmp8 = (ctmpt[:, :, 0:8, :], "ctmp")
    WB = P.sb("WB", [128, 32, 128], BF16)
    Wi = P.sb("Wi", [128, 32, 128], BF16)
    WCr = P.sb("WCr", [128, 16, 8, 16], BF16)
    WCi = P.sb("WCi", [128, 16, 8, 16], BF16)
    cprod(cpr, cpi, bbr, bbi, rpr_t[:], rpi_t[:], ["rpr", "rpi"], 8, ctmp8)
    for gl in range(32):
        q, base = gl // 2, 64 * (gl % 2)
        b = P.bank()
        ps = P.banks[b][:, 0:128]

        def mmt(e, q=q, base=base, ps=ps):
            e.matmul(ps[:, 0:64], lhsT=cprt[base:base + 64, q, :, :], rhs=IDENT[base:base + 64, base:base + 64], start=True, stop=True)
            return e.matmul(ps[:, 64:128], lhsT=cpit[base:base + 64, q, :, :], rhs=IDENT[base:base + 64, base:base + 64], start=True, stop=True)
        P.op("pe", mmt, r=["cpr", "cpi", "ident"], w=[("bank", b)])
        P.op("act", lambda e, gl=gl, ps=ps: e.activation(out=WB[:, gl, :], in_=ps, func=AF.Copy), r=[("bank", b)], w=["WB"])
    gr, grt = new("gr", (128, 16, 9, 16))
    gi, git = new("gi", (128, 16, 9, 16))
    cprod(gr, gi, (vin["cr"][:], "cr"), (vin["ci"][:], "ci"), pwr_t[:], pwi_t[:], ["pwr", "pwi"], 9, ctmp, neg_imag=True)
    P.op("act", lambda e: e.activation(out=WCr[:], in_=grt[:, :, 1:9, :], func=AF.Copy), r=["gr"], w=["WCr"])
    P.op("act", lambda e: e.activation(out=WCi[:], in_=git[:, :, 1:9, :], func=AF.Copy), r=["gi"], w=["WCi"])
    cprod(cpr, cpi, bbr, bbi, npr_t[:], npi_t[:], ["npr", "npi"], 8, ctmp8)
    mask = P.sb("mask", [128, 8, 16], F32)
    P.op("pool", lambda e: e.affine_select(out=mask[:], in_=ONES[:].rearrange("p (a b) -> p a b", a=8), pattern=[[16, 8], [0, 16]],
                                           compare_op=ALU.is_ge, fill=0.0, base=15, channel_multiplier=-1), r=["ones"], w=["mask"])
    for gl in range(32):
        q, base = gl // 2, 64 * (gl % 2)
        b = P.bank()
        ps = P.banks[b][:, 0:128]

        def mmi(e, q=q, base=base, ps=ps):
            e.matmul(ps, lhsT=cprt[base:base + 64, q, :, :], rhs=grt[base:base + 64, q, 0:8, :], start=True, stop=False)
            return e.matmul(ps, lhsT=cpit[base:base + 64, q, :, :], rhs=git[base:base + 64, q, 0:8, :], start=False, stop=True)
        P.op("pe", mmi, r=["cpr", "cpi", "gr", "gi"], w=[("bank", b)])
        P.op("dve", lambda e, gl=gl, ps=ps: e.tensor_tensor(out=Wi[:, gl, :], in0=ps, in1=mask[:].rearrange("p a b -> p (a b)"), op=ALU.mult),
             r=[("bank", b), "mask"], w=["Wi"])
    A8r2 = P.sb("A8r2", [128, 2, 16], F32)
    A8x = P.sb("A8x", [128, 2, 16], F32)
    P.op("pool", lambda e: e.tensor_copy(out=A8r2[:, 0, :], in_=pwr_t[:, :, 8]), r=["pwr"], w=["A8r2"])
    P.op("pool", lambda e: e.tensor_copy(out=A8r2[:, 1, :], in_=pwr_t[:, :, 8]), r=["pwr"], w=["A8r2"])
    P.op("pool", lambda e: e.tensor_copy(out=A8x[:, 1, :], in_=pwi_t[:, :, 8]), r=["pwi"], w=["A8x"])
    P.op("dve", lambda e: e.tensor_scalar(out=A8x[:, 0, :], in0=pwi_t[:, :, 8], scalar1=-1.0, scalar2=None, op0=ALU.mult), r=["pwi", "A8x"], w=["A8x"])

    Zb = P.sb("Zb", [128, 8, 240], BF16)
    P.op("pool", lambda e: e.memset(Zb[:], 0.0), w=["Zb"])
    for r_ in range(8):
        P.op("pool", lambda e, r_=r_: e.affine_select(out=Zb[:, r_, 112:128], in_=ONES[:, 0:16], pattern=[[-1, 16]], compare_op=ALU.is_equal,
                                                      fill=0.0, base=-16 * r_, channel_multiplier=1), r=["ones"], w=["Zb"])
    Ub = P.sb("Ub", [128, 32, NJ], BF16)
    HJ = NJ // 2
    for tl in range(4):
        for hf in range(2):
            ubf = bool(io.get("u_bf16"))
            if ubf:
                ub16, ubk = P.rbuf("ub16", [128, S // 2], BF16, n=2)
                ut2, utk2 = P.rbuf("ustmpb", [128, S // 2], BF16, n=2)
                ug = io["u_g"]
                P.sel_load(ub16[:], ut2[:], [(lambda base: base, ug[hf][128 * tl:128 * tl + 128, :])],
                           [(lambda base: base, ug[hf][512 + 128 * tl:512 + 128 * tl + 128, :])], sel4, ubk, utk2, extra_r=ug.keys())
            else:
                us, usk = P.rbuf("ustage", [128, S // 2], F32, n=1)
            if ubf:
                pass
            elif "u_g" in io:
                ut2, utk2 = P.rbuf("ustmp", [128, S // 2], F32, n=1)
                ug = io["u_g"]
                P.sel_load(us[:], ut2[:], [(lambda base: base, ug[hf][128 * tl:128 * tl + 128, :])],
                           [(lambda base: base, ug[hf][512 + 128 * tl:512 + 128 * tl + 128, :])], sel4, usk, utk2)
            else:
                P.dma(us[:], uT_d[128 * tl:128 * tl + 128, hf * (S // 2):(hf + 1) * (S // 2)], w=[usk])
            if not ubf:
                ub16, ubk = P.rbuf("ub16", [128, S // 2], BF16, n=1)
                P.op("pool", lambda e, us=us, ub16=ub16: e.tensor_copy(out=ub16[:], in_=us[:]), r=[usk], w=[ubk])
            ubv = ub16[:].rearrange("p (j s) -> p s j", s=8)
            for g8 in range(8):
                gl = 8 * tl + g8
                b = P.bank()
                ps = P.banks[b][:, 0:HJ]

                def mmu(e, ps=ps, g8=g8, ubv=ubv):
                    ins = None
                    for s_ in range(8):
                        ins = e.matmul(ps, lhsT=Zb[:, g8, 112 - 16 * s_:240 - 16 * s_], rhs=ubv[:, s_, :], start=(s_ == 0), stop=(s_ == 7))
                    return ins
                P.op("pe", mmu, r=["Zb", ubk], w=[("bank", b)])
                P.op("act", lambda e, ps=ps, gl=gl, hf=hf: e.activation(out=Ub[:, gl, hf * HJ:(hf + 1) * HJ], in_=ps, func=AF.Copy),
                     r=[("bank", b)], w=[("Ub", gl)])

    JB = 64
    hist = P.sb("hist", [128, 2, 16, NJ + 1], BF16)
    P.op("pool", lambda e: e.memset(hist[:, :, :, 0:1], 0.0), w=["hist0"])
    a8r = (pwr_t[:, :, 8], "pwr")
    a8i = (pwi_t[:, :, 8], "pwi")
    m8, m8t = new("m8")
    ur, urt = new("ur")
    ui, uit = new("ui")
    tt(t1, a8r, a8r, ALU.mult)
    tt(t2, a8i, a8i, ALU.mult)
    tt(t1, t1, t2, ALU.add)
    act(m8, t1, AF.Sqrt)
    P.op("dve", lambda e: e.reciprocal(out=t1[0], in_=m8[0]), r=["m8"], w=["t1"])
    tt(ur, a8r, t1, ALU.mult)
    tt(ui, a8i, t1, ALU.mult)
    Er_t = P.sb("Er", [128, 16, JB], F32)
    Ei_t = P.sb("Ei", [128, 16, JB], F32)
    P.op("pool", lambda e: e.memset(Er_t[:, :, 0:1], 1.0), w=["Er"])
    P.op("pool", lambda e: e.memset(Ei_t[:, :, 0:1], 0.0), w=["Ei"])
    stp_r, sprt = new("stp_r")
    stp_i, spit = new("stp_i")
    P.op("dve", lambda e: e.tensor_copy(out=sprt[:], in_=urt[:]), r=["ur"], w=["stp_r"])
    P.op("dve", lambda e: e.tensor_copy(out=spit[:], in_=uit[:]), r=["ui"], w=["stp_i"])
    etmp_t = P.sb("etmp", [128, 16, JB], F32)
    w_ = 1
    while w_ < JB:
        shp = [128, 16, w_]
        e0r = (Er_t[:, :, 0:w_], "Er")
        e0i = (Ei_t[:, :, 0:w_], "Ei")
        e1r = (Er_t[:, :, w_:2 * w_], "Er")
        e1i = (Ei_t[:, :, w_:2 * w_], "Ei")
        tmpv = (etmp_t[:, :, 0:w_], "etmp")
        sr = (sprt[:].unsqueeze(2).to_broadcast(shp), "stp_r")
        si = (spit[:].unsqueeze(2).to_broadcast(shp), "stp_i")
        tt(e1r, e0r, sr, ALU.mult)
        tt(tmpv, e0i, si, ALU.mult)
        tt(e1r, e1r, tmpv, ALU.subtract)
        tt(e1i, e0r, si, ALU.mult)
        tt(tmpv, e0i, sr, ALU.mult)
        tt(e1i, e1i, tmpv, ALU.add)
        w_ *= 2
        if w_ < JB:
            tt(t1, stp_r, stp_r, ALU.mult)
            tt(t2, stp_i, stp_i, ALU.mult)
            tt(t3, stp_r, stp_i, ALU.mult)
            tt(stp_r, t1, t2, ALU.subtract)
            ts(stp_i, t3, 2.0, None, ALU.mult)
    Er = (Er_t[:], "Er")
    Ei = (Ei_t[:], "Ei")
    P.barrier()

    def carve(tile4, idx):
        flat = tile4[:].rearrange("p a b c -> p (a b c)")
        return flat[:, idx * 16 * JB:(idx + 1) * 16 * JB].rearrange("p (a b) -> p a b", a=16)
    Zc = [carve(grt, 0), carve(grt, 1)]
    Zt = [carve(git, 0), carve(git, 1)]
    Xt = [carve(ctmpt, 0), carve(ctmpt, 1)]
    Xu = [carve(cprt, 0), carve(cprt, 1)]
    tA = carve(cpit, 0)
    inr, inrt = new("inr")
    ini, init_ = new("ini")
    P.op("pool", lambda e: e.memset(inrt[:], 0.0), w=["inr"])
    P.op("pool", lambda e: e.memset(init_[:], 0.0), w=["ini"])
    nblk = NJ // JB if stage in (3, 9) else 0
    def z_stage(blk):
        j0 = blk * JB
        for bq in range(4):
            b = P.bank()
            ps = P.banks[b]

            def mmz(e, bq=bq, ps=ps, j0=j0):
                ins = None
                for ql in range(4):
                    q = 4 * bq + ql
                    for c in range(2):
                        col = (ql * 2 + c) * JB
                        e.matmul(ps[0:64, col:col + JB], lhsT=WB[:, 2 * q, 64 * c:64 * c + 64], rhs=Ub[:, 2 * q, j0:j0 + JB], start=True, stop=True)
                        ins = e.matmul(ps[64:128, col:col + JB], lhsT=WB[:, 2 * q + 1, 64 * c:64 * c + 64], rhs=Ub[:, 2 * q + 1, j0:j0 + JB], start=True, stop=True)
                return ins
            P.op("pe", mmz, r=["WB"] + [("Ub", g) for g in range(8 * bq, 8 * bq + 8)], w=[("bank", b)])
            psv = ps[:, 0:8 * JB].rearrange("p (q c j) -> p q c j", q=4, c=2)
            for c in range(2):
                P.op("act", lambda e, bq=bq, c=c, psv=psv: e.activation(out=Zc[c][:, 4 * bq:4 * bq + 4, :], in_=psv[:, :, c, :], func=AF.Copy),
                     r=[("bank", b)], w=[f"Z{c}"])
    if nblk:
        z_stage(0)
    for blk in range(nblk):
        j0 = blk * JB
        Zr = (Zc[0][:], "Z0")
        Zi = (Zc[1][:], "Z1")
        Ztr = (Zt[0][:], "Zt0")
        Zti = (Zt[1][:], "Zt1")
        tAv = (tA[:], "tA")
        tt(Ztr, Zr, Er, ALU.mult)
        tt(tAv, Zi, Ei, ALU.mult)
        tt(Ztr, Ztr, tAv, ALU.add)
        tt(Zti, Zi, Er, ALU.mult)
        tt(tAv, Zr, Ei, ALU.mult)
        tt(Zti, Zti, tAv, ALU.subtract)
        if blk + 1 < nblk:
            z_stage(blk + 1)
        if blk > 0:
            xlr = (Xu[0][:, :, JB - 1], "Xu0")
            xli = (Xu[1][:, :, JB - 1], "Xu1")
            tt(inr, xlr, ur, ALU.mult)
            tt(t1, xli, ui, ALU.mult)
            tt(inr, inr, t1, ALU.subtract)
            tt(ini, xlr, ui, ALU.mult)
            tt(t1, xli, ur, ALU.mult)
            tt(ini, ini, t1, ALU.add)
        for c, (ink, intile) in enumerate((("inr", inrt), ("ini", init_))):
            for q in range(16):
                P.op("dve", lambda e, c=c, q=q, intile=intile: e.tensor_tensor_scan(
                    out=Xt[c][:, q, :], data0=m8t[:, q:q + 1].to_broadcast([128, JB]), data1=Zt[c][:, q, :],
                    initial=intile[:, q:q + 1], op0=ALU.mult, op1=ALU.add),
                    r=["m8", f"Zt{c}", ink, f"Xt{c}"], w=[f"Xt{c}"])
        Xtr = (Xt[0][:], "Xt0")
        Xti = (Xt[1][:], "Xt1")
        Xr = (Xu[0][:], "Xu0")
        Xi = (Xu[1][:], "Xu1")
        tt(Xr, Xtr, Er, ALU.mult)
        tt(tAv, Xti, Ei, ALU.mult)
        tt(Xr, Xr, tAv, ALU.subtract)
        tt(Xi, Xtr, Ei, ALU.mult)
        tt(tAv, Xti, Er, ALU.mult)
        tt(Xi, Xi, tAv, ALU.add)
        for c in range(2):
            P.op("act", lambda e, c=c, j0=j0: e.activation(out=hist[:, c, :, j0 + 1:j0 + 1 + JB], in_=Xu[c][:], func=AF.Copy),
                 r=[f"Xu{c}"], w=["hist"])

    for gl in range(32 if stage >= 4 else 0):
        q, base = gl // 2, 64 * (gl % 2)
        b = P.bank()
        ps = P.banks[b][:, 0:NJ]

        def mmy(e, gl=gl, q=q, base=base, ps=ps):
            e.matmul(ps, lhsT=Wi[:, gl, :], rhs=Ub[:, gl, :], start=True, stop=False)
            e.matmul(ps, lhsT=WCr[base:base + 64, q, :, :], rhs=hist[base:base + 64, 0, q, 0:NJ], start=False, stop=False)
            return e.matmul(ps, lhsT=WCi[base:base + 64, q, :, :], rhs=hist[base:base + 64, 1, q, 0:NJ], start=False, stop=True)
        P.op("pe", mmy, r=["Wi", "WCr", "WCi", ("Ub", gl), "hist", "hist0"], w=[("bank", b)])
        P.op("act", lambda e, gl=gl, ps=ps: e.activation(out=Ub[:, gl, :], in_=ps, func=AF.Copy), r=[("bank", b)], w=[("Ub", gl)])
    for tl in range(4 if stage >= 4 else 0):
        for hf in range(2):
            yt, ytk = P.rbuf("ustage", [128, S // 2], F32, n=2 if io.get("u_bf16") else 1)
            ytv = yt[:].rearrange("p (j s) -> p s j", s=8)
            for t_ in range(8):
                b = P.bank()
                ps = P.banks[b][:, 0:HJ]

                def mmv(e, ps=ps, t_=t_, tl=tl, hf=hf):
                    ins = None
                    for g8 in range(8):
                        ins = e.matmul(ps, lhsT=Zb[:, t_, 112 - 16 * g8:240 - 16 * g8], rhs=Ub[:, 8 * tl + g8, hf * HJ:(hf + 1) * HJ],
                                       start=(g8 == 0), stop=(g8 == 7))
                    return ins
                P.op("pe", mmv, r=["Zb"] + [("Ub", 8 * tl + g8) for g8 in range(8)], w=[("bank", b)])
                P.op("act", lambda e, ps=ps, t_=t_, ytv=ytv: e.activation(out=ytv[:, t_, :], in_=ps, func=AF.Copy), r=[("bank", b)], w=[ytk])
            P.dma(yT_d[128 * tl:128 * tl + 128, hf * (S // 2):(hf + 1) * (S // 2)], yt[:], r=[ytk], w=[("yrow", tl)], q="pool")
            if io.get("gath") is not None and hf == 1:
                io["gath"].gather(only=[tl], r=[("yrow", tl)])


def s5_pair_layout(a, gh):
    sub = a[32 * gh:32 * gh + 32]
    sub = sub.reshape((16, 2) + sub.shape[1:])
    sub = np.moveaxis(sub, 0, 2)
    return np.ascontiguousarray(sub.reshape((128, 16) + sub.shape[3:]))


L2_IN = dict(projT=[IN_COLS, S], fbB=[128, 8], w2=[16, 256], b2c=[128, 2], gnorm=[128, 128])


def build_L2(v=0, selm=False):
    P = K()
    io = {k: P.inp(k, sh) for k, sh in L2_IN.items() if not (selm and k == "projT")}
    if selm:
        io["proj_g"] = P.inp("proj_g", [2, IN_COLS, NT])
        io["sel"] = P.inp("sel", [128, 2])
        io["mixT"] = P.out("mixT", [512, S])
    else:
        io["mixT"] = P.out("mixT", [D, S])
    phase_L2(P, io, v)
    return P.build()


def phase_L2(P, io, v):
    P.make_ident()
    IDENT, ONES = P.ident, P.ones
    mixT = io["mixT"]
    RB = io.get("rowbase", dict(fq=0, fk=512, fv=1024, ff=1536, gq=1544, gk=1800, gv=2056, gg=2568, glr=3080))
    own_out = bool(io.get("own_out"))
    selm = "sel" in io
    if selm:
        sel = P.load("sel_sb", io["sel"], [128, 2])
        pg = io["proj_g"]
        T16 = P.sb("T16", [128, S], F32)

        def pieces(r0, nr):
            return [((lambda base, h=h: base[:, h * NT:(h + 1) * NT]), pg[h][r0:r0 + nr, :]) for h in range(2)]

        def load_rows(dst_tile, p0, nr, row_of_v, dkey, extra_w=()):
            P.sel_load(dst_tile[p0:p0 + nr, :], T16[p0:p0 + nr, :], pieces(row_of_v(0), nr), pieces(row_of_v(1), nr), sel, dkey, "T16", (p0, p0 + nr),
                       extra_w=extra_w)

        def load_rows_nosel(dst_tile, p0, nr, r0, dkey):
            for fn, src in pieces(r0, nr):
                P.dma(fn(dst_tile[p0:p0 + nr, :]), src, w=[dkey])
    else:
        projT = io["projT"]

        def load_rows(dst_tile, p0, nr, row_of_v, dkey, extra_w=()):
            r0 = row_of_v(v)
            P.dma(dst_tile[p0:p0 + nr, :], projT[r0:r0 + nr, :], w=[dkey] + list(extra_w))

        def load_rows_nosel(dst_tile, p0, nr, r0, dkey):
            P.dma(dst_tile[p0:p0 + nr, :], projT[r0:r0 + nr, :], w=[dkey])
    gn_d = io["gnorm"]

    A16 = P.sb("A16", [128, S], F32)
    B16 = P.sb("B16", [128, S], F32)
    C16 = P.sb("C16", [128, 8192], BF16)
    D16 = P.sb("D16", [128, S], F32)
    E8a = P.sb("E8a", [128, S], BF16)
    E8b = P.sb("E8b", [128, S], BF16)
    E8c = P.sb("E8c", [128, S], BF16)
    E8d = P.sb("E8d", [128, S], BF16)
    ones_bf = P.sb("ones_bf", [128, 128], BF16)
    ident_bf = P.sb("ident_bf", [128, 128], BF16)
    P.op("act", lambda e: e.activation(out=ones_bf[:], in_=ONES[:], func=AF.Copy), r=["ones"], w=["ones_bf"])
    P.op("act", lambda e: e.activation(out=ident_bf[:], in_=IDENT[:], func=AF.Copy), r=["ident"], w=["ident_bf"])
    zer = P.sb("zer", [128, 512], F32)
    P.op("pool", lambda e: e.memset(zer[:], 0.0), w=["zer"])
    negone = P.sb("negone", [128, 1], F32)
    P.op("pool", lambda e: e.memset(negone[:], -1.0), w=["negone"])
    nfb8 = P.load("nfb_sb", io["fbB"], [128, 8])
    P.op("dve", lambda e: e.tensor_scalar(out=nfb8[:], in0=nfb8[:], scalar1=-1.0, scalar2=None, op0=ALU.mult), r=["nfb_sb"], w=["nfb_sb"])
    if selm:
        nfb = P.sb("nfb4", [128, 4], F32)
        P.op("dve", lambda e: e.tensor_scalar(out=nfb[:], in0=nfb8[:, 0:4], scalar1=sel[:, 0:1], scalar2=None, op0=ALU.mult), r=["nfb_sb", "sel_sb"], w=["nfb_sb"])
        P.op("dve", lambda e: e.scalar_tensor_tensor(out=nfb[:], in0=nfb8[:, 4:8], scalar=sel[:, 1:2], in1=nfb[:], op0=ALU.mult, op1=ALU.add),
             r=["nfb_sb", "sel_sb"], w=["nfb_sb"])
    else:
        nfb = nfb8[:, 4 * v:4 * v + 4]
    maskneg = P.sb("maskneg", [128, 4, 512], F32)
    for d in range(4):
        P.op("pool", lambda e, d=d: e.affine_select(out=maskneg[:, d, :], in_=zer[:], pattern=[[1, 512]], compare_op=ALU.is_ge,
                                                    fill=-30000.0, base=-128 * d, channel_multiplier=-1), r=["zer"], w=["maskneg"])

    vext = E8d[:].rearrange("p (b m) -> p b m", b=32)
    P.op("pool", lambda e: e.memset(vext[:, :, 64:128], 1.0), w=["khat"])
    sel64 = P.sb("sel64", [128, 64], F32)
    P.op("pool", lambda e: e.memset(sel64[:], 0.0), w=["sel64"])
    P.op("pool", lambda e: e.memset(sel64[64:65, :], 1.0), w=["sel64"])
    qa, ka, hi_t = E8a, E8b, E8c
    ncs = P.sb("ncs", [128, 32], F32)
    for i in range(4):
        load_rows(A16, 0, 64, lambda v_: RB["fq"] + (4 * v_ + i) * 64, "A16lo")
        load_rows(B16, 0, 64, lambda v_: RB["fk"] + (4 * v_ + i) * 64, "B16")
        load_rows(B16, 64, 64, lambda v_: RB["fv"] + (4 * v_ + i) * 64, "B16v")
        for g4 in range(4):
            bv = P.bank()
            psv = P.banks[bv]

            def mmv(e, g4=g4, psv=psv):
                ins = None
                for bl in range(8):
                    blk = 8 * g4 + bl
                    ins = e.matmul(psv[:, 64 * bl:64 * bl + 64], lhsT=B16[64:128, 128 * blk:128 * blk + 128], rhs=IDENT[64:128, 64:128], start=True, stop=True)
                return ins
            P.op("pe", mmv, r=["B16v", "ident"], w=[("bank", bv)])
            P.op("act", lambda e, g4=g4, psv=psv: e.activation(out=vext[:, 8 * g4:8 * g4 + 8, 0:64], in_=psv.rearrange("p (b d) -> p b d", b=8), func=AF.Copy),
                 r=[("bank", bv)], w=["khat"])
        P.op("pool", lambda e: e.memset(A16[64:128, :], 0.0), w=["A16hi"])
        load_rows(A16, 64, 1, lambda v_: RB["ff"] + 4 * v_ + i, "A16hi")
        load_rows(A16, 96, 1, lambda v_: RB["ff"] + 4 * v_ + i, "A16hi")
        P.op("act", lambda e: e.activation(out=qa[0:64, :], in_=A16[0:64, :], func=AF.Copy, scale=0.125), r=["A16lo"], w=["qa_lo"])
        P.op("act", lambda e: e.activation(out=ka[0:64, :], in_=B16[0:64, :], func=AF.Copy), r=["B16"], w=["ka"])
        P.op("pool", lambda e: e.memset(ka[64:128, :], 0.0), w=["ka"])
        P.op("pool", lambda e: e.memset(ka[64:65, :], 1.0), w=["ka"])
        P.op("pool", lambda e: e.memset(ka[96:97, :], 1.0), w=["ka"])
        P.op("act", lambda e, i=i: e.activation(out=A16[64:128, :], in_=A16[64:128, :], func=AF.Exp, scale=-1.0, bias=nfb[64:128, i:i + 1]),
             r=["A16hi", "nfb_sb"], w=["A16hi"])
        P.op("act", lambda e: e.activation(out=A16[64:128, :], in_=A16[64:128, :], func=AF.Ln, bias=1.0), r=["A16hi"], w=["A16hi"])
        P.op("dve", lambda e: e.tensor_tensor_scan(out=A16[64:128, :], data0=ONES[64:128, 0:1].to_broadcast([64, S]), data1=A16[64:128, :],
                                                   initial=0.0, op0=ALU.mult, op1=ALU.subtract), r=["A16hi", "ones"], w=["A16hi"])
        P.op("act", lambda e: e.activation(out=hi_t[64:128, :], in_=A16[64:128, :], func=AF.Copy), r=["A16hi"], w=["hi_t"])
        P.op("pool", lambda e: e.tensor_copy(out=qa[64:96, :], in_=hi_t[64:96, :]), r=["hi_t"], w=["qa_hi"])
        P.op("dve", lambda e: e.tensor_tensor(out=qa[96:128, :], in0=A16[96:128, :], in1=hi_t[96:128, :], op=ALU.subtract),
             r=["A16hi", "hi_t"], w=["qa_hi"])
        bn = P.bank()
        psn = P.banks[bn]

        def mmn(e, psn=psn):
            ins = None
            for blk in range(32):
                ins = e.matmul(psn[:, blk:blk + 1], lhsT=A16[64:65, 128 * blk:128 * blk + 128], rhs=negone[64:65, 0:1], start=True, stop=True)
            return ins
        P.op("pe", mmn, r=["A16hi", "negone"], w=[("bank", bn)])
        P.op("dve", lambda e, psn=psn: e.tensor_copy(out=ncs[:], in_=psn[:, 0:32]), r=[("bank", bn)], w=["ncs"])
        for I in range(8):
            bo = P.bank()
            P.reserved.add(bo)
            ps_o = P.banks[bo][:, :]
            nJ = 4 * I + 4
            LA = 3
            pend = []
            for J in range(nJ + LA):
                if J < nJ:
                    b = P.bank()
                    ps_s = P.banks[b][:, :]
                    P.op("pe", lambda e, ps_s=ps_s, J=J, I=I: e.matmul(ps_s, lhsT=ka[:, 128 * J:128 * J + 128], rhs=qa[:, 512 * I:512 * I + 512], start=True, stop=True),
                         r=["ka", "qa_lo", "qa_hi"], w=[("bank", b)])
                    pT, pk = P.rbuf("pT", [128, 512], BF16, n=5)
                    if J >= 4 * I:
                        tmpm, tmk = P.rbuf("fa", [128, 512], F32)
                        P.op("dve", lambda e, ps_s=ps_s, tmpm=tmpm, d=J - 4 * I: e.tensor_tensor(out=tmpm[:], in0=ps_s, in1=maskneg[:, d, :], op=ALU.add),
                             r=[("bank", b), "maskneg"], w=[tmk])
                        P.op("act", lambda e, tmpm=tmpm, pT=pT, J=J: e.activation(out=pT[:], in_=tmpm[:], func=AF.Exp, bias=ncs[:, J:J + 1]),
                             r=[tmk, "ncs"], w=[pk])
                    else:
                        P.op("act", lambda e, ps_s=ps_s, pT=pT, J=J: e.activation(out=pT[:], in_=ps_s, func=AF.Exp, bias=ncs[:, J:J + 1]),
                             r=[("bank", b), "ncs"], w=[pk])
                    pend.append((J, pT, pk))
                if J >= LA:
                    Jo, pTo, pko = pend.pop(0)
                    P.op("pe", lambda e, pTo=pTo, Jo=Jo, ps_o=ps_o, nJ=nJ: e.matmul(ps_o, lhsT=vext[:, Jo, :], rhs=pTo[:], start=(Jo == 0), stop=(Jo == nJ - 1)),
                         r=[pko, "khat"], w=[("bank", bo)])
            xs, xsk = P.rbuf("fx", [128, 512], F32)
            P.op("act", lambda e, xs=xs, ps_o=ps_o: e.activation(out=xs[:], in_=ps_o, func=AF.Copy), r=[("bank", bo)], w=[xsk])
            P.reserved.discard(bo)
            bd = P.bank()
            ps_d = P.banks[bd][0:64, :]
            P.op("pe", lambda e, xs=xs, ps_d=ps_d: e.matmul(ps_d, lhsT=sel64[:], rhs=xs[:], start=True, stop=True), r=[xsk, "sel64"], w=[("bank", bd)])
            rd, rdk = P.rbuf("rd", [64, 512], F32)
            ob, obk = P.rbuf("osb", [64, 512], BF16 if io.get("out_bf16") else F32, n=3)
            P.op("dve", lambda e, rd=rd, ps_d=ps_d: e.reciprocal(out=rd[:], in_=ps_d), r=[("bank", bd)], w=[rdk])
            P.op("dve", lambda e, rd=rd, ob=ob, xs=xs: e.tensor_tensor(out=ob[:], in0=xs[0:64, :], in1=rd[:], op=ALU.mult), r=[xsk, rdk], w=[obk])
            fr0 = i * 64 if (selm or own_out) else (4 * v + i) * 64
            P.dma(mixT[fr0:fr0 + 64, 512 * I:512 * I + 512], ob[:], r=[obk], w=[("foxrow", i)], q="pool")
            if io.get("gath") is not None and I == 7 and i % 2 == 1:
                io["gath"].gather(only=[i // 2], r=[("foxrow", i - 1), ("foxrow", i)])

    gvb = C16[:].rearrange("p (b i d) -> p b i d", b=32, i=2)
    for i in range(2):
        load_rows(D16, 0, 128, lambda v_: RB["gv"] + (2 * v_ + i) * 128, "D16")
        for g4 in range(8):
            bv = P.bank()
            psv = P.banks[bv]

            def mmgv(e, g4=g4, psv=psv):
                ins = None
                for bl in range(4):
                    blk = 4 * g4 + bl
                    ins = e.matmul(psv[:, 128 * bl:128 * bl + 128], lhsT=D16[:, 128 * blk:128 * blk + 128], rhs=IDENT[:], start=True, stop=True)
                return ins
            P.op("pe", mmgv, r=["D16", "ident"], w=[("bank", bv)])
            P.op("act", lambda e, g4=g4, psv=psv, i=i: e.activation(out=gvb[:, 4 * g4:4 * g4 + 4, i, :], in_=psv.rearrange("p (b d) -> p b d", b=4), func=AF.Copy),
                 r=[("bank", bv)], w=["C16", ("C16", 0), ("C16", 1), ("C16", 2), ("C16", 3)])
    load_rows_nosel(B16, 0, 16, RB["glr"], "B16")
    glr_b = E8d[0:16, :]
    P.op("act", lambda e: e.activation(out=glr_b, in_=B16[0:16, :], func=AF.Copy), r=["B16"], w=["khat"])
    w2_all = P.load("w2_all", io["w2"], [16, 256])
    if selm:
        w2_f = P.sb("w2_f", [16, 128], F32)
        P.op("dve", lambda e: e.tensor_scalar(out=w2_f[:], in0=w2_all[:, 0:128], scalar1=sel[0:16, 0:1], scalar2=None, op0=ALU.mult), r=["w2_all", "sel_sb"], w=["w2_f"])
        P.op("dve", lambda e: e.scalar_tensor_tensor(out=w2_f[:], in0=w2_all[:, 128:256], scalar=sel[0:16, 1:2], in1=w2_f[:], op0=ALU.mult, op1=ALU.add),
             r=["w2_all", "sel_sb", "w2_f"], w=["w2_f"])
    else:
        w2_f = w2_all[:, 128 * v:128 * v + 128]
    w2_b = P.sb("w2_b", [16, 128], BF16)
    P.op("act", lambda e: e.activation(out=w2_b[:], in_=w2_f[:], func=AF.Copy), r=["w2_f", "w2_all"], w=["w2_b"])
    nb2f = P.load("nb2_sb", io["b2c"], [128, 2])
    P.op("dve", lambda e: e.tensor_scalar(out=nb2f[:], in0=nb2f[:], scalar1=-1.0, scalar2=None, op0=ALU.mult), r=["nb2_sb"], w=["nb2_sb"])
    if selm:
        nb2t = P.sb("nb2sel", [128, 1], F32)
        P.op("dve", lambda e: e.tensor_scalar(out=nb2t[:], in0=nb2f[:, 0:1], scalar1=sel[:, 0:1], scalar2=None, op0=ALU.mult), r=["nb2_sb", "sel_sb"], w=["nb2_sb"])
        P.op("dve", lambda e: e.scalar_tensor_tensor(out=nb2t[:], in0=nb2f[:, 1:2], scalar=sel[:, 1:2], in1=nb2t[:], op0=ALU.mult, op1=ALU.add),
             r=["nb2_sb", "sel_sb"], w=["nb2_sb"])
        nb2 = nb2t[:, 0:1]
    else:
        nb2 = nb2f[:, v:v + 1]
    gnb = P.load("gn_sb", gn_d, [128, 128])
    rmask = P.sb("rmask", [128, 8, 64], F32)
    P.op("pool", lambda e: e.memset(rmask[:], 1.0), w=["rmask"])
    P.op("pool", lambda e: e.memset(rmask[:, :, 0:1], 0.0), w=["rmask"])
    gmask = P.sb("gmask", [128, 128], F32)
    P.op("pool", lambda e: e.affine_select(out=gmask[:], in_=ONES[:], pattern=[[1, 128]], compare_op=ALU.is_ge, fill=0.0, base=0, channel_multiplier=-1),
         r=["ones"], w=["gmask"])
    P.op("pool", lambda e: e.memset(gmask[0:64, 64:128], 0.0), w=["gmask"])
    CL = D16
    for n0 in range(0, S, 512):
        b = P.bank()
        ps = P.banks[b][:, :]
        P.op("pe", lambda e, ps=ps, n0=n0: e.matmul(ps, lhsT=w2_b[:], rhs=glr_b[:, n0:n0 + 512], start=True, stop=True), r=["w2_b", "khat"], w=[("bank", b)])
        ta, tak = P.rbuf("fa", [128, 512], F32)
        P.op("act", lambda e, ps=ps, ta=ta: e.activation(out=ta[:], in_=ps, func=AF.Exp, scale=-1.0, bias=nb2), r=[("bank", b), "nb2_sb"], w=[tak])
        P.op("act", lambda e, ta=ta: e.activation(out=ta[:], in_=ta[:], func=AF.Ln, bias=1.0), r=[tak], w=[tak])
        P.op("dve", lambda e, ta=ta, n0=n0: e.tensor_tensor_scan(out=CL[:, n0:n0 + 512], data0=rmask[:].rearrange("p a b -> p (a b)"), data1=ta[:],
                                                              initial=0.0, op0=ALU.mult, op1=ALU.add), r=[tak, "rmask", "D16"], w=["D16"])
    load_rows(A16, 0, 128, lambda v_: RB["gq"] + 128 * v_, "A16lo", extra_w=["A16hi"])
    load_rows(B16, 0, 128, lambda v_: RB["gk"] + 128 * v_, "B16")
    qt, kt, khT, khat = E8a, E8b, E8c, E8d
    dcol = P.sb("dcol", [128, 64], F32)
    P.op("act", lambda e: e.activation(out=dcol[:], in_=CL[:].rearrange("p (c t) -> p c t", t=64)[:, :, 63], func=AF.Exp, scale=-1.0 / 16), r=["D16"], w=["dcol"])
    for n0 in range(0, S, 512):
        ta, tak = P.rbuf("fa", [128, 512], F32)
        tb, tbk = P.rbuf("fb", [128, 512], F32)
        P.op("act", lambda e, ta=ta, n0=n0: e.activation(out=ta[:], in_=CL[:, n0:n0 + 512], func=AF.Exp, scale=-1.0 / 16), r=["D16"], w=[tak])
        P.op("dve", lambda e, ta=ta, n0=n0: e.scalar_tensor_tensor(out=qt[:, n0:n0 + 512], in0=A16[:, n0:n0 + 512], scalar=0.125, in1=ta[:], op0=ALU.mult, op1=ALU.mult),
             r=[tak, "A16lo", "A16hi"], w=["qt"])
        P.op("act", lambda e, tb=tb, n0=n0: e.activation(out=tb[:], in_=CL[:, n0:n0 + 512], func=AF.Exp, scale=1.0 / 16), r=["D16"], w=[tbk])
        P.op("pool", lambda e, tb=tb, n0=n0: e.tensor_tensor(out=kt[:, n0:n0 + 512], in0=B16[:, n0:n0 + 512], in1=tb[:], op=ALU.mult), r=[tbk, "B16"], w=["kt"])
        ta2, tak2 = P.rbuf("fa", [128, 512], F32)
        cl_last = CL[:, n0:n0 + 512].rearrange("p (c t) -> p c t", t=64)[:, :, 63:64].to_broadcast([128, 8, 64])
        P.op("dve", lambda e, ta2=ta2, n0=n0, cl_last=cl_last: e.tensor_tensor(out=ta2[:].rearrange("p (c t) -> p c t", t=64),
                                                                                in0=CL[:, n0:n0 + 512].rearrange("p (c t) -> p c t", t=64),
                                                                                in1=cl_last, op=ALU.subtract), r=["D16"], w=[tak2])
        P.op("act", lambda e, ta2=ta2: e.activation(out=ta2[:], in_=ta2[:], func=AF.Exp, scale=1.0 / 16), r=[tak2], w=[tak2])
        P.op("dve", lambda e, ta2=ta2, n0=n0: e.tensor_tensor(out=khT[:, n0:n0 + 512], in0=B16[:, n0:n0 + 512], in1=ta2[:], op=ALU.mult), r=[tak2, "B16"], w=["khT"])
    khat_v = khat[:].rearrange("p (b m) -> p b m", b=32)
    for blk in range(32):
        b = P.bank()
        ps = P.banks[b][:, 0:128]
        P.op("pe", lambda e, ps=ps, blk=blk: e.matmul(ps, lhsT=khT[:, 128 * blk:128 * blk + 128], rhs=ident_bf[:], start=True, stop=True),
             r=["khT", "ident_bf"], w=[("bank", b)])
        P.op("act", lambda e, ps=ps, blk=blk: e.activation(out=khat_v[:, blk, :], in_=ps, func=AF.Copy), r=[("bank", b)], w=["khat"])
    Sb = P.sb("Sb", [128, 65, 128], BF16)
    Sf = [P.sb(f"Sf{i}", [128, 128], F32) for i in range(2)]
    P.op("pool", lambda e: e.memset(Sb[:, 0, :], 0.0), w=["Sb0"])
    P.op("pool", lambda e: e.memset(Sf[0][:], 0.0), w=["Sf0"])
    for blk in range(32):
        bb_ = [P.bank(), P.bank()]

        def mms(e, blk=blk, bb_=bb_):
            ins = None
            for par in range(2):
                for i in range(2):
                    ins = e.matmul(P.banks[bb_[par]][64 * i:64 * i + 64, 0:128],
                                   lhsT=khat_v[64 * par:64 * par + 64, blk, 64 * i:64 * i + 64],
                                   rhs=gvb[64 * par:64 * par + 64, blk, i, :], start=True, stop=True)
            return ins
        P.op("pe", mms, r=["khat", "C16"], w=[("bank", bb_[0]), ("bank", bb_[1])])
        for par in range(2):
            j = 2 * blk + par
            so, sok = Sf[j % 2], f"Sf{j % 2}"
            sn, snk = Sf[(j + 1) % 2], f"Sf{(j + 1) % 2}"
            P.op("dve", lambda e, so=so, sn=sn, par=par, j=j, bb_=bb_: e.scalar_tensor_tensor(
                out=sn[:], in0=so[:], scalar=dcol[:, j:j + 1], in1=P.banks[bb_[par]][:, 0:128], op0=ALU.mult, op1=ALU.add),
                r=[sok, "dcol", ("bank", bb_[par]), snk], w=[snk])
            P.op("act", lambda e, sn=sn, j=j: e.activation(out=Sb[:, j + 1, :], in_=sn[:], func=AF.Copy), r=[snk], w=["Sb"])
    items = [(blk, i) for blk in range(32) for i in range(2)]
    stA, stB = {}, {}
    cur = {}

    def stage_a(n):
        blk, i = items[n]
        b = P.bank()
        ps_a = P.banks[b][:, 0:128]
        P.op("pe", lambda e: e.matmul(ps_a, lhsT=kt[64 * i:64 * i + 64, 128 * blk:128 * blk + 128],
                                      rhs=qt[64 * i:64 * i + 64, 128 * blk:128 * blk + 128], start=True, stop=True),
             r=["kt", "qt"], w=[("bank", b)])
        am, amk = P.rbuf("am", [128, 128], BF16, n=3)
        P.op("dve", lambda e: e.tensor_tensor(out=am[:], in0=ps_a, in1=gmask[:], op=ALU.mult), r=[("bank", b), "gmask"], w=[amk])
        stA[n] = (am, amk)

    def stage_b(n):
        blk, i = items[n]
        am, amk = stA.pop(n)
        b2_ = P.bank()
        ps_o = P.banks[b2_][:, 0:128]

        def mmo2(e):
            e.matmul(ps_o, lhsT=am[:], rhs=gvb[:, blk, i, :], start=True, stop=False)
            ins = None
            for par in range(2):
                t0 = 128 * blk + 64 * par
                ins = e.matmul(ps_o[64 * par:64 * par + 64, :], lhsT=qt[64 * i:64 * i + 64, t0:t0 + 64],
                               rhs=Sb[64 * i:64 * i + 64, 2 * blk + par, :], start=False, stop=True)
            return ins
        P.op("pe", mmo2, r=[amk, "C16", "qt", "Sb", "Sb0"], w=[("bank", b2_)])
        junk, jk = P.rbuf("junk", [128, 128], F32)
        st, stk = P.rbuf("gst", [128, 2], F32, n=3)
        P.op("act", lambda e: e.activation(out=junk[:], in_=ps_o, func=AF.Square, accum_out=st[:, 0:1]), r=[("bank", b2_)], w=[jk, stk])
        P.op("dve", lambda e: e.tensor_scalar(out=st[:, 1:2], in0=st[:, 0:1], scalar1=1.0 / 128, scalar2=EPS, op0=ALU.mult, op1=ALU.add), r=[stk], w=[stk])
        P.op("act", lambda e: e.activation(out=st[:, 1:2], in_=st[:, 1:2], func=AF.Sqrt), r=[stk], w=[stk])
        P.op("dve", lambda e: e.reciprocal(out=st[:, 1:2], in_=st[:, 1:2]), r=[stk], w=[stk])
        tq, tqk = P.rbuf("tq", [128, 128], F32, n=3)
        P.op("dve", lambda e: e.scalar_tensor_tensor(out=tq[:], in0=ps_o, scalar=st[:, 1:2], in1=gnb[:], op0=ALU.mult, op1=ALU.mult),
             r=[("bank", b2_), stk, "gn_sb"], w=[tqk])
        stB[n] = (tq, tqk)

    def stage_c(n):
        blk, i = items[n]
        tq, tqk = stB.pop(n)
        if blk % 4 == 0 and i == 0:
            cur["ggc"], cur["ggk"] = P.rbuf("ggc", [128, 2, 512], F32)
            for i_ in range(2):
                if selm:
                    hh_, cc0 = (128 * blk) // NT, (128 * blk) % NT
                    gt, gtk = P.rbuf("ggt", [128, 512], F32)
                    P.sel_load(cur["ggc"][:, i_, :], gt[:],
                               [(lambda base: base, pg[hh_][2568 + i_ * 128:2568 + i_ * 128 + 128, cc0:cc0 + 512])],
                               [(lambda base: base, pg[hh_][2568 + (2 + i_) * 128:2568 + (2 + i_) * 128 + 128, cc0:cc0 + 512])],
                               sel, cur["ggk"], gtk)
                else:
                    gr0 = RB["gg"] + (2 * v + i_) * 128
                    P.dma(cur["ggc"][:, i_, :], projT[gr0:gr0 + 128, 128 * blk:128 * blk + 512], w=[cur["ggk"]])
            cur["yo"], cur["yok"] = P.rbuf("yo", [128, 2, 512], BF16 if io.get("out_bf16") else F32)
        ggc, ggk, yo, yok = cur["ggc"], cur["ggk"], cur["yo"], cur["yok"]
        bt = P.bank()
        pst = P.banks[bt][:, 0:128]
        P.op("pe", lambda e: e.matmul(pst, lhsT=tq[:], rhs=IDENT[:], start=True, stop=True), r=[tqk, "ident"], w=[("bank", bt)])
        sg, sgk = P.rbuf("sg", [128, 128], F32)
        c0 = (blk % 4) * 128
        P.op("act", lambda e: e.activation(out=sg[:], in_=ggc[:, i, c0:c0 + 128], func=AF.Silu), r=[ggk], w=[sgk])
        P.op("dve", lambda e: e.tensor_tensor(out=yo[:, i, c0:c0 + 128], in0=pst, in1=sg[:], op=ALU.mult), r=[("bank", bt), sgk], w=[yok])
        if blk % 4 == 3 and i == 1:
            for i_ in range(2):
                r0 = (256 + i_ * 128) if (selm or own_out) else (512 + (2 * v + i_) * 128)
                P.dma(mixT[r0:r0 + 128, 128 * (blk - 3):128 * (blk + 1)], yo[:, i_, :], r=[yok], q="pool")
    NI = len(items)
    for step in range(NI + 2):
        if step < NI:
            stage_a(step)
        if 0 <= step - 1 < NI:
            stage_b(step - 1)
        if 0 <= step - 2 < NI:
            stage_c(step - 2)


S5P = ("lamr", "lami", "logdt", "br", "bi", "cr", "ci")


def phase_mod(P, io):
    c = P.load("c_col", io["ccol"], [128, 8])
    csil = P.sb("c_silf", [128, 8], F32)
    P.op("act", lambda e: e.activation(out=csil[:], in_=c[:], func=AF.Silu), r=["c_col"], w=["c_silf"])
    for t, (wk, bk) in enumerate((("mod_w", "mod_b"), ("mw1", "mb1"), ("mw2", "mb2"), ("mw3", "mb3"))):
        W = io[wk]
        bc = P.load(f"bc{t}", io[bk], [128, 24])
        modc = P.sb(f"modc{t}", [128, 24], F32)
        Wv = W.rearrange("(kt p) m -> p kt m", p=128)
        for c0 in range(0, 3 * D, 512):
            st, stk = P.rbuf("mst", [128, 8, 512], F32, n=3)
            P.dma(st[:], Wv[:, :, c0:c0 + 512], w=[stk])
            b = P.bank()
            ps = P.banks[b]

            def mm(e, st=st, ps=ps):
                ins = None
                for m in range(4):
                    for kt in range(8):
                        ins = e.matmul(ps[:, m:m + 1], lhsT=st[:, kt, 128 * m:128 * m + 128], rhs=csil[:, kt:kt + 1], start=(kt == 0), stop=(kt == 7))
                return ins
            P.op("pe", mm, r=[stk, "c_silf"], w=[("bank", b)])
            col = c0 // 128
            P.op("dve", lambda e, ps=ps, col=col, modc=modc, bc=bc: e.tensor_tensor(out=modc[:, col:col + 4], in0=ps[:, 0:4], in1=bc[:, col:col + 4], op=ALU.add),
                 r=[("bank", b), f"bc{t}"], w=[f"modc{t}"])
        P.dma(io["mods"][t], modc[:], r=[f"modc{t}"], q="pool")


def build_fused_dup():
    P = K()
    I = {}

    def inp(name, shape):
        I[name] = P.inp(name, shape)
        return I[name]
    xT = inp("xT", [D, S])
    inp("ccol", [128, 8])
    inp("sel", [128, 2])
    for n, sh in (("mod_w", [D, 3 * D]), ("mod_b", [128, 24]), ("norm_w", [128, 8]), ("w_in", [D, IN_COLS]),
                  ("fbB", [128, 8]), ("w2", [16, 256]), ("b2c", [128, 2]), ("gnorm", [128, 128]),
                  ("w_o", [D, D]), ("nw1", [128, 8]), ("mw1", [D, 3 * D]), ("mb1", [128, 24]),
                  ("Wg", [D, DFF]), ("Wu", [D, DFF]), ("Wd", [DFF, D]),
                  ("nw2", [128, 8]), ("mw2", [D, 3 * D]), ("mb2", [128, 24]), ("w_in2", [D, D]),
                  ("dskip", [128, 8]), ("w_glu", [D, D]), ("w_o2", [D, D]),
                  ("nw3", [128, 8]), ("mw3", [D, 3 * D]), ("mb3", [128, 24]), ("router", [D, NE]),
                  ("Eg", [NE, D, DFF]), ("Eu", [NE, D, DFF]), ("Ed", [NE, DFF, D]), ("fnorm", [128, 8])):
        inp(n, sh)
    for gh in range(2):
        for n in S5P:
            inp(f"{n}{gh}", [128, 16] if n in ("lamr", "lami", "logdt") else [128, 16, 16])
    outT = P.out("outT", [D, NT])
    projT_s = P.dram("projT_s", [IN_COLS, S])
    modc0_s = P.dram("modc0_s", [128, 24])
    mix_s = P.dram("mix_s", [D, S])
    x2T_s = P.dram("x2T_s", [D, S])
    uT_s = P.dram("uT_s", [D, S])
    modc2_s = P.dram("modc2_s", [128, 24])
    s5y_s = P.dram("s5y_s", [D, S])
    hs = [slice(v * NT, (v + 1) * NT) for v in range(2)]
    mods = P.dram("mods_s", [4, 128, 24])
    P.begin_phase("m_")
    phase_mod(P, dict(ccol=I["ccol"], mods=mods, **{k: I[k] for k in ("mod_w", "mod_b", "mw1", "mb1", "mw2", "mb2", "mw3", "mb3")}))
    for v in range(2):
        P.begin_phase(f"a{v}_")
        phase_L1(P, dict(xT=xT[:, hs[v]], ccol=I["ccol"], mod_w=I["mod_w"], mod_b=I["mod_b"], norm_w=I["norm_w"], w_in=I["w_in"],
                         projT=projT_s[:, hs[v]], modc=modc0_s, pre0=mods[0]))
    for v in range(2):
        P.begin_phase(f"b{v}_")
        phase_L2(P, dict(projT=projT_s, fbB=I["fbB"], w2=I["w2"], b2c=I["b2c"], gnorm=I["gnorm"], mixT=mix_s), v)
    for v in range(2):
        P.begin_phase(f"c{v}_")
        io = {k: I[k] for k in ("w_o", "ccol", "nw1", "mw1", "mb1", "Wg", "Wu", "Wd", "nw2", "mw2", "mb2", "w_in2")}
        io.update(x0T=xT[:, hs[v]], mixT=mix_s[:, hs[v]], modc0=modc0_s, x2T=x2T_s[:, hs[v]], uT=uT_s[:, hs[v]], modc2=modc2_s,
                  pre1=mods[1], pre2=mods[2])
        phase_L3(P, io)
    for v in range(2):
        P.begin_phase(f"d{v}_")
        io = {n: I[f"{n}{v}"] for n in S5P}
        io.update(uT=uT_s[512 * v:512 * v + 512, :], s5yT=s5y_s[512 * v:512 * v + 512, :])
        phase_L4(P, io)
    P.begin_phase("e_")
    io = {k: I[k] for k in ("dskip", "w_glu", "w_o2", "ccol", "nw3", "mw3", "mb3", "router", "Eg", "Eu", "Ed", "fnorm", "sel")}
    io.update(x2T=x2T_s, s5yT=s5y_s, uT=uT_s, modc2=modc2_s, outT=outT, pre3=mods[3])
    phase_L5(P, io)
    return P.build()


class Gath:
    def __init__(self, P, name, rows, cols, bounds, dtype=F32):
        self.P = P
        self.own = P.dram(name + "_own", [rows, cols], dtype)
        self.bounds = bounds
        self.dsts = [P.dram(f"{name}_g{k}", [2 * (b1 - b0), cols], dtype) for k, (b0, b1) in enumerate(bounds)]

    def gather(self, only=None, r=()):
        for k, (b0, b1) in enumerate(self.bounds):
            if only is None or k in only:
                self.P.allgather_pair(self.own[b0:b1, :], self.dsts[k], r=r, w=[("gath", id(self), k)])

    def keys(self):
        return [("gath", id(self), k) for k in range(len(self.bounds))]

    def __getitem__(self, h):
        return _GathRank(self, h)


class _GathRank:
    def __init__(self, g, h):
        self.g, self.h = g, h

    def __getitem__(self, key):
        rs, cs = key
        r0, r1 = rs.start, rs.stop
        for k, (b0, b1) in enumerate(self.g.bounds):
            if b0 <= r0 and r1 <= b1:
                n = b1 - b0
                return self.g.dsts[k][self.h * n + (r0 - b0):self.h * n + (r1 - b0), cs]
        raise AssertionError(f"rows {r0}:{r1} straddle gather chunks")


OWN_ROWBASE = dict(fq=0, fk=256, fv=512, ff=768, gq=772, gk=900, gv=1028, gg=1284, glr=1540)
OWN_COLS = 1556


def own_cols(hh):
    r = np.arange
    return np.concatenate([r(256 * hh, 256 * hh + 256), 512 + r(256 * hh, 256 * hh + 256), 1024 + r(256 * hh, 256 * hh + 256),
                           1536 + r(4 * hh, 4 * hh + 4), 1544 + r(128 * hh, 128 * hh + 128), 1800 + r(128 * hh, 128 * hh + 128),
                           2056 + r(256 * hh, 256 * hh + 256), 2568 + r(256 * hh, 256 * hh + 256), r(3080, 3096)])


PROJ_BOUNDS = [(0, 256), (256, 512), (512, 768), (768, 1024), (1024, 1280), (1280, 1536), (1536, 1672), (1672, 1800),
               (1800, 1928), (1928, 2056), (2056, 2312), (2312, 2568), (2568, 2824), (2824, 3080), (3080, 3096)]


def build_fused():
    P = K()
    I = {}

    def inp(name, shape):
        I[name] = P.inp(name, shape)
        return I[name]
    xT = inp("xT", [D, NT])
    xT_full = inp("xT_full", [D, S])
    inp("ccol", [128, 8])
    inp("sel", [128, 2])
    for n, sh in (("mod_w", [D, 3 * D]), ("mod_b", [128, 24]), ("norm_w", [128, 8]), ("w_in", [D, OWN_COLS]),
                  ("fbB", [128, 8]), ("w2", [16, 256]), ("b2c", [128, 2]), ("gnorm", [128, 128]),
                  ("w_o", [D, D]), ("nw1", [128, 8]), ("mw1", [D, 3 * D]), ("mb1", [128, 24]),
                  ("Wg", [D, DFF]), ("Wu", [D, DFF]), ("Wd", [DFF, D]),
                  ("nw2", [128, 8]), ("mw2", [D, 3 * D]), ("mb2", [128, 24]), ("w_in2", [D, D]),
                  ("dskip", [128, 8]), ("w_glu", [D, D]), ("w_o2", [D, D]),
                  ("nw3", [128, 8]), ("mw3", [D, 3 * D]), ("mb3", [128, 24]), ("router", [D, NE]),
                  ("Eg", [NE, D, DFF]), ("Eu", [NE, D, DFF]), ("Ed", [NE, DFF, D]), ("fnorm", [128, 8])):
        inp(n, sh)
    for n in S5P:
        inp(n, [128, 16] if n in ("lamr", "lami", "logdt") else [128, 16, 16])
    outT = P.out("outT", [D, NT])
    mods = P.dram("mods_s", [4, 128, 24])
    modc0_s = P.dram("modc0_s", [128, 24])
    modc2_s = P.dram("modc2_s", [128, 24])
    proj_own = P.dram("proj_own", [OWN_COLS, S])
    mix_g = Gath(P, "mix", 512, S, [(0, 256), (256, 512)], BF16)
    u_g = Gath(P, "ub", D, NT, [(0, 512), (512, 1024)], BF16)
    s5y_g = Gath(P, "s5y", 512, S, [(128 * k, 128 * k + 128) for k in range(4)])
    mix_own, s5y_own = mix_g.own, s5y_g.own
    uT_own = P.dram("uT_own", [D, NT])
    x2T_own = P.dram("x2T_own", [D, NT])

    P.begin_phase("m_")
    phase_mod(P, dict(ccol=I["ccol"], mods=mods, **{k: I[k] for k in ("mod_w", "mod_b", "mw1", "mb1", "mw2", "mb2", "mw3", "mb3")}))
    P.begin_phase("a_")
    phase_L1(P, dict(xT=xT_full, ccol=I["ccol"], mod_w=I["mod_w"], mod_b=I["mod_b"], norm_w=I["norm_w"], w_in=I["w_in"],
                     projT=proj_own, modc=modc0_s, pre0=mods[0]))
    P.begin_phase("b_")
    phase_L2(P, dict(projT=proj_own, rowbase=OWN_ROWBASE, own_out=True, fbB=I["fbB"], w2=I["w2"], b2c=I["b2c"], gnorm=I["gnorm"],
                     mixT=mix_own, out_bf16=True), 0)
    P.begin_phase("c_")
    mix_g.gather()
    io = {k: I[k] for k in ("w_o", "ccol", "nw1", "mw1", "mb1", "Wg", "Wu", "Wd", "nw2", "mw2", "mb2", "w_in2", "sel")}
    io.update(x0T=xT, mix_g=mix_g, mix_bf16=True, modc0=modc0_s, x2T=x2T_own, uT=uT_own, uT_bf=u_g.own, modc2=modc2_s,
              pre1=mods[1], pre2=mods[2])
    phase_L3(P, io)
    P.begin_phase("d_")
    u_g.gather()
    io = {n: I[n] for n in S5P}
    io.update(u_g=u_g, u_bf16=True, sel=I["sel"], s5yT=s5y_own, gath=s5y_g)
    phase_L4(P, io)
    P.begin_phase("e_")
    io = {k: I[k] for k in ("dskip", "w_glu", "w_o2", "ccol", "nw3", "mw3", "mb3", "router", "Eg", "Eu", "Ed", "fnorm", "sel")}
    io.update(x2T=x2T_own, uT=uT_own, s5y_g=s5y_g, modc2=modc2_s, outT=outT, pre3=mods[3])
    phase_L5(P, io)
    return P.build()


def kernel(**inputs):
    inp = {k: np.asarray(v, dtype=np.float32) for k, v in inputs.items()}
    x = inp["x"]
    c = inp["c"]
    shared = {
        "mod_w": inp["e_mod_mix_w"][0], "mod_b": fm_cols(inp["e_mod_mix_b"][0]), "norm_w": fm_cols(inp["e_norm_mix"][0]),
        "gnorm": np.ascontiguousarray(np.broadcast_to(inp["e_gla_norm"][0][None, :], (128, 128))),
        "w_o": inp["e_w_o"][0], "nw1": fm_cols(inp["e_norm_ffn"][0]), "mw1": inp["e_mod_ffn_w"][0], "mb1": fm_cols(inp["e_mod_ffn_b"][0]),
        "Wg": inp["e_ffn_gate"][0], "Wu": inp["e_ffn_up"][0], "Wd": inp["e_ffn_down"][0],
        "nw2": fm_cols(inp["o_norm_mix"][0]), "mw2": inp["o_mod_mix_w"][0], "mb2": fm_cols(inp["o_mod_mix_b"][0]),
        "w_in2": inp["o_w_in"][0], "dskip": fm_cols(inp["o_d_skip"][0]), "w_glu": inp["o_w_glu"][0], "w_o2": inp["o_w_o"][0],
        "nw3": fm_cols(inp["o_norm_ffn"][0]), "mw3": inp["o_mod_ffn_w"][0], "mb3": fm_cols(inp["o_mod_ffn_b"][0]),
        "router": inp["o_router"][0], "Eg": inp["o_exp_gate"][0], "Eu": inp["o_exp_up"][0], "Ed": inp["o_exp_down"][0],
        "fnorm": fm_cols(inp["final_norm"]),
    }
    s5 = []
    for gh in range(2):
        s5.append({
            "lamr": s5_pair_layout(inp["o_lam_re"][0], gh), "lami": s5_pair_layout(inp["o_lam_im"][0], gh),
            "logdt": s5_pair_layout(np.broadcast_to(inp["o_log_dt"][0][:, None], (64, 64)), gh),
            "br": s5_pair_layout(inp["o_b_re"][0], gh), "bi": s5_pair_layout(inp["o_b_im"][0], gh),
            "cr": s5_pair_layout(inp["o_c_re"][0].transpose(0, 2, 1), gh), "ci": s5_pair_layout(inp["o_c_im"][0].transpose(0, 2, 1), gh)})
    onehot = np.eye(2, dtype=np.float32)
    xT_full = [np.ascontiguousarray(x[b].T) for b in range(NB)]
    percore = []
    for hh in range(2):
        order = [hh, 1 - hh]
        fb = inp["e_fox_fb"][0].reshape(2, 4)[order].reshape(8)
        w2 = inp["e_gla_w2"][0].reshape(16, 2, 128)[:, order, :].reshape(16, 256)
        b2 = inp["e_gla_b2"][0].reshape(2, 128)[order]
        percore.append({"w_in": np.ascontiguousarray(inp["e_w_in"][0][:, own_cols(hh)]),
                        "fbB": np.ascontiguousarray(np.broadcast_to(fb[None, :], (128, 8))),
                        "w2": np.ascontiguousarray(w2), "b2c": np.ascontiguousarray(b2.T)})
    maps = []
    for core in range(NCORE):
        b, hh = core // 2, core % 2
        m = dict(shared)
        m.update(s5[hh])
        m.update(percore[hh])
        m["xT"] = np.ascontiguousarray(x[b, hh * NT:(hh + 1) * NT].T)
        m["xT_full"] = xT_full[b]
        m["ccol"] = fm_cols(c[b])
        m["sel"] = np.ascontiguousarray(np.broadcast_to(onehot[hh][None, :], (128, 2)))
        maps.append(m)
    res = run_bass_kernel_spmd(build_fused(), maps, core_ids=list(range(NCORE))).results
    out = np.empty((NB, S, D), dtype=np.float32)
    for core in range(NCORE):
        b, hh = core // 2, core % 2
        out[b, hh * NT:(hh + 1) * NT] = res[core]["outT"].T
    return out
```

```python
import contextlib
import numpy as np
import concourse.bass as bass
import concourse.mybir as mybir
from concourse.bass_utils import run_bass_kernel_spmd

F32 = mybir.dt.float32
BF16 = mybir.dt.bfloat16
AF = mybir.ActivationFunctionType
ALU = mybir.AluOpType
AX = mybir.AxisListType

D = 1024
S = 4096
NB = 4
DFF = 2816
NE = 8
EPS = 1e-6
NCORE = 8


class Prog:
    NDSEM = 12
    ENGS = ("pe", "act", "dve", "pool", "sp")

    def __init__(self):
        self.nc = bass.Bass("TRN2", target_bir_lowering=False)
        self.ops = []
        self.uid = 0
        self.banks = [self.ps(f"bank{i}", [128, 512], F32) for i in range(8)]
        self.bank_i = 0
        self.reserved = set()
        self.rot = {}
        self.prefix = ""
        self.phase_base = None

    def inp(self, name, shape, dtype=F32):
        return self.nc.dram_tensor(name, list(shape), dtype, kind="ExternalInput").ap()

    def out(self, name, shape, dtype=F32):
        return self.nc.dram_tensor(name, list(shape), dtype, kind="ExternalOutput").ap()

    def sb(self, name, shape, dtype=F32):
        return self.nc.alloc_sbuf_tensor(self.prefix + name, list(shape), dtype)

    def dram(self, name, shape, dtype=F32):
        return self.nc.dram_tensor(name, list(shape), dtype, kind="Internal").ap()

    def begin_phase(self, prefix):
        if self.phase_base is not None:
            self.nc.sbuf_base = self.phase_base
        else:
            self.phase_base = self.nc.sbuf_base
        self.prefix = prefix
        self.rot = {}
        self.ops.append(("barrier", None, (), (), False))
        self.on_phase()

    def on_phase(self):
        pass

    def barrier(self):
        self.ops.append(("barrier", None, (), (), False))

    def allgather_pair(self, src, dst, r=(), w=()):
        groups = [[2 * i, 2 * i + 1] for i in range(NCORE // 2)]
        self.ops.append(("pool", lambda e: e.collective_compute("AllGather", ALU.bypass, replica_groups=groups, ins=[src], outs=[dst]),
                         tuple(r), tuple(w), "cc"))

    def sel_load(self, dst, tmp, pieces0, pieces1, sel, dkey, tkey, prange=(0, 128), extra_w=(), extra_r=()):
        a, b = prange
        for fn, src in pieces0:
            self.dma(fn(dst), src, r=list(extra_r), w=[dkey] + list(extra_w))
        for fn, src in pieces1:
            self.dma(fn(tmp), src, r=list(extra_r), w=[tkey])
        self.op("dve", lambda e: e.tensor_scalar(out=dst, in0=dst, scalar1=sel[a:b, 0:1], scalar2=None, op0=ALU.mult), r=[dkey, "sel_sb"], w=[dkey])
        self.op("dve", lambda e: e.scalar_tensor_tensor(out=dst, in0=tmp, scalar=sel[a:b, 1:2], in1=dst, op0=ALU.mult, op1=ALU.add),
                r=[dkey, tkey, "sel_sb"], w=[dkey])

    def ps(self, name, shape, dtype=F32):
        return self.nc.alloc_psum_tensor(name, list(shape), dtype)

    def bank(self):
        while True:
            i = self.bank_i
            self.bank_i = (i + 1) % 8
            if i not in self.reserved:
                return i

    def rbuf(self, name, shape, dtype=F32, n=2):
        if name not in self.rot:
            self.rot[name] = [[self.sb(f"{name}{i}", shape, dtype) for i in range(n)], 0]
        lst, i = self.rot[name]
        self.rot[name][1] = (i + 1) % len(lst)
        return lst[i], (self.prefix + name, i)

    def op(self, eng, fn, r=(), w=(), dma=False):
        self.ops.append((eng, fn, tuple(r), tuple(w), dma))

    def dma(self, out, in_, r=(), w=(), q="sp", **kw):
        self.op(q, lambda e: e.dma_start(out=out, in_=in_, **kw), r, w, dma=True)

    def build(self):
        nc = self.nc
        nds = self.NDSEM
        seq = {e: 0 for e in self.ENGS}
        dcount = {e: 0 for e in self.ENGS}
        last_w = {}
        last_r = {}
        known = {e: {} for e in self.ENGS}
        plan = {e: [] for e in self.ENGS}
        pending = {e: {} for e in self.ENGS}
        cur_tok = {}
        for (eng, fn, r, w, is_dma) in self.ops:
            if eng == "barrier":
                for e in self.ENGS:
                    for s_, v_ in cur_tok.items():
                        if pending[e].get(s_, 0) < v_:
                            pending[e][s_] = v_
                continue
            need = dict(pending[eng])
            pending[eng] = {}

            def add(tok):
                if tok is None:
                    return
                s, v = tok
                if need.get(s, 0) < v:
                    need[s] = v
            for k in r:
                add(last_w.get(k))
                if isinstance(k, tuple) and k[0] == "bank":
                    own = ("c", eng)
                    for sn_, t in last_r.get(k, {}).items():
                        if sn_ != own:
                            add(t)
            for k in w:
                add(last_w.get(k))
                for t in last_r.get(k, {}).values():
                    add(t)
            if is_dma == "cc":
                ncc = getattr(self, "_ncc", 0)
                self._ncc = ncc + 1
                sname = ("cc", ncc)
                tok = (sname, 1)
                inc = 1
            elif is_dma:
                d = dcount[eng]
                dcount[eng] += 1
                sname = ("d", eng, d % nds)
                tok = (sname, 16 * (d // nds + 1))
                if d >= nds:
                    add((sname, 16 * (d // nds)))
                inc = 16
            else:
                seq[eng] += 1
                sname = ("c", eng)
                tok = (sname, seq[eng])
                inc = 1
            waits = []
            for s, v in need.items():
                if eng == "pe" and s == ("c", "pe"):
                    continue
                if known[eng].get(s, 0) >= v:
                    continue
                known[eng][s] = v
                waits.append((s, v))
            plan[eng].append((fn, waits, sname, inc))
            cur_tok[tok[0]] = tok[1]
            for k in w:
                last_w[k] = tok
                last_r[k] = {}
            for k in r:
                last_r.setdefault(k, {})[sname] = tok
        final_waits = []
        for e in self.ENGS:
            if seq[e]:
                final_waits.append((("c", e), seq[e]))
            for i in range(min(nds, dcount[e])):
                cnt = (dcount[e] - 1 - i) // nds + 1
                final_waits.append((("d", e, i), 16 * cnt))
        for i in range(getattr(self, "_ncc", 0)):
            final_waits.append((("cc", i), 1))
        semnames = set()
        for e in self.ENGS:
            for (_, waits, sname, _) in plan[e]:
                semnames.add(sname)
        sems = {}
        with contextlib.ExitStack() as st:
            for s in sorted(semnames, key=str):
                sems[s] = st.enter_context(nc.semaphore("_".join(str(x) for x in s)))
            block = st.enter_context(nc.Block())

            def run(engname, final=False):
                def body(e):
                    for (fn, waits, sname, inc) in plan[engname]:
                        for (s, v) in waits:
                            e.wait_ge(sems[s], v)
                        ins = fn(e)
                        ins.then_inc(sems[sname], inc)
                    if final:
                        for (s, v) in final_waits:
                            e.wait_ge(sems[s], v)
                return body
            block.tensor(run("pe"))
            block.scalar(run("act"))
            block.vector(run("dve"))
            block.gpsimd(run("pool"))
            block.sync(run("sp", final=True))
        return nc


class K(Prog):
    SLOT = 1408
    NSLOT = 4

    def __init__(self):
        super().__init__()
        self.on_phase()

    def on_phase(self):
        self.wst = None
        self.slot_i = 0
        self.ones = self.sb("ones_f", [128, 128], F32)
        ones = self.ones
        self.op("pool", lambda e: e.memset(ones[:], 1.0), w=["ones"])

    def make_ident(self):
        self.ident = self.sb("ident_f", [128, 128], F32)
        ident, ones = self.ident, self.ones
        self.op("pool", lambda e: e.affine_select(out=ident[:], in_=ones[:], pattern=[[-1, 128]],
                                                   compare_op=ALU.is_equal, fill=0.0, base=0, channel_multiplier=1),
                r=["ones"], w=["ident"])

    def load(self, name, dram_ap, shape, dtype=F32):
        t = self.sb(name, shape, dtype)
        self.dma(t[:], dram_ap, w=[name, t.name])
        return t

    def wload(self, Wap, k0, KT, c0, cb):
        if self.wst is None:
            self.wst = [self.sb(f"wst{i}", [128, self.SLOT], F32) for i in range(self.NSLOT)]
            self.wbf = [self.sb(f"wbf{i}", [128, self.SLOT], BF16) for i in range(self.NSLOT)]
        s = self.slot_i
        self.slot_i = (s + 1) % self.NSLOT
        assert KT * cb <= self.SLOT
        st = self.wst[s][:, 0:KT * cb].rearrange("p (kt m) -> p kt m", kt=KT)
        bf = self.wbf[s][:, 0:KT * cb].rearrange("p (kt m) -> p kt m", kt=KT)
        src = Wap[k0:k0 + KT * 128, c0:c0 + cb].rearrange("(kt p) m -> p kt m", p=128)
        self.dma(st, src, w=[("wst", s)])
        self.cast_i = getattr(self, "cast_i", 0) + 1
        if self.cast_i % 2:
            self.op("act", lambda e: e.activation(out=bf, in_=st, func=AF.Copy), r=[("wst", s)], w=[("wbf", s)])
        else:
            self.op("dve", lambda e: e.tensor_copy(out=bf, in_=st), r=[("wst", s)], w=[("wbf", s)])
        return bf, ("wbf", s)

    def linear(self, Ws, k0, Kdim, M, rhs, rkeys, N, evac, cbs=128):
        KT = Kdim // 128
        cblocks = [(c0, min(cbs, M - c0)) for c0 in range(0, M, cbs)]
        PF = max(1, self.NSLOT // len(Ws) - 1)
        loaded = {}
        for bi, (c0, cb) in enumerate(cblocks):
            for bj in range(bi, min(bi + PF + 1, len(cblocks))):
                if bj not in loaded:
                    loaded[bj] = [self.wload(W, k0, KT, cblocks[bj][0], cblocks[bj][1]) for W in Ws]
            blocks = loaded.pop(bi)
            for m0 in range(0, cb, 128):
                msz = min(128, cb - m0)
                for n0 in range(0, N, 512):
                    nsz = min(512, N - n0)
                    pss = []
                    for (bf, key) in blocks:
                        b = self.bank()
                        ps = self.banks[b][0:msz, 0:nsz]

                        def mm(e, bf=bf, ps=ps, m0=m0, msz=msz, n0=n0, nsz=nsz):
                            ins = None
                            for kt in range(KT):
                                ins = e.matmul(ps, lhsT=bf[:, kt, m0:m0 + msz], rhs=rhs(kt, n0, nsz),
                                               start=(kt == 0), stop=(kt == KT - 1))
                            return ins
                        self.op("pe", mm, r=[key] + list(rkeys), w=[("bank", b)])
                        pss.append((ps, ("bank", b)))
                    evac(c0 + m0, msz, n0, nsz, pss)

    def adaln(self, tag, csil, W, bcols, pre=None):
        modc = self.sb(f"modc_{tag}", [128, 24], F32)
        key = f"modc_{tag}"
        if pre is not None:
            self.dma(modc[:], pre, w=[key])
            return modc, key

        def evac(m0, msz, n0, nsz, pss):
            ps, pk = pss[0]
            col = m0 // 128
            self.op("dve", lambda e: e.tensor_tensor(out=modc[:, col:col + 1], in0=ps, in1=bcols[:, col:col + 1], op=ALU.add),
                    r=[pk, bcols.name], w=[key])
        self.linear([W], 0, D, 3 * D, lambda kt, n0, nsz: csil[:, kt:kt + 1], [csil.name], 1, evac)
        return modc, key

    def silu_c(self, ccol_ap):
        c = self.load("c_col", ccol_ap, [128, 8])
        csil = self.sb("c_sil", [128, 8], BF16)
        self.op("act", lambda e: e.activation(out=csil[:], in_=c[:], func=AF.Silu), r=["c_col"], w=["c_sil", csil.name])
        return csil

    def wmod(self, tag, modc, mkey, normw):
        wm = self.sb(f"wm_{tag}", [128, 8], F32)
        self.op("dve", lambda e: e.scalar_tensor_tensor(out=wm[:], in0=modc[:, 8:16], scalar=1.0, in1=normw[:],
                                                         op0=ALU.add, op1=ALU.mult),
                r=[mkey, normw.name], w=[wm.name])
        return wm

    def rmsnorm(self, xget, N, wm, shc, ckeys, outs, after=None):
        epsc = self.epsc
        if getattr(self, "ones_b_phase", None) != self.prefix:
            self.ones_b = self.sb("ones_b16", [128, 128], BF16)
            ob_, on_ = self.ones_b, self.ones
            self.op("act", lambda e: e.activation(out=ob_[:], in_=on_[:], func=AF.Copy), r=["ones"], w=["ones_b16"])
            self.ones_b_phase = self.prefix
        ones = self.ones_b
        for n0 in range(0, N, 512):
            xa, xk = xget(n0)
            b = self.bank()
            ps = self.banks[b][:, 0:512]
            for kt in range(8):
                sq, sk = self.rbuf("sq", [128, 512], BF16)
                self.op("act", lambda e, sq=sq, kt=kt, xa=xa: e.activation(out=sq[:], in_=xa[:, kt, :], func=AF.Square),
                        r=[xk], w=[sk])
                self.op("pe", lambda e, sq=sq, kt=kt, ps=ps: e.matmul(ps, lhsT=ones[:], rhs=sq[:], start=(kt == 0), stop=(kt == 7)),
                        r=["ones_b16", sk], w=[("bank", b)])
            rs, rk = self.rbuf("rstd", [128, 512], F32)
            self.op("act", lambda e, rs=rs, ps=ps: e.activation(out=rs[:], in_=ps, func=AF.Sqrt, scale=1.0 / D, bias=epsc[:]),
                    r=[("bank", b), "epsc"], w=[rk])
            self.op("dve", lambda e, rs=rs: e.reciprocal(out=rs[:], in_=rs[:]), r=[rk], w=[rk])
            for kt in range(8):
                tm, tk = self.rbuf("nt", [128, 512], F32)
                self.op("dve", lambda e, tm=tm, kt=kt, xa=xa, rs=rs: e.scalar_tensor_tensor(
                    out=tm[:], in0=xa[:, kt, :], scalar=wm[:, kt:kt + 1], in1=rs[:], op0=ALU.mult, op1=ALU.mult),
                    r=[xk, wm.name, rk], w=[tk])
                for (ot, ok, oeng) in outs:
                    if callable(ot):
                        dst, ok = ot(kt, n0)
                    else:
                        dst = ot[:, kt, n0:n0 + 512]
                    if shc is None:
                        if oeng == "act":
                            self.op("act", lambda e, dst=dst, tm=tm: e.activation(out=dst, in_=tm[:], func=AF.Copy), r=[tk], w=[ok])
                        else:
                            self.op(oeng, lambda e, dst=dst, tm=tm: e.tensor_copy(out=dst, in_=tm[:]), r=[tk], w=[ok])
                    else:
                        sh = shc[:, kt:kt + 1]
                        if oeng == "act":
                            self.op("act", lambda e, dst=dst, tm=tm, sh=sh: e.activation(out=dst, in_=tm[:], func=AF.Identity, bias=sh),
                                    r=[tk] + list(ckeys), w=[ok])
                        else:
                            self.op(oeng, lambda e, dst=dst, tm=tm, sh=sh: e.tensor_scalar(out=dst, in0=tm[:], scalar1=sh, scalar2=None, op0=ALU.add),
                                    r=[tk] + list(ckeys), w=[ok])
            if after is not None:
                after(n0)

    def consts(self):
        self.epsc = self.sb("epsc", [128, 1], F32)
        epsc = self.epsc
        self.op("pool", lambda e: e.memset(epsc[:], EPS), w=["epsc"])

    def ffn(self, hb, hkey, N, Wg, Wu, Wd, xres, xkey, gcol, gkeys, hidden, gate_b=None, gbkey=None):
        HF = DFF // 2
        for half in range(2):
            f0 = half * HF

            def evac_gu(m0, msz, n0, nsz, pss, f0=f0):
                (pg, kg), (pu, ku) = pss
                mt = (m0 - f0) // 128
                ta, tak = self.rbuf("fa", [128, 512], F32)
                tb, tbk = self.rbuf("fb", [128, 512], F32)
                self.op("act", lambda e: e.activation(out=ta[0:msz, 0:nsz], in_=pg, func=AF.Silu), r=[kg], w=[tak])
                dst = hidden[0:msz, mt, n0:n0 + nsz]
                if gate_b is None:
                    self.op("dve", lambda e: e.tensor_tensor(out=dst, in0=ta[0:msz, 0:nsz], in1=pu, op=ALU.mult),
                            r=[tak, ku], w=["hidden"])
                else:
                    self.op("dve", lambda e: e.tensor_tensor(out=tb[0:msz, 0:nsz], in0=ta[0:msz, 0:nsz], in1=pu, op=ALU.mult),
                            r=[tak, ku], w=[tbk])
                    self.op("pool", lambda e: e.tensor_tensor(out=dst, in0=tb[0:msz, 0:nsz], in1=gate_b[0:msz, n0:n0 + nsz], op=ALU.mult),
                            r=[tbk, gbkey], w=["hidden"])
            WgH = Wg[:, f0:f0 + HF]
            WuH = Wu[:, f0:f0 + HF]
            self.linear([WgH, WuH], 0, D, HF, lambda kt, n0, nsz: hb[:, kt, n0:n0 + nsz], [hkey], N,
                        lambda m0, msz, n0, nsz, pss, f0=f0: evac_gu(m0 + f0, msz, n0, nsz, pss))

            def evac_d(m0, msz, n0, nsz, pss):
                ps, pk = pss[0]
                mt = m0 // 128
                dst = xres[:, mt, n0:n0 + nsz]
                self.op("dve", lambda e: e.scalar_tensor_tensor(out=dst, in0=ps, scalar=gcol[:, mt:mt + 1], in1=dst,
                                                                 op0=ALU.mult, op1=ALU.add),
                        r=[pk, xkey] + list(gkeys), w=[xkey])
            self.linear([Wd], f0, HF, D, lambda kt, n0, nsz: hidden[:, kt, n0:n0 + nsz], ["hidden"], N, evac_d)


def fm_cols(v):
    v = np.asarray(v, dtype=np.float32)
    return np.ascontiguousarray(v.reshape(-1, 128).T)


IN_COLS = 3096
NT = 2048


def io_L1(P):
    return dict(xT=P.inp("xT", [D, NT]), ccol=P.inp("ccol", [128, 8]), mod_w=P.inp("mod_w", [D, 3 * D]), mod_b=P.inp("mod_b", [128, 24]),
                norm_w=P.inp("norm_w", [128, 8]), w_in=P.inp("w_in", [D, IN_COLS]), projT=P.out("projT", [IN_COLS, NT]), modc=P.out("modc", [128, 24]))


def build_L1():
    P = K()
    phase_L1(P, io_L1(P))
    return P.build()


def phase_L1(P, io):
    P.consts()
    xT, ccol, mw, mb, nw, w_in, projT, modo = (io[k] for k in ("xT", "ccol", "mod_w", "mod_b", "norm_w", "w_in", "projT", "modc"))
    csil = P.silu_c(ccol)
    bcols = P.load("mod_b_sb", mb, [128, 24])
    normw = P.load("norm_w_sb", nw, [128, 8])
    modc, mkey = P.adaln("m0", csil, mw, bcols, io.get("pre0"))
    P.dma(modo, modc[:], r=[mkey], q="pool")
    wm = P.wmod("m0", modc, mkey, normw)
    ntok = xT.shape[1]
    ncols = w_in.shape[1]
    hb = P.sb("hb", [128, 8, ntok], BF16)
    xv = xT.rearrange("(kt p) n -> p kt n", p=128)

    def xget(n0):
        xb, xk = P.rbuf("xchunk", [128, 8, 512], F32)
        P.dma(xb[:], xv[:, :, n0:n0 + 512], w=[xk])
        return xb, xk
    P.rmsnorm(xget, ntok, wm, modc, [mkey], [(hb, "hb", "act")])

    gath = io.get("gath")
    gdone = set()

    def evac(m0, msz, n0, nsz, pss):
        ps, pk = pss[0]
        ob, okk = P.rbuf("osb", [128, 512], F32, n=6)
        P.op("dve", lambda e: e.tensor_copy(out=ob[0:msz, 0:nsz], in_=ps), r=[pk], w=[okk])
        P.dma(projT[m0:m0 + msz, n0:n0 + nsz], ob[0:msz, 0:nsz], r=[okk], w=[("projrow", m0 // 128)], q="pool")
        if gath is not None and n0 + nsz == ntok:
            for k, (b0, b1) in enumerate(gath.bounds):
                if k not in gdone and b1 <= m0 + msz:
                    gdone.add(k)
                    P.allgather_pair(gath.own[b0:b1, :], gath.dsts[k], r=[("projrow", t) for t in range(b0 // 128, (b1 - 1) // 128 + 1)])
    P.linear([w_in], 0, D, ncols, lambda kt, n0, nsz: hb[:, kt, n0:n0 + nsz], ["hb"], ntok, evac)
    assert gath is None or len(gdone) == len(gath.bounds)


NP_ = 1024


def load_cast_chunks(P, srcT, t0, N, dst, dkey):
    sv = srcT.rearrange("(kt p) n -> p kt n", p=128)
    for n0 in range(0, N, 512):
        xb, xk = P.rbuf("xchunk", [128, 8, 512], F32)
        P.dma(xb[:], sv[:, :, t0 + n0:t0 + n0 + 512], w=[xk])
        P.op("act", lambda e, xb=xb, n0=n0: e.activation(out=dst[:, :, n0:n0 + 512], in_=xb[:], func=AF.Copy), r=[xk], w=[dkey])


L3_IN = dict(x0T=[D, NT], mixT=[D, NT], modc0=[128, 24], w_o=[D, D], ccol=[128, 8], nw1=[128, 8], mw1=[D, 3 * D], mb1=[128, 24],
             Wg=[D, DFF], Wu=[D, DFF], Wd=[DFF, D], nw2=[128, 8], mw2=[D, 3 * D], mb2=[128, 24], w_in2=[D, D])
L3_OUT = dict(x2T=[D, NT], uT=[D, NT], modc2=[128, 24])


def build_L3():
    P = K()
    io = {k: P.inp(k, v) for k, v in L3_IN.items()}
    io.update({k: P.out(k, v) for k, v in L3_OUT.items()})
    phase_L3(P, io)
    return P.build()


def phase_L3(P, io):
    P.consts()
    (x0T, mixT, modc0_d, w_o, ccol, nw1, mw1, mb1, Wg, Wu, Wd, nw2, mw2, mb2, w_in2, x2T, uT, modo) = (
        io.get(k) for k in ("x0T", "mixT", "modc0", "w_o", "ccol", "nw1", "mw1", "mb1", "Wg", "Wu", "Wd", "nw2", "mw2", "mb2", "w_in2",
                            "x2T", "uT", "modc2"))
    csil = P.silu_c(ccol)
    sel3 = P.load("sel_sb", io["sel"], [128, 2]) if "sel" in io else None
    modc0 = P.load("modc0_sb", modc0_d, [128, 24])
    b1 = P.load("mb1_sb", mb1, [128, 24])
    b2 = P.load("mb2_sb", mb2, [128, 24])
    n1 = P.load("nw1_sb", nw1, [128, 8])
    n2 = P.load("nw2_sb", nw2, [128, 8])
    modc1, mk1 = P.adaln("m1", csil, mw1, b1, io.get("pre1"))
    modc2, mk2 = P.adaln("m2", csil, mw2, b2, io.get("pre2"))
    P.dma(modo, modc2[:], r=[mk2], q="pool")
    wm1 = P.wmod("m1", modc1, mk1, n1)
    wm2 = P.wmod("m2", modc2, mk2, n2)

    xres = P.sb("xres", [128, 8, NP_], F32)
    hbA = P.sb("hbA", [128, 8, NP_], BF16)
    hbB = P.sb("hbB", [128, 8, NP_], BF16)
    hidden = P.sb("hidden", [128, 11, NP_], BF16)
    x0v = x0T.rearrange("(kt p) n -> p kt n", p=128)
    x2v = x2T.rearrange("(kt p) n -> p kt n", p=128)
    for ps_ in range(NT // NP_):
        t0 = ps_ * NP_
        P.dma(xres[:], x0v[:, :, t0:t0 + NP_], w=["xres"])
        if "mix_g" in io:
            mg = io["mix_g"]
            for n0 in range(0, NP_, 512):
                mixbf = bool(io.get("mix_bf16"))
                if mixbf:
                    xb2, xk2 = P.rbuf("xchunkb", [128, 8, 512], BF16)
                else:
                    xb, xk = P.rbuf("xchunk", [128, 8, 512], F32)
                    xb2, xk2 = P.rbuf("xchunk", [128, 8, 512], F32)

                def mpieces(h):
                    c0 = h * NT + t0 + n0
                    out = []
                    for kt_, (rk, r0) in enumerate(((0, 0), (0, 128), (1, 0), (1, 128), (0, 256), (0, 384), (1, 256), (1, 384))):
                        src = mg[rk][r0:r0 + 128, c0:c0 + 512]
                        out.append(((lambda base, kt_=kt_: base[:, kt_, :]), src))
                    return out
                if mixbf:
                    P.sel_load(hbA[:, :, n0:n0 + 512], xb2[:], mpieces(0), mpieces(1), sel3, "hbA", xk2, extra_r=mg.keys())
                else:
                    P.sel_load(xb[:], xb2[:], mpieces(0), mpieces(1), sel3, xk, xk2)
                    P.op("act", lambda e, xb=xb, n0=n0: e.activation(out=hbA[:, :, n0:n0 + 512], in_=xb[:], func=AF.Copy), r=[xk], w=["hbA"])
        else:
            load_cast_chunks(P, mixT, t0, NP_, hbA, "hbA")

        def evac_o(m0, msz, n0, nsz, pss):
            ps, pk = pss[0]
            mt = m0 // 128
            dst = xres[:, mt, n0:n0 + nsz]
            P.op("dve", lambda e: e.scalar_tensor_tensor(out=dst, in0=ps, scalar=modc0[:, 16 + mt:17 + mt], in1=dst,
                                                          op0=ALU.mult, op1=ALU.add),
                 r=[pk, "xres", "modc0_sb"], w=["xres"])
        P.linear([w_o], 0, D, D, lambda kt, n0, nsz: hbA[:, kt, n0:n0 + nsz], ["hbA"], NP_, evac_o)
        P.rmsnorm(lambda n0: (xres[:, :, n0:n0 + 512], "xres"), NP_, wm1, modc1, [mk1], [(hbB, "hbB", "act")])
        P.ffn(hbB, "hbB", NP_, Wg, Wu, Wd, xres, "xres", modc1[:, 16:24], [mk1], hidden)
        P.dma(x2v[:, :, t0:t0 + NP_], xres[:], r=["xres"], q="pool")
        P.rmsnorm(lambda n0: (xres[:, :, n0:n0 + 512], "xres"), NP_, wm2, modc2, [mk2], [(hbA, "hbA", "act")])

        def evac_u(m0, msz, n0, nsz, pss, t0=t0):
            ps, pk = pss[0]
            ob, okk = P.rbuf("osb", [128, 512], F32, n=3)
            P.op("dve", lambda e: e.tensor_copy(out=ob[0:msz, 0:nsz], in_=ps), r=[pk], w=[okk])
            P.dma(uT[m0:m0 + msz, t0 + n0:t0 + n0 + nsz], ob[0:msz, 0:nsz], r=[okk], q="pool")
            if io.get("uT_bf") is not None:
                obb, obk2 = P.rbuf("osbb", [128, 512], BF16, n=3)
                P.op("act", lambda e: e.activation(out=obb[0:msz, 0:nsz], in_=ob[0:msz, 0:nsz], func=AF.Copy), r=[okk], w=[obk2])
                P.dma(io["uT_bf"][m0:m0 + msz, t0 + n0:t0 + n0 + nsz], obb[0:msz, 0:nsz], r=[obk2], q="pool")
        P.linear([w_in2], 0, D, D, lambda kt, n0, nsz: hbA[:, kt, n0:n0 + nsz], ["hbA"], NP_, evac_u)


L5_IN = dict(x2T=[D, NT], s5yT=[D, NT], uT=[D, NT], dskip=[128, 8], w_glu=[D, D], w_o2=[D, D], modc2=[128, 24], ccol=[128, 8],
             nw3=[128, 8], mw3=[D, 3 * D], mb3=[128, 24], router=[D, NE], Eg=[NE, D, DFF], Eu=[NE, D, DFF], Ed=[NE, DFF, D], fnorm=[128, 8])


def build_L5():
    P = K()
    io = {k: P.inp(k, v) for k, v in L5_IN.items()}
    io["outT"] = P.out("outT", [D, NT])
    phase_L5(P, io)
    return P.build()


def phase_L5(P, io):
    P.consts()
    P.make_ident()
    IDENT, ONES = P.ident, P.ones
    (x2T, s5yT, uT, dsk, w_glu, w_o2, modc2_d, ccol, nw3, mw3, mb3, router, Eg, Eu, Ed, fnw, outT) = (
        io.get(k) for k in ("x2T", "s5yT", "uT", "dskip", "w_glu", "w_o2", "modc2", "ccol", "nw3", "mw3", "mb3", "router", "Eg", "Eu", "Ed",
                        "fnorm", "outT"))
    sel = P.load("sel_sb", io["sel"], [128, 2]) if "sel" in io else None
    syg = io.get("s5y_g")
    csil = P.silu_c(ccol)
    modc2 = P.load("modc2_sb", modc2_d, [128, 24])
    b3 = P.load("mb3_sb", mb3, [128, 24])
    n3 = P.load("nw3_sb", nw3, [128, 8])
    fn = P.load("fnorm_sb", fnw, [128, 8])
    dcol = P.load("dskip_sb", dsk, [128, 8])
    rt = P.load("router_sb", router.rearrange("(kt p) e -> p kt e", p=128), [128, 8, NE])
    modc3, mk3 = P.adaln("m3", csil, mw3, b3, io.get("pre3"))
    wm3 = P.wmod("m3", modc3, mk3, n3)

    xres = P.sb("xres", [128, 8, NP_], F32)
    hbA = P.sb("hbA", [128, 8, NP_], BF16)
    hidden = P.sb("hidden", [128, 11, NP_], BF16)
    gate_b = P.sb("gate_b", [128, NE, NP_], BF16)
    ygb = hidden
    x2v = x2T.rearrange("(kt p) n -> p kt n", p=128)
    syv = s5yT.rearrange("(kt p) n -> p kt n", p=128) if s5yT is not None else None
    uv = uT.rearrange("(kt p) n -> p kt n", p=128)
    ov = outT.rearrange("(kt p) n -> p kt n", p=128)
    for ps_ in range(NT // NP_):
        t0 = ps_ * NP_
        if sel is None or syg is not None:
            P.dma(xres[:], x2v[:, :, t0:t0 + NP_], w=["xres"])
        else:
            for n0 in range(0, NP_, 512):
                xb, xk = P.rbuf("xchunk", [128, 8, 512], F32)
                P.dma(xres[:, :, n0:n0 + 512], x2v[:, :, t0 + n0:t0 + n0 + 512], w=["xres"])
                P.dma(xb[:], x2v[:, :, NT + t0 + n0:NT + t0 + n0 + 512], w=[xk])
                P.op("dve", lambda e, n0=n0: e.tensor_scalar(out=xres[:, :, n0:n0 + 512], in0=xres[:, :, n0:n0 + 512], scalar1=sel[:, 0:1], scalar2=None, op0=ALU.mult),
                     r=["xres", "sel_sb"], w=["xres"])
                P.op("dve", lambda e, n0=n0, xb=xb: e.scalar_tensor_tensor(out=xres[:, :, n0:n0 + 512], in0=xb[:], scalar=sel[:, 1:2], in1=xres[:, :, n0:n0 + 512],
                                                                          op0=ALU.mult, op1=ALU.add), r=["xres", xk, "sel_sb"], w=["xres"])
        for n0 in range(0, NP_, 512):
            if sel is None:
                sb_, sk = P.rbuf("xchunk", [128, 8, 512], F32)
                P.dma(sb_[:], syv[:, :, t0 + n0:t0 + n0 + 512], w=[sk])
                ub, uk = P.rbuf("xchunk", [128, 8, 512], F32)
                P.dma(ub[:], uv[:, :, t0 + n0:t0 + n0 + 512], w=[uk])
            for kt in range(8):
                ta, tak = P.rbuf("fa", [128, 512], F32)
                tb, tbk = P.rbuf("fb", [128, 512], F32)
                if syg is not None:
                    c0 = t0 + n0
                    pcs = []
                    rk_, r0_ = kt // 4, (kt % 4) * 128
                    for src in (syg[rk_][r0_:r0_ + 128, c0:c0 + 512], syg[rk_][r0_:r0_ + 128, NT + c0:NT + c0 + 512], uv[:, kt, c0:c0 + 512]):
                        pb_, pk_ = P.rbuf("pc", [128, 512], F32, n=6)
                        P.dma(pb_[:], src, w=[pk_])
                        pcs.append((pb_, pk_))
                    (s0, s0k), (s1, s1k), (u0, u0k) = pcs
                    P.op("dve", lambda e, s0=s0: e.tensor_scalar(out=s0[:], in0=s0[:], scalar1=sel[:, 0:1], scalar2=None, op0=ALU.mult), r=[s0k, "sel_sb"], w=[s0k])
                    P.op("dve", lambda e, s0=s0, s1=s1: e.scalar_tensor_tensor(out=s0[:], in0=s1[:], scalar=sel[:, 1:2], in1=s0[:], op0=ALU.mult, op1=ALU.add),
                         r=[s0k, s1k, "sel_sb"], w=[s0k])
                    P.op("dve", lambda e, ta=ta, u0=u0, s0=s0, kt=kt: e.scalar_tensor_tensor(out=ta[:], in0=u0[:], scalar=dcol[:, kt:kt + 1], in1=s0[:], op0=ALU.mult, op1=ALU.add),
                         r=[s0k, u0k, "dskip_sb"], w=[tak])
                elif sel is not None:
                    c0 = t0 + n0
                    pcs = []
                    for (src, off) in ((syv, 0), (uv, 0), (syv, NT), (uv, NT)):
                        pb_, pk_ = P.rbuf("pc", [128, 512], F32, n=8)
                        P.dma(pb_[:], src[:, kt, off + c0:off + c0 + 512], w=[pk_])
                        pcs.append((pb_, pk_))
                    (s0, s0k), (u0, u0k), (s1, s1k), (u1, u1k) = pcs
                    P.op("dve", lambda e, u0=u0, s0=s0, kt=kt: e.scalar_tensor_tensor(out=s0[:], in0=u0[:], scalar=dcol[:, kt:kt + 1], in1=s0[:], op0=ALU.mult, op1=ALU.add),
                         r=[s0k, u0k, "dskip_sb"], w=[s0k])
                    P.op("dve", lambda e, u1=u1, s1=s1, kt=kt: e.scalar_tensor_tensor(out=s1[:], in0=u1[:], scalar=dcol[:, kt:kt + 1], in1=s1[:], op0=ALU.mult, op1=ALU.add),
                         r=[s1k, u1k, "dskip_sb"], w=[s1k])
                    P.op("dve", lambda e, s0=s0: e.tensor_scalar(out=s0[:], in0=s0[:], scalar1=sel[:, 0:1], scalar2=None, op0=ALU.mult), r=[s0k, "sel_sb"], w=[s0k])
                    P.op("dve", lambda e, ta=ta, s0=s0, s1=s1: e.scalar_tensor_tensor(out=ta[:], in0=s1[:], scalar=sel[:, 1:2], in1=s0[:], op0=ALU.mult, op1=ALU.add),
                         r=[s0k, s1k, "sel_sb"], w=[tak])
                else:
                    P.op("dve", lambda e, ta=ta, ub=ub, sb_=sb_, kt=kt: e.scalar_tensor_tensor(
                        out=ta[:], in0=ub[:, kt, :], scalar=dcol[:, kt:kt + 1], in1=sb_[:, kt, :], op0=ALU.mult, op1=ALU.add),
                        r=[sk, uk, "dskip_sb"], w=[tak])
                P.op("act", lambda e, ta=ta, tb=tb: e.activation(out=tb[:], in_=ta[:], func=AF.Square), r=[tak], w=[tbk])
                P.op("dve", lambda e, tb=tb: e.tensor_scalar(out=tb[:], in0=tb[:], scalar1=0.044715, scalar2=1.0, op0=ALU.mult, op1=ALU.add),
                     r=[tbk], w=[tbk])
                P.op("dve", lambda e, ta=ta, tb=tb: e.tensor_tensor(out=tb[:], in0=tb[:], in1=ta[:], op=ALU.mult), r=[tak, tbk], w=[tbk])
                P.op("act", lambda e, tb=tb: e.activation(out=tb[:], in_=tb[:], func=AF.Sigmoid, scale=1.5957691216057308), r=[tbk], w=[tbk])
                P.op("pool", lambda e, ta=ta, tb=tb, kt=kt, n0=n0: e.tensor_tensor(out=hbA[:, kt, n0:n0 + 512], in0=ta[:], in1=tb[:], op=ALU.mult),
                     r=[tak, tbk], w=["hbA"])

        def evac_glu(m0, msz, n0, nsz, pss):
            ps, pk = pss[0]
            mt = m0 // 128
            ta, tak = P.rbuf("fa", [128, 512], F32)
            P.op("act", lambda e: e.activation(out=ta[:, 0:nsz], in_=ps, func=AF.Sigmoid), r=[pk], w=[tak])
            P.op("dve", lambda e: e.tensor_tensor(out=ygb[:, mt, n0:n0 + nsz], in0=hbA[:, mt, n0:n0 + nsz], in1=ta[:, 0:nsz], op=ALU.mult),
                 r=[tak, "hbA"], w=["hidden"])
        P.linear([w_glu], 0, D, D, lambda kt, n0, nsz: hbA[:, kt, n0:n0 + nsz], ["hbA"], NP_, evac_glu)

        def evac_o(m0, msz, n0, nsz, pss):
            ps, pk = pss[0]
            mt = m0 // 128
            dst = xres[:, mt, n0:n0 + nsz]
            P.op("dve", lambda e: e.scalar_tensor_tensor(out=dst, in0=ps, scalar=modc2[:, 16 + mt:17 + mt], in1=dst,
                                                          op0=ALU.mult, op1=ALU.add),
                 r=[pk, "xres", "modc2_sb"], w=["xres"])
        P.linear([w_o2], 0, D, D, lambda kt, n0, nsz: ygb[:, kt, n0:n0 + nsz], ["hidden"], NP_, evac_o)

        cur = {}

        def xget(n0):
            cur["hf"], cur["hfk"] = P.rbuf("xchunk", [128, 8, 512], F32)
            return xres[:, :, n0:n0 + 512], "xres"

        def hf_dst(kt, n0):
            return cur["hf"][:, kt, :], cur["hfk"]

        def router_chunk(n0):
            hf, hfk = cur["hf"], cur["hfk"]
            T = []
            for tt in range(4):
                t = dict(tt=tt)
                t["b"] = P.bank()
                t["lps"] = P.banks[t["b"]][:, 0:NE]
                t["lg"], t["lgk"] = P.rbuf("lg", [128, NE], F32, n=4)
                t["l2"], t["l2k"] = P.rbuf("lg2", [128, NE], F32, n=4)
                t["sc"], t["sck"] = P.rbuf("rsc", [128, 4], F32, n=4)
                t["dg"], t["dgk"] = P.rbuf("dg", [128, NE, 128], F32, n=4)
                T.append(t)

            def s_logits(t):
                tt, lps = t["tt"], t["lps"]

                def mm(e):
                    ins = None
                    for kt in range(8):
                        ins = e.matmul(lps, lhsT=hf[:, kt, tt * 128:(tt + 1) * 128], rhs=rt[:, kt, :], start=(kt == 0), stop=(kt == 7))
                    return ins
                P.op("pe", mm, r=[hfk, "router_sb"], w=[("bank", t["b"])])
            stages = [
                s_logits,
                lambda t: P.op("dve", lambda e: e.tensor_copy(out=t["lg"][:], in_=t["lps"]), r=[("bank", t["b"])], w=[t["lgk"]]),
                lambda t: P.op("dve", lambda e: e.tensor_reduce(out=t["sc"][:, 0:1], in_=t["lg"][:], axis=AX.X, op=ALU.max), r=[t["lgk"]], w=[t["sck"]]),
                lambda t: P.op("dve", lambda e: e.tensor_scalar(out=t["l2"][:], in0=t["lg"][:], scalar1=t["sc"][:, 0:1], scalar2=-1e30,
                                                                  op0=ALU.is_equal, op1=ALU.mult), r=[t["lgk"], t["sck"]], w=[t["l2k"]]),
                lambda t: P.op("dve", lambda e: e.tensor_tensor(out=t["l2"][:], in0=t["l2"][:], in1=t["lg"][:], op=ALU.add), r=[t["lgk"], t["l2k"]], w=[t["l2k"]]),
                lambda t: P.op("dve", lambda e: e.tensor_reduce(out=t["sc"][:, 1:2], in_=t["l2"][:], axis=AX.X, op=ALU.max), r=[t["l2k"], t["sck"]], w=[t["sck"]]),
                lambda t: P.op("dve", lambda e: e.tensor_scalar(out=t["sc"][:, 2:3], in0=t["sc"][:, 0:1], scalar1=-1.0, scalar2=None, op0=ALU.mult),
                               r=[t["sck"]], w=[t["sck"]]),
                lambda t: P.op("dve", lambda e: e.tensor_scalar(out=t["l2"][:], in0=t["lg"][:], scalar1=t["sc"][:, 1:2], scalar2=None, op0=ALU.is_ge),
                               r=[t["lgk"], t["sck"], t["l2k"]], w=[t["l2k"]]),
                lambda t: P.op("act", lambda e: e.activation(out=t["lg"][:], in_=t["lg"][:], func=AF.Exp, bias=t["sc"][:, 2:3]), r=[t["lgk"], t["sck"]], w=[t["lgk"]]),
                lambda t: P.op("dve", lambda e: e.tensor_tensor(out=t["lg"][:], in0=t["lg"][:], in1=t["l2"][:], op=ALU.mult), r=[t["lgk"], t["l2k"]], w=[t["lgk"]]),
                lambda t: P.op("dve", lambda e: e.tensor_reduce(out=t["sc"][:, 3:4], in_=t["lg"][:], axis=AX.X, op=ALU.add), r=[t["lgk"], t["sck"]], w=[t["sck"]]),
                lambda t: P.op("dve", lambda e: e.reciprocal(out=t["sc"][:, 3:4], in_=t["sc"][:, 3:4]), r=[t["sck"]], w=[t["sck"]]),
                lambda t: P.op("dve", lambda e: e.tensor_scalar(out=t["lg"][:], in0=t["lg"][:], scalar1=t["sc"][:, 3:4], scalar2=None, op0=ALU.mult),
                               r=[t["lgk"], t["sck"]], w=[t["lgk"]]),
            ]
            for st_ in stages:
                for t in T:
                    st_(t)
            for t in T:
                for ex in range(NE):
                    P.op("dve", lambda e, t=t, ex=ex: e.tensor_scalar(out=t["dg"][:, ex, :], in0=IDENT[:], scalar1=t["lg"][:, ex:ex + 1], scalar2=None, op0=ALU.mult),
                         r=["ident", t["lgk"]], w=[t["dgk"]])
            for t in T:
                tok = n0 + t["tt"] * 128
                for half in range(2):
                    b2 = P.bank()
                    gps = P.banks[b2][:, 0:512]
                    P.op("pe", lambda e, gps=gps, t=t, half=half: e.matmul(gps, lhsT=ONES[:], rhs=t["dg"][:, 4 * half:4 * half + 4, :], start=True, stop=True),
                         r=["ones", t["dgk"]], w=[("bank", b2)])
                    P.op("act", lambda e, gps=gps, half=half, tok=tok: e.activation(
                        out=gate_b[:, 4 * half:4 * half + 4, tok:tok + 128], in_=gps.rearrange("p (e n) -> p e n", e=4), func=AF.Copy),
                        r=[("bank", b2)], w=["gate_b"])
        P.rmsnorm(xget, NP_, wm3, modc3, [mk3], [(hbA, "hbA", "act"), (hf_dst, None, "pool")], after=router_chunk)

        for ex in range(NE):
            P.ffn(hbA, "hbA", NP_, Eg[ex], Eu[ex], Ed[ex], xres, "xres", modc3[:, 16:24], [mk3], hidden,
                  gate_b=gate_b[:, ex, :], gbkey="gate_b")

        def xget2(n0):
            cur["o"], cur["ok"] = P.rbuf("xchunk", [128, 8, 512], F32)
            return xres[:, :, n0:n0 + 512], "xres"

        def o_dst(kt, n0):
            return cur["o"][:, kt, :], cur["ok"]

        def store(n0, t0=t0):
            P.dma(ov[:, :, t0 + n0:t0 + n0 + 512], cur["o"][:], r=[cur["ok"]], q="pool")
        P.rmsnorm(xget2, NP_, fn, None, [], [(o_dst, None, "act")], after=store)


NJ = S // 8
JH = 256


def io_L4(P):
    io = dict(uT=P.inp("uT", [512, S]), s5yT=P.out("s5yT", [512, S]))
    for n in ("lamr", "lami", "logdt"):
        io[n] = P.inp(n + "_d", [128, 16])
    for n in ("br", "bi", "cr", "ci"):
        io[n] = P.inp(n + "_d", [128, 16, 16])
    return io


def build_L4(stage=9):
    P = K()
    phase_L4(P, io_L4(P), stage)
    return P.build()


def phase_L4(P, io, stage=9):
    P.make_ident()
    IDENT, ONES = P.ident, P.ones
    uT_d = io.get("uT")
    yT_d = io["s5yT"]
    sel4 = P.load("sel_sb", io["sel"], [128, 2]) if "sel" in io else None
    sc_in = {n: P.load(n, io[n], [128, 16]) for n in ("lamr", "lami", "logdt")}
    vin = {n: P.load(n, io[n], [128, 16, 16]) for n in ("br", "bi", "cr", "ci")}

    def act(o, i, func, **kw):
        P.op("act", lambda e: e.activation(out=o[0], in_=i[0], func=func, **kw), r=[i[1]], w=[o[1]])

    def tt(o, a, b, op, eng="dve"):
        P.op(eng, lambda e: e.tensor_tensor(out=o[0], in0=a[0], in1=b[0], op=op), r=[a[1], b[1], o[1]], w=[o[1]])

    def ts(o, a, s1, s2, op0, op1=None):
        if op1 is None:
            P.op("dve", lambda e: e.tensor_scalar(out=o[0], in0=a[0], scalar1=s1, scalar2=None, op0=op0), r=[a[1], o[1]], w=[o[1]])
        else:
            P.op("dve", lambda e: e.tensor_scalar(out=o[0], in0=a[0], scalar1=s1, scalar2=s2, op0=op0, op1=op1), r=[a[1], o[1]], w=[o[1]])

    def new(name, shape=(128, 16), dt=F32):
        t = P.sb(name, list(shape), dt)
        return (t[:], name), t

    lamr = (sc_in["lamr"][:], "lamr")
    lami = (sc_in["lami"][:], "lami")
    logdt = (sc_in["logdt"][:], "logdt")
    dt_, _ = new("dt")
    mag, _ = new("mag")
    ang, _ = new("ang")
    t1, _ = new("t1")
    t2, _ = new("t2")
    sn, _ = new("sn")
    cs, _ = new("cs")
    ar, art = new("ar")
    ai, ait = new("ai")
    PI = float(np.pi)
    act(dt_, logdt, AF.Exp)
    tt(t1, lamr, dt_, ALU.mult)
    act(mag, t1, AF.Exp)
    tt(ang, lami, dt_, ALU.mult)
    ki_t = P.sb("ki", [128, 16], mybir.dt.int32)
    t3, _ = new("t3")

    def wrap_angle(dst, src, offset):
        ts(t3, src, 1.0 / (2 * PI), offset / (2 * PI), ALU.mult, ALU.add)
        P.op("dve", lambda e: e.tensor_copy(out=ki_t[:], in_=t3[0]), r=["t3"], w=["ki"])
        P.op("dve", lambda e: e.tensor_copy(out=t3[0], in_=ki_t[:]), r=["ki"], w=["t3"])
        ts(dst, src, offset, None, ALU.add)
        P.op("dve", lambda e: e.scalar_tensor_tensor(out=dst[0], in0=t3[0], scalar=-2 * PI, in1=dst[0], op0=ALU.mult, op1=ALU.add),
             r=["t3", dst[1]], w=[dst[1]])
        ts(t3, dst, PI, None, ALU.is_gt)
        P.op("dve", lambda e: e.scalar_tensor_tensor(out=dst[0], in0=t3[0], scalar=-2 * PI, in1=dst[0], op0=ALU.mult, op1=ALU.add),
             r=["t3", dst[1]], w=[dst[1]])
        ts(dst, dst, PI, None, ALU.min)
        ts(dst, dst, -PI, None, ALU.max)
    wrap_angle(t1, ang, 0.0)
    act(sn, t1, AF.Sin)
    wrap_angle(t2, ang, 0.5 * PI)
    act(cs, t2, AF.Sin)
    tt(ar, mag, cs, ALU.mult)
    tt(ai, mag, sn, ALU.mult)
    den, _ = new("den")
    nr, _ = new("nr")
    fr, frt = new("fr")
    fi, fit = new("fi")
    tt(den, lamr, lamr, ALU.mult)
    tt(t2, lami, lami, ALU.mult)
    tt(den, den, t2, ALU.add)
    P.op("dve", lambda e: e.reciprocal(out=den[0], in_=den[0]), r=["den"], w=["den"])
    ts(nr, ar, -1.0, None, ALU.add)
    tt(fr, nr, lamr, ALU.mult)
    tt(t2, ai, lami, ALU.mult)
    tt(fr, fr, t2, ALU.add)
    tt(fr, fr, den, ALU.mult)
    tt(fi, ai, lamr, ALU.mult)
    tt(t2, nr, lami, ALU.mult)
    tt(fi, fi, t2, ALU.subtract)
    tt(fi, fi, den, ALU.mult)
    bbr, bbrt = new("bbr", (128, 16, 16))
    bbi, bbit = new("bbi", (128, 16, 16))
    tv, tvt = new("tv", (128, 16, 16))
    frb = (frt[:].unsqueeze(2).to_broadcast([128, 16, 16]), "fr")
    fib = (fit[:].unsqueeze(2).to_broadcast([128, 16, 16]), "fi")
    br = (vin["br"][:], "br")
    bi = (vin["bi"][:], "bi")
    tt(bbr, frb, br, ALU.mult)
    tt(tv, fib, bi, ALU.mult)
    tt(bbr, bbr, tv, ALU.subtract)
    tt(bbi, frb, bi, ALU.mult)
    tt(tv, fib, br, ALU.mult)
    tt(bbi, bbi, tv, ALU.add)
    pwr_t = P.sb("pwr", [128, 16, 9], F32)
    pwi_t = P.sb("pwi", [128, 16, 9], F32)
    npr_t = P.sb("npr", [128, 16, 8], F32)
    npi_t = P.sb("npi", [128, 16, 8], F32)
    rpr_t = P.sb("rpr", [128, 16, 8], F32)
    rpi_t = P.sb("rpi", [128, 16, 8], F32)
    iar, _ = new("iar")
    iai, _ = new("iai")
    tt(t1, ar, ar, ALU.mult)
    tt(t2, ai, ai, ALU.mult)
    tt(t1, t1, t2, ALU.add)
    P.op("dve", lambda e: e.reciprocal(out=t1[0], in_=t1[0]), r=["t1"], w=["t1"])
    tt(iar, ar, t1, ALU.mult)
    tt(iai, ai, t1, ALU.mult)
    ts(iai, iai, -1.0, None, ALU.mult)

    def powers(prt, pit, name_r, name_i, a_r, a_i, n):
        P.op("pool", lambda e: e.memset(prt[:, :, 0:1], 1.0), w=[name_r])
        P.op("pool", lambda e: e.memset(pit[:, :, 0:1], 0.0), w=[name_i])
        for m in range(1, n):
            pr0 = (prt[:, :, m - 1], name_r)
            pi0 = (pit[:, :, m - 1], name_i)
            pr1 = (prt[:, :, m], name_r)
            pi1 = (pit[:, :, m], name_i)
            tt(pr1, pr0, a_r, ALU.mult)
            tt(t2, pi0, a_i, ALU.mult)
            tt(pr1, pr1, t2, ALU.subtract)
            tt(pi1, pr0, a_i, ALU.mult)
            tt(t2, pi0, a_r, ALU.mult)
            tt(pi1, pi1, t2, ALU.add)
    powers(pwr_t, pwi_t, "pwr", "pwi", ar, ai, 9)
    powers(npr_t, npi_t, "npr", "npi", iar, iai, 8)
    for s_ in range(8):
        P.op("pool", lambda e, s_=s_: e.tensor_copy(out=rpr_t[:, :, s_], in_=pwr_t[:, :, 7 - s_]), r=["pwr"], w=["rpr"])
        P.op("pool", lambda e, s_=s_: e.tensor_copy(out=rpi_t[:, :, s_], in_=pwi_t[:, :, 7 - s_]), r=["pwi"], w=["rpi"])

    def cprod(outr, outi, xr, xi, pr_ap, pi_ap, pkeys, n, tmp, neg_imag=False):
        shape = [128, 16, n, 16]
        xrb = (xr[0].unsqueeze(2).to_broadcast(shape), xr[1])
        xib = (xi[0].unsqueeze(2).to_broadcast(shape), xi[1])
        prb = (pr_ap.unsqueeze(3).to_broadcast(shape), pkeys[0])
        pib = (pi_ap.unsqueeze(3).to_broadcast(shape), pkeys[1])
        tt(outr, xrb, prb, ALU.mult)
        tt(tmp, xib, pib, ALU.mult)
        tt(outr, outr, tmp, ALU.subtract)
        tt(outi, xrb, pib, ALU.mult)
        tt(tmp, xib, prb, ALU.mult)
        tt(outi, outi, tmp, ALU.add)
        if neg_imag:
            ts(outi, outi, -1.0, None, ALU.mult)

    cpr, cprt = new("cpr", (128, 16, 8, 16))
    cpi, cpit = new("cpi", (128, 16, 8, 16))
    ctmp, ctmpt = new("ctmp", (128, 16, 9, 16))
    ctmp8 = (ctmpt[:, :, 0:8, :], "ctmp")
    WB = P.sb("WB", [128, 32, 128], BF16)
    Wi = P.sb("Wi", [128, 32, 128], BF16)
    WCr = P.sb("WCr", [128, 16, 8, 16], BF16)
    WCi = P.sb("WCi", [128, 16, 8, 16], BF16)
    cprod(cpr, cpi, bbr, bbi, rpr_t[:], rpi_t[:], ["rpr", "rpi"], 8, ctmp8)
    for gl in range(32):
        q, base = gl // 2, 64 * (gl % 2)
        b = P.bank()
        ps = P.banks[b][:, 0:128]

        def mmt(e, q=q, base=base, ps=ps):
            e.matmul(ps[:, 0:64], lhsT=cprt[base:base + 64, q, :, :], rhs=IDENT[base:base + 64, base:base + 64], start=True, stop=True)
            return e.matmul(ps[:, 64:128], lhsT=cpit[base:base + 64, q, :, :], rhs=IDENT[base:base + 64, base:base + 64], start=True, stop=True)
        P.op("pe", mmt, r=["cpr", "cpi", "ident"], w=[("bank", b)])
        P.op("act", lambda e, gl=gl, ps=ps: e.activation(out=WB[:, gl, :], in_=ps, func=AF.Copy), r=[("bank", b)], w=["WB"])
    gr, grt = new("gr", (128, 16, 9, 16))
    gi, git = new("gi", (128, 16, 9, 16))
    cprod(gr, gi, (vin["cr"][:], "cr"), (vin["ci"][:], "ci"), pwr_t[:], pwi_t[:], ["pwr", "pwi"], 9, ctmp, neg_imag=True)
    P.op("act", lambda e: e.activation(out=WCr[:], in_=grt[:, :, 1:9, :], func=AF.Copy), r=["gr"], w=["WCr"])
    P.op("act", lambda e: e.activation(out=WCi[:], in_=git[:, :, 1:9, :], func=AF.Copy), r=["gi"], w=["WCi"])
    cprod(cpr, cpi, bbr, bbi, npr_t[:], npi_t[:], ["npr", "npi"], 8, ctmp8)
    mask = P.sb("mask", [128, 8, 16], F32)
    P.op("pool", lambda e: e.affine_select(out=mask[:], in_=ONES[:].rearrange("p (a b) -> p a b", a=8), pattern=[[16, 8], [0, 16]],
                                           compare_op=ALU.is_ge, fill=0.0, base=15, channel_multiplier=-1), r=["ones"], w=["mask"])
    for gl in range(32):
        q, base = gl // 2, 64 * (gl % 2)
        b = P.bank()
        ps = P.banks[b][:, 0:128]

        def mmi(e, q=q, base=base, ps=ps):
            e.matmul(ps, lhsT=cprt[base:base + 64, q, :, :], rhs=grt[base:base + 64, q, 0:8, :], start=True, stop=False)
            return e.matmul(ps, lhsT=cpit[base:base + 64, q, :, :], rhs=git[base:base + 64, q, 0:8, :], start=False, stop=True)
        P.op("pe", mmi, r=["cpr", "cpi", "gr", "gi"], w=[("bank", b)])
        P.op("dve", lambda e, gl=gl, ps=ps: e.tensor_tensor(out=Wi[:, gl, :], in0=ps, in1=mask[:].rearrange("p a b -> p (a b)"), op=ALU.mult),
             r=[("bank", b), "mask"], w=["Wi"])
    A8r2 = P.sb("A8r2", [128, 2, 16], F32)
    A8x = P.sb("A8x", [128, 2, 16], F32)
    P.op("pool", lambda e: e.tensor_copy(out=A8r2[:, 0, :], in_=pwr_t[:, :, 8]), r=["pwr"], w=["A8r2"])
    P.op("pool", lambda e: e.tensor_copy(out=A8r2[:, 1, :], in_=pwr_t[:, :, 8]), r=["pwr"], w=["A8r2"])
    P.op("pool", lambda e: e.tensor_copy(out=A8x[:, 1, :], in_=pwi_t[:, :, 8]), r=["pwi"], w=["A8x"])
    P.op("dve", lambda e: e.tensor_scalar(out=A8x[:, 0, :], in0=pwi_t[:, :, 8], scalar1=-1.0, scalar2=None, op0=ALU.mult), r=["pwi", "A8x"], w=["A8x"])

    Zb = P.sb("Zb", [128, 8, 240], BF16)
    P.op("pool", lambda e: e.memset(Zb[:], 0.0), w=["Zb"])
    for r_ in range(8):
        P.op("pool", lambda e, r_=r_: e.affine_select(out=Zb[:, r_, 112:128], in_=ONES[:, 0:16], pattern=[[-1, 16]], compare_op=ALU.is_equal,
                                                      fill=0.0, base=-16 * r_, channel_multiplier=1), r=["ones"], w=["Zb"])
    Ub = P.sb("Ub", [128, 32, NJ], BF16)
    HJ = NJ // 2
    for tl in range(4):
        for hf in range(2):
            ubf = bool(io.get("u_bf16"))
            if ubf:
                ub16, ubk = P.rbuf("ub16", [128, S // 2], BF16, n=2)
                ut2, utk2 = P.rbuf("ustmpb", [128, S // 2], BF16, n=2)
                ug = io["u_g"]
                P.sel_load(ub16[:], ut2[:], [(lambda base: base, ug[hf][128 * tl:128 * tl + 128, :])],
                           [(lambda base: base, ug[hf][512 + 128 * tl:512 + 128 * tl + 128, :])], sel4, ubk, utk2, extra_r=ug.keys())
            else:
                us, usk = P.rbuf("ustage", [128, S // 2], F32, n=1)
            if ubf:
                pass
            elif "u_g" in io:
                ut2, utk2 = P.rbuf("ustmp", [128, S // 2], F32, n=1)
                ug = io["u_g"]
                P.sel_load(us[:], ut2[:], [(lambda base: base, ug[hf][128 * tl:128 * tl + 128, :])],
                           [(lambda base: base, ug[hf][512 + 128 * tl:512 + 128 * tl + 128, :])], sel4, usk, utk2)
            else:
                P.dma(us[:], uT_d[128 * tl:128 * tl + 128, hf * (S // 2):(hf + 1) * (S // 2)], w=[usk])
            if not ubf:
                ub16, ubk = P.rbuf("ub16", [128, S // 2], BF16, n=1)
                P.op("pool", lambda e, us=us, ub16=ub16: e.tensor_copy(out=ub16[:], in_=us[:]), r=[usk], w=[ubk])
            ubv = ub16[:].rearrange("p (j s) -> p s j", s=8)
            for g8 in range(8):
                gl = 8 * tl + g8
                b = P.bank()
                ps = P.banks[b][:, 0:HJ]

                def mmu(e, ps=ps, g8=g8, ubv=ubv):
                    ins = None
                    for s_ in range(8):
                        ins = e.matmul(ps, lhsT=Zb[:, g8, 112 - 16 * s_:240 - 16 * s_], rhs=ubv[:, s_, :], start=(s_ == 0), stop=(s_ == 7))
                    return ins
                P.op("pe", mmu, r=["Zb", ubk], w=[("bank", b)])
                P.op("act", lambda e, ps=ps, gl=gl, hf=hf: e.activation(out=Ub[:, gl, hf * HJ:(hf + 1) * HJ], in_=ps, func=AF.Copy),
                     r=[("bank", b)], w=[("Ub", gl)])

    JB = 64
    hist = P.sb("hist", [128, 2, 16, NJ + 1], BF16)
    P.op("pool", lambda e: e.memset(hist[:, :, :, 0:1], 0.0), w=["hist0"])
    a8r = (pwr_t[:, :, 8], "pwr")
    a8i = (pwi_t[:, :, 8], "pwi")
    m8, m8t = new("m8")
    ur, urt = new("ur")
    ui, uit = new("ui")
    tt(t1, a8r, a8r, ALU.mult)
    tt(t2, a8i, a8i, ALU.mult)
    tt(t1, t1, t2, ALU.add)
    act(m8, t1, AF.Sqrt)
    P.op("dve", lambda e: e.reciprocal(out=t1[0], in_=m8[0]), r=["m8"], w=["t1"])
    tt(ur, a8r, t1, ALU.mult)
    tt(ui, a8i, t1, ALU.mult)
    Er_t = P.sb("Er", [128, 16, JB], F32)
    Ei_t = P.sb("Ei", [128, 16, JB], F32)
    P.op("pool", lambda e: e.memset(Er_t[:, :, 0:1], 1.0), w=["Er"])
    P.op("pool", lambda e: e.memset(Ei_t[:, :, 0:1], 0.0), w=["Ei"])
    stp_r, sprt = new("stp_r")
    stp_i, spit = new("stp_i")
    P.op("dve", lambda e: e.tensor_copy(out=sprt[:], in_=urt[:]), r=["ur"], w=["stp_r"])
    P.op("dve", lambda e: e.tensor_copy(out=spit[:], in_=uit[:]), r=["ui"], w=["stp_i"])
    etmp_t = P.sb("etmp", [128, 16, JB], F32)
    w_ = 1
    while w_ < JB:
        shp = [128, 16, w_]
        e0r = (Er_t[:, :, 0:w_], "Er")
        e0i = (Ei_t[:, :, 0:w_], "Ei")
        e1r = (Er_t[:, :, w_:2 * w_], "Er")
        e1i = (Ei_t[:, :, w_:2 * w_], "Ei")
        tmpv = (etmp_t[:, :, 0:w_], "etmp")
        sr = (sprt[:].unsqueeze(2).to_broadcast(shp), "stp_r")
        si = (spit[:].unsqueeze(2).to_broadcast(shp), "stp_i")
        tt(e1r, e0r, sr, ALU.mult)
        tt(tmpv, e0i, si, ALU.mult)
        tt(e1r, e1r, tmpv, ALU.subtract)
        tt(e1i, e0r, si, ALU.mult)
        tt(tmpv, e0i, sr, ALU.mult)
        tt(e1i, e1i, tmpv, ALU.add)
        w_ *= 2
        if w_ < JB:
            tt(t1, stp_r, stp_r, ALU.mult)
            tt(t2, stp_i, stp_i, ALU.mult)
            tt(t3, stp_r, stp_i, ALU.mult)
            tt(stp_r, t1, t2, ALU.subtract)
            ts(stp_i, t3, 2.0, None, ALU.mult)
    Er = (Er_t[:], "Er")
    Ei = (Ei_t[:], "Ei")
    P.barrier()

    def carve(tile4, idx):
        flat = tile4[:].rearrange("p a b c -> p (a b c)")
        return flat[:, idx * 16 * JB:(idx + 1) * 16 * JB].rearrange("p (a b) -> p a b", a=16)
    Zc = [carve(grt, 0), carve(grt, 1)]
    Zt = [carve(git, 0), carve(git, 1)]
    Xt = [carve(ctmpt, 0), carve(ctmpt, 1)]
    Xu = [carve(cprt, 0), carve(cprt, 1)]
    tA = carve(cpit, 0)
    inr, inrt = new("inr")
    ini, init_ = new("ini")
    P.op("pool", lambda e: e.memset(inrt[:], 0.0), w=["inr"])
    P.op("pool", lambda e: e.memset(init_[:], 0.0), w=["ini"])
    nblk = NJ // JB if stage in (3, 9) else 0
    def z_stage(blk):
        j0 = blk * JB
        for bq in range(4):
            b = P.bank()
            ps = P.banks[b]

            def mmz(e, bq=bq, ps=ps, j0=j0):
                ins = None
                for ql in range(4):
                    q = 4 * bq + ql
                    for c in range(2):
                        col = (ql * 2 + c) * JB
                        e.matmul(ps[0:64, col:col + JB], lhsT=WB[:, 2 * q, 64 * c:64 * c + 64], rhs=Ub[:, 2 * q, j0:j0 + JB], start=True, stop=True)
                        ins = e.matmul(ps[64:128, col:col + JB], lhsT=WB[:, 2 * q + 1, 64 * c:64 * c + 64], rhs=Ub[:, 2 * q + 1, j0:j0 + JB], start=True, stop=True)
                return ins
            P.op("pe", mmz, r=["WB"] + [("Ub", g) for g in range(8 * bq, 8 * bq + 8)], w=[("bank", b)])
            psv = ps[:, 0:8 * JB].rearrange("p (q c j) -> p q c j", q=4, c=2)
            for c in range(2):
                P.op("act", lambda e, bq=bq, c=c, psv=psv: e.activation(out=Zc[c][:, 4 * bq:4 * bq + 4, :], in_=psv[:, :, c, :], func=AF.Copy),
                     r=[("bank", b)], w=[f"Z{c}"])
    if nblk:
        z_stage(0)
    for blk in range(nblk):
        j0 = blk * JB
        Zr = (Zc[0][:], "Z0")
        Zi = (Zc[1][:], "Z1")
        Ztr = (Zt[0][:], "Zt0")
        Zti = (Zt[1][:], "Zt1")
        tAv = (tA[:], "tA")
        tt(Ztr, Zr, Er, ALU.mult)
        tt(tAv, Zi, Ei, ALU.mult)
        tt(Ztr, Ztr, tAv, ALU.add)
        tt(Zti, Zi, Er, ALU.mult)
        tt(tAv, Zr, Ei, ALU.mult)
        tt(Zti, Zti, tAv, ALU.subtract)
        if blk + 1 < nblk:
            z_stage(blk + 1)
        if blk > 0:
            xlr = (Xu[0][:, :, JB - 1], "Xu0")
            xli = (Xu[1][:, :, JB - 1], "Xu1")
            tt(inr, xlr, ur, ALU.mult)
            tt(t1, xli, ui, ALU.mult)
            tt(inr, inr, t1, ALU.subtract)
            tt(ini, xlr, ui, ALU.mult)
            tt(t1, xli, ur, ALU.mult)
            tt(ini, ini, t1, ALU.add)
        for c, (ink, intile) in enumerate((("inr", inrt), ("ini", init_))):
            for q in range(16):
                P.op("dve", lambda e, c=c, q=q, intile=intile: e.tensor_tensor_scan(
                    out=Xt[c][:, q, :], data0=m8t[:, q:q + 1].to_broadcast([128, JB]), data1=Zt[c][:, q, :],
                    initial=intile[:, q:q + 1], op0=ALU.mult, op1=ALU.add),
                    r=["m8", f"Zt{c}", ink, f"Xt{c}"], w=[f"Xt{c}"])
        Xtr = (Xt[0][:], "Xt0")
        Xti = (Xt[1][:], "Xt1")
        Xr = (Xu[0][:], "Xu0")
        Xi = (Xu[1][:], "Xu1")
        tt(Xr, Xtr, Er, ALU.mult)
        tt(tAv, Xti, Ei, ALU.mult)
        tt(Xr, Xr, tAv, ALU.subtract)
        tt(Xi, Xtr, Ei, ALU.mult)
        tt(tAv, Xti, Er, ALU.mult)
        tt(Xi, Xi, tAv, ALU.add)
        for c in range(2):
            P.op("act", lambda e, c=c, j0=j0: e.activation(out=hist[:, c, :, j0 + 1:j0 + 1 + JB], in_=Xu[c][:], func=AF.Copy),
                 r=[f"Xu{c}"], w=["hist"])

    for gl in range(32 if stage >= 4 else 0):
        q, base = gl // 2, 64 * (gl % 2)
        b = P.bank()
        ps = P.banks[b][:, 0:NJ]

        def mmy(e, gl=gl, q=q, base=base, ps=ps):
            e.matmul(ps, lhsT=Wi[:, gl, :], rhs=Ub[:, gl, :], start=True, stop=False)
            e.matmul(ps, lhsT=WCr[base:base + 64, q, :, :], rhs=hist[base:base + 64, 0, q, 0:NJ], start=False, stop=False)
            return e.matmul(ps, lhsT=WCi[base:base + 64, q, :, :], rhs=hist[base:base + 64, 1, q, 0:NJ], start=False, stop=True)
        P.op("pe", mmy, r=["Wi", "WCr", "WCi", ("Ub", gl), "hist", "hist0"], w=[("bank", b)])
        P.op("act", lambda e, gl=gl, ps=ps: e.activation(out=Ub[:, gl, :], in_=ps, func=AF.Copy), r=[("bank", b)], w=[("Ub", gl)])
    for tl in range(4 if stage >= 4 else 0):
        for hf in range(2):
            yt, ytk = P.rbuf("ustage", [128, S // 2], F32, n=2 if io.get("u_bf16") else 1)
            ytv = yt[:].rearrange("p (j s) -> p s j", s=8)
            for t_ in range(8):
                b = P.bank()
                ps = P.banks[b][:, 0:HJ]

                def mmv(e, ps=ps, t_=t_, tl=tl, hf=hf):
                    ins = None
                    for g8 in range(8):
                        ins = e.matmul(ps, lhsT=Zb[:, t_, 112 - 16 * g8:240 - 16 * g8], rhs=Ub[:, 8 * tl + g8, hf * HJ:(hf + 1) * HJ],
                                       start=(g8 == 0), stop=(g8 == 7))
                    return ins
                P.op("pe", mmv, r=["Zb"] + [("Ub", 8 * tl + g8) for g8 in range(8)], w=[("bank", b)])
                P.op("act", lambda e, ps=ps, t_=t_, ytv=ytv: e.activation(out=ytv[:, t_, :], in_=ps, func=AF.Copy), r=[("bank", b)], w=[ytk])
            P.dma(yT_d[128 * tl:128 * tl + 128, hf * (S // 2):(hf + 1) * (S // 2)], yt[:], r=[ytk], w=[("yrow", tl)], q="pool")
            if io.get("gath") is not None and hf == 1:
                io["gath"].gather(only=[tl], r=[("yrow", tl)])


def s5_pair_layout(a, gh):
    sub = a[32 * gh:32 * gh + 32]
    sub = sub.reshape((16, 2) + sub.shape[1:])
    sub = np.moveaxis(sub, 0, 2)
    return np.ascontiguousarray(sub.reshape((128, 16) + sub.shape[3:]))


L2_IN = dict(projT=[IN_COLS, S], fbB=[128, 8], w2=[16, 256], b2c=[128, 2], gnorm=[128, 128])


def build_L2(v=0, selm=False):
    P = K()
    io = {k: P.inp(k, sh) for k, sh in L2_IN.items() if not (selm and k == "projT")}
    if selm:
        io["proj_g"] = P.inp("proj_g", [2, IN_COLS, NT])
        io["sel"] = P.inp("sel", [128, 2])
        io["mixT"] = P.out("mixT", [512, S])
    else:
        io["mixT"] = P.out("mixT", [D, S])
    phase_L2(P, io, v)
    return P.build()


def phase_L2(P, io, v):
    P.make_ident()
    IDENT, ONES = P.ident, P.ones
    mixT = io["mixT"]
    RB = io.get("rowbase", dict(fq=0, fk=512, fv=1024, ff=1536, gq=1544, gk=1800, gv=2056, gg=2568, glr=3080))
    own_out = bool(io.get("own_out"))
    selm = "sel" in io
    if selm:
        sel = P.load("sel_sb", io["sel"], [128, 2])
        pg = io["proj_g"]
        T16 = P.sb("T16", [128, S], F32)

        def pieces(r0, nr):
            return [((lambda base, h=h: base[:, h * NT:(h + 1) * NT]), pg[h][r0:r0 + nr, :]) for h in range(2)]

        def load_rows(dst_tile, p0, nr, row_of_v, dkey, extra_w=()):
            P.sel_load(dst_tile[p0:p0 + nr, :], T16[p0:p0 + nr, :], pieces(row_of_v(0), nr), pieces(row_of_v(1), nr), sel, dkey, "T16", (p0, p0 + nr),
                       extra_w=extra_w)

        def load_rows_nosel(dst_tile, p0, nr, r0, dkey):
            for fn, src in pieces(r0, nr):
                P.dma(fn(dst_tile[p0:p0 + nr, :]), src, w=[dkey])
    else:
        projT = io["projT"]

        def load_rows(dst_tile, p0, nr, row_of_v, dkey, extra_w=()):
            r0 = row_of_v(v)
            P.dma(dst_tile[p0:p0 + nr, :], projT[r0:r0 + nr, :], w=[dkey] + list(extra_w))

        def load_rows_nosel(dst_tile, p0, nr, r0, dkey):
            P.dma(dst_tile[p0:p0 + nr, :], projT[r0:r0 + nr, :], w=[dkey])
    gn_d = io["gnorm"]

    A16 = P.sb("A16", [128, S], F32)
    B16 = P.sb("B16", [128, S], F32)
    C16 = P.sb("C16", [128, 8192], BF16)
    D16 = P.sb("D16", [128, S], F32)
    E8a = P.sb("E8a", [128, S], BF16)
    E8b = P.sb("E8b", [128, S], BF16)
    E8c = P.sb("E8c", [128, S], BF16)
    E8d = P.sb("E8d", [128, S], BF16)
    ones_bf = P.sb("ones_bf", [128, 128], BF16)
    ident_bf = P.sb("ident_bf", [128, 128], BF16)
    P.op("act", lambda e: e.activation(out=ones_bf[:], in_=ONES[:], func=AF.Copy), r=["ones"], w=["ones_bf"])
    P.op("act", lambda e: e.activation(out=ident_bf[:], in_=IDENT[:], func=AF.Copy), r=["ident"], w=["ident_bf"])
    zer = P.sb("zer", [128, 512], F32)
    P.op("pool", lambda e: e.memset(zer[:], 0.0), w=["zer"])
    negone = P.sb("negone", [128, 1], F32)
    P.op("pool", lambda e: e.memset(negone[:], -1.0), w=["negone"])
    nfb8 = P.load("nfb_sb", io["fbB"], [128, 8])
    P.op("dve", lambda e: e.tensor_scalar(out=nfb8[:], in0=nfb8[:], scalar1=-1.0, scalar2=None, op0=ALU.mult), r=["nfb_sb"], w=["nfb_sb"])
    if selm:
        nfb = P.sb("nfb4", [128, 4], F32)
        P.op("dve", lambda e: e.tensor_scalar(out=nfb[:], in0=nfb8[:, 0:4], scalar1=sel[:, 0:1], scalar2=None, op0=ALU.mult), r=["nfb_sb", "sel_sb"], w=["nfb_sb"])
        P.op("dve", lambda e: e.scalar_tensor_tensor(out=nfb[:], in0=nfb8[:, 4:8], scalar=sel[:, 1:2], in1=nfb[:], op0=ALU.mult, op1=ALU.add),
             r=["nfb_sb", "sel_sb"], w=["nfb_sb"])
    else:
        nfb = nfb8[:, 4 * v:4 * v + 4]
    maskneg = P.sb("maskneg", [128, 4, 512], F32)
    for d in range(4):
        P.op("pool", lambda e, d=d: e.affine_select(out=maskneg[:, d, :], in_=zer[:], pattern=[[1, 512]], compare_op=ALU.is_ge,
                                                    fill=-30000.0, base=-128 * d, channel_multiplier=-1), r=["zer"], w=["maskneg"])

    vext = E8d[:].rearrange("p (b m) -> p b m", b=32)
    P.op("pool", lambda e: e.memset(vext[:, :, 64:128], 1.0), w=["khat"])
    sel64 = P.sb("sel64", [128, 64], F32)
    P.op("pool", lambda e: e.memset(sel64[:], 0.0), w=["sel64"])
    P.op("pool", lambda e: e.memset(sel64[64:65, :], 1.0), w=["sel64"])
    qa, ka, hi_t = E8a, E8b, E8c
    ncs = P.sb("ncs", [128, 32], F32)
    for i in range(4):
        load_rows(A16, 0, 64, lambda v_: RB["fq"] + (4 * v_ + i) * 64, "A16lo")
        load_rows(B16, 0, 64, lambda v_: RB["fk"] + (4 * v_ + i) * 64, "B16")
        load_rows(B16, 64, 64, lambda v_: RB["fv"] + (4 * v_ + i) * 64, "B16v")
        for g4 in range(4):
            bv = P.bank()
            psv = P.banks[bv]

            def mmv(e, g4=g4, psv=psv):
                ins = None
                for bl in range(8):
                    blk = 8 * g4 + bl
                    ins = e.matmul(psv[:, 64 * bl:64 * bl + 64], lhsT=B16[64:128, 128 * blk:128 * blk + 128], rhs=IDENT[64:128, 64:128], start=True, stop=True)
                return ins
            P.op("pe", mmv, r=["B16v", "ident"], w=[("bank", bv)])
            P.op("act", lambda e, g4=g4, psv=psv: e.activation(out=vext[:, 8 * g4:8 * g4 + 8, 0:64], in_=psv.rearrange("p (b d) -> p b d", b=8), func=AF.Copy),
                 r=[("bank", bv)], w=["khat"])
        P.op("pool", lambda e: e.memset(A16[64:128, :], 0.0), w=["A16hi"])
        load_rows(A16, 64, 1, lambda v_: RB["ff"] + 4 * v_ + i, "A16hi")
        load_rows(A16, 96, 1, lambda v_: RB["ff"] + 4 * v_ + i, "A16hi")
        P.op("act", lambda e: e.activation(out=qa[0:64, :], in_=A16[0:64, :], func=AF.Copy, scale=0.125), r=["A16lo"], w=["qa_lo"])
        P.op("act", lambda e: e.activation(out=ka[0:64, :], in_=B16[0:64, :], func=AF.Copy), r=["B16"], w=["ka"])
        P.op("pool", lambda e: e.memset(ka[64:128, :], 0.0), w=["ka"])
        P.op("pool", lambda e: e.memset(ka[64:65, :], 1.0), w=["ka"])
        P.op("pool", lambda e: e.memset(ka[96:97, :], 1.0), w=["ka"])
        P.op("act", lambda e, i=i: e.activation(out=A16[64:128, :], in_=A16[64:128, :], func=AF.Exp, scale=-1.0, bias=nfb[64:128, i:i + 1]),
             r=["A16hi", "nfb_sb"], w=["A16hi"])
        P.op("act", lambda e: e.activation(out=A16[64:128, :], in_=A16[64:128, :], func=AF.Ln, bias=1.0), r=["A16hi"], w=["A16hi"])
        P.op("dve", lambda e: e.tensor_tensor_scan(out=A16[64:128, :], data0=ONES[64:128, 0:1].to_broadcast([64, S]), data1=A16[64:128, :],
                                                   initial=0.0, op0=ALU.mult, op1=ALU.subtract), r=["A16hi", "ones"], w=["A16hi"])
        P.op("act", lambda e: e.activation(out=hi_t[64:128, :], in_=A16[64:128, :], func=AF.Copy), r=["A16hi"], w=["hi_t"])
        P.op("pool", lambda e: e.tensor_copy(out=qa[64:96, :], in_=hi_t[64:96, :]), r=["hi_t"], w=["qa_hi"])
        P.op("dve", lambda e: e.tensor_tensor(out=qa[96:128, :], in0=A16[96:128, :], in1=hi_t[96:128, :], op=ALU.subtract),
             r=["A16hi", "hi_t"], w=["qa_hi"])
        bn = P.bank()
        psn = P.banks[bn]

        def mmn(e, psn=psn):
            ins = None
            for blk in range(32):
                ins = e.matmul(psn[:, blk:blk + 1], lhsT=A16[64:65, 128 * blk:128 * blk + 128], rhs=negone[64:65, 0:1], start=True, stop=True)
            return ins
        P.op("pe", mmn, r=["A16hi", "negone"], w=[("bank", bn)])
        P.op("dve", lambda e, psn=psn: e.tensor_copy(out=ncs[:], in_=psn[:, 0:32]), r=[("bank", bn)], w=["ncs"])
        for I in range(8):
            bo = P.bank()
            P.reserved.add(bo)
            ps_o = P.banks[bo][:, :]
            nJ = 4 * I + 4
            LA = 3
            pend = []
            for J in range(nJ + LA):
                if J < nJ:
                    b = P.bank()
                    ps_s = P.banks[b][:, :]
                    P.op("pe", lambda e, ps_s=ps_s, J=J, I=I: e.matmul(ps_s, lhsT=ka[:, 128 * J:128 * J + 128], rhs=qa[:, 512 * I:512 * I + 512], start=True, stop=True),
                         r=["ka", "qa_lo", "qa_hi"], w=[("bank", b)])
                    pT, pk = P.rbuf("pT", [128, 512], BF16, n=5)
                    if J >= 4 * I:
                        tmpm, tmk = P.rbuf("fa", [128, 512], F32)
                        P.op("dve", lambda e, ps_s=ps_s, tmpm=tmpm, d=J - 4 * I: e.tensor_tensor(out=tmpm[:], in0=ps_s, in1=maskneg[:, d, :], op=ALU.add),
                             r=[("bank", b), "maskneg"], w=[tmk])
                        P.op("act", lambda e, tmpm=tmpm, pT=pT, J=J: e.activation(out=pT[:], in_=tmpm[:], func=AF.Exp, bias=ncs[:, J:J + 1]),
                             r=[tmk, "ncs"], w=[pk])
                    else:
                        P.op("act", lambda e, ps_s=ps_s, pT=pT, J=J: e.activation(out=pT[:], in_=ps_s, func=AF.Exp, bias=ncs[:, J:J + 1]),
                             r=[("bank", b), "ncs"], w=[pk])
                    pend.append((J, pT, pk))
                if J >= LA:
                    Jo, pTo, pko = pend.pop(0)
                    P.op("pe", lambda e, pTo=pTo, Jo=Jo, ps_o=ps_o, nJ=nJ: e.matmul(ps_o, lhsT=vext[:, Jo, :], rhs=pTo[:], start=(Jo == 0), stop=(Jo == nJ - 1)),
                         r=[pko, "khat"], w=[("bank", bo)])
            xs, xsk = P.rbuf("fx", [128, 512], F32)
            P.op("act", lambda e, xs=xs, ps_o=ps_o: e.activation(out=xs[:], in_=ps_o, func=AF.Copy), r=[("bank", bo)], w=[xsk])
            P.reserved.discard(bo)
            bd = P.bank()
            ps_d = P.banks[bd][0:64, :]
            P.op("pe", lambda e, xs=xs, ps_d=ps_d: e.matmul(ps_d, lhsT=sel64[:], rhs=xs[:], start=True, stop=True), r=[xsk, "sel64"], w=[("bank", bd)])
            rd, rdk = P.rbuf("rd", [64, 512], F32)
            ob, obk = P.rbuf("osb", [64, 512], BF16 if io.get("out_bf16") else F32, n=3)
            P.op("dve", lambda e, rd=rd, ps_d=ps_d: e.reciprocal(out=rd[:], in_=ps_d), r=[("bank", bd)], w=[rdk])
            P.op("dve", lambda e, rd=rd, ob=ob, xs=xs: e.tensor_tensor(out=ob[:], in0=xs[0:64, :], in1=rd[:], op=ALU.mult), r=[xsk, rdk], w=[obk])
            fr0 = i * 64 if (selm or own_out) else (4 * v + i) * 64
            P.dma(mixT[fr0:fr0 + 64, 512 * I:512 * I + 512], ob[:], r=[obk], w=[("foxrow", i)], q="pool")
            if io.get("gath") is not None and I == 7 and i % 2 == 1:
                io["gath"].gather(only=[i // 2], r=[("foxrow", i - 1), ("foxrow", i)])

    gvb = C16[:].rearrange("p (b i d) -> p b i d", b=32, i=2)
    for i in range(2):
        load_rows(D16, 0, 128, lambda v_: RB["gv"] + (2 * v_ + i) * 128, "D16")
        for g4 in range(8):
            bv = P.bank()
            psv = P.banks[bv]

            def mmgv(e, g4=g4, psv=psv):
                ins = None
                for bl in range(4):
                    blk = 4 * g4 + bl
                    ins = e.matmul(psv[:, 128 * bl:128 * bl + 128], lhsT=D16[:, 128 * blk:128 * blk + 128], rhs=IDENT[:], start=True, stop=True)
                return ins
            P.op("pe", mmgv, r=["D16", "ident"], w=[("bank", bv)])
            P.op("act", lambda e, g4=g4, psv=psv, i=i: e.activation(out=gvb[:, 4 * g4:4 * g4 + 4, i, :], in_=psv.rearrange("p (b d) -> p b d", b=4), func=AF.Copy),
                 r=[("bank", bv)], w=["C16", ("C16", 0), ("C16", 1), ("C16", 2), ("C16", 3)])
    load_rows_nosel(B16, 0, 16, RB["glr"], "B16")
    glr_b = E8d[0:16, :]
    P.op("act", lambda e: e.activation(out=glr_b, in_=B16[0:16, :], func=AF.Copy), r=["B16"], w=["khat"])
    w2_all = P.load("w2_all", io["w2"], [16, 256])
    if selm:
        w2_f = P.sb("w2_f", [16, 128], F32)
        P.op("dve", lambda e: e.tensor_scalar(out=w2_f[:], in0=w2_all[:, 0:128], scalar1=sel[0:16, 0:1], scalar2=None, op0=ALU.mult), r=["w2_all", "sel_sb"], w=["w2_f"])
        P.op("dve", lambda e: e.scalar_tensor_tensor(out=w2_f[:], in0=w2_all[:, 128:256], scalar=sel[0:16, 1:2], in1=w2_f[:], op0=ALU.mult, op1=ALU.add),
             r=["w2_all", "sel_sb", "w2_f"], w=["w2_f"])
    else:
        w2_f = w2_all[:, 128 * v:128 * v + 128]
    w2_b = P.sb("w2_b", [16, 128], BF16)
    P.op("act", lambda e: e.activation(out=w2_b[:], in_=w2_f[:], func=AF.Copy), r=["w2_f", "w2_all"], w=["w2_b"])
    nb2f = P.load("nb2_sb", io["b2c"], [128, 2])
    P.op("dve", lambda e: e.tensor_scalar(out=nb2f[:], in0=nb2f[:], scalar1=-1.0, scalar2=None, op0=ALU.mult), r=["nb2_sb"], w=["nb2_sb"])
    if selm:
        nb2t = P.sb("nb2sel", [128, 1], F32)
        P.op("dve", lambda e: e.tensor_scalar(out=nb2t[:], in0=nb2f[:, 0:1], scalar1=sel[:, 0:1], scalar2=None, op0=ALU.mult), r=["nb2_sb", "sel_sb"], w=["nb2_sb"])
        P.op("dve", lambda e: e.scalar_tensor_tensor(out=nb2t[:], in0=nb2f[:, 1:2], scalar=sel[:, 1:2], in1=nb2t[:], op0=ALU.mult, op1=ALU.add),
             r=["nb2_sb", "sel_sb"], w=["nb2_sb"])
        nb2 = nb2t[:, 0:1]
    else:
        nb2 = nb2f[:, v:v + 1]
    gnb = P.load("gn_sb", gn_d, [128, 128])
    rmask = P.sb("rmask", [128, 8, 64], F32)
    P.op("pool", lambda e: e.memset(rmask[:], 1.0), w=["rmask"])
    P.op("pool", lambda e: e.memset(rmask[:, :, 0:1], 0.0), w=["rmask"])
    gmask = P.sb("gmask", [128, 128], F32)
    P.op("pool", lambda e: e.affine_select(out=gmask[:], in_=ONES[:], pattern=[[1, 128]], compare_op=ALU.is_ge, fill=0.0, base=0, channel_multiplier=-1),
         r=["ones"], w=["gmask"])
    P.op("pool", lambda e: e.memset(gmask[0:64, 64:128], 0.0), w=["gmask"])
    CL = D16
    for n0 in range(0, S, 512):
        b = P.bank()
        ps = P.banks[b][:, :]
        P.op("pe", lambda e, ps=ps, n0=n0: e.matmul(ps, lhsT=w2_b[:], rhs=glr_b[:, n0:n0 + 512], start=True, stop=True), r=["w2_b", "khat"], w=[("bank", b)])
        ta, tak = P.rbuf("fa", [128, 512], F32)
        P.op("act", lambda e, ps=ps, ta=ta: e.activation(out=ta[:], in_=ps, func=AF.Exp, scale=-1.0, bias=nb2), r=[("bank", b), "nb2_sb"], w=[tak])
        P.op("act", lambda e, ta=ta: e.activation(out=ta[:], in_=ta[:], func=AF.Ln, bias=1.0), r=[tak], w=[tak])
        P.op("dve", lambda e, ta=ta, n0=n0: e.tensor_tensor_scan(out=CL[:, n0:n0 + 512], data0=rmask[:].rearrange("p a b -> p (a b)"), data1=ta[:],
                                                              initial=0.0, op0=ALU.mult, op1=ALU.add), r=[tak, "rmask", "D16"], w=["D16"])
    load_rows(A16, 0, 128, lambda v_: RB["gq"] + 128 * v_, "A16lo", extra_w=["A16hi"])
    load_rows(B16, 0, 128, lambda v_: RB["gk"] + 128 * v_, "B16")
    qt, kt, khT, khat = E8a, E8b, E8c, E8d
    dcol = P.sb("dcol", [128, 64], F32)
    P.op("act", lambda e: e.activation(out=dcol[:], in_=CL[:].rearrange("p (c t) -> p c t", t=64)[:, :, 63], func=AF.Exp, scale=-1.0 / 16), r=["D16"], w=["dcol"])
    for n0 in range(0, S, 512):
        ta, tak = P.rbuf("fa", [128, 512], F32)
        tb, tbk = P.rbuf("fb", [128, 512], F32)
        P.op("act", lambda e, ta=ta, n0=n0: e.activation(out=ta[:], in_=CL[:, n0:n0 + 512], func=AF.Exp, scale=-1.0 / 16), r=["D16"], w=[tak])
        P.op("dve", lambda e, ta=ta, n0=n0: e.scalar_tensor_tensor(out=qt[:, n0:n0 + 512], in0=A16[:, n0:n0 + 512], scalar=0.125, in1=ta[:], op0=ALU.mult, op1=ALU.mult),
             r=[tak, "A16lo", "A16hi"], w=["qt"])
        P.op("act", lambda e, tb=tb, n0=n0: e.activation(out=tb[:], in_=CL[:, n0:n0 + 512], func=AF.Exp, scale=1.0 / 16), r=["D16"], w=[tbk])
        P.op("pool", lambda e, tb=tb, n0=n0: e.tensor_tensor(out=kt[:, n0:n0 + 512], in0=B16[:, n0:n0 + 512], in1=tb[:], op=ALU.mult), r=[tbk, "B16"], w=["kt"])
        ta2, tak2 = P.rbuf("fa", [128, 512], F32)
        cl_last = CL[:, n0:n0 + 512].rearrange("p (c t) -> p c t", t=64)[:, :, 63:64].to_broadcast([128, 8, 64])
        P.op("dve", lambda e, ta2=ta2, n0=n0, cl_last=cl_last: e.tensor_tensor(out=ta2[:].rearrange("p (c t) -> p c t", t=64),
                                                                                in0=CL[:, n0:n0 + 512].rearrange("p (c t) -> p c t", t=64),
                                                                                in1=cl_last, op=ALU.subtract), r=["D16"], w=[tak2])
        P.op("act", lambda e, ta2=ta2: e.activation(out=ta2[:], in_=ta2[:], func=AF.Exp, scale=1.0 / 16), r=[tak2], w=[tak2])
        P.op("dve", lambda e, ta2=ta2, n0=n0: e.tensor_tensor(out=khT[:, n0:n0 + 512], in0=B16[:, n0:n0 + 512], in1=ta2[:], op=ALU.mult), r=[tak2, "B16"], w=["khT"])
    khat_v = khat[:].rearrange("p (b m) -> p b m", b=32)
    for blk in range(32):
        b = P.bank()
        ps = P.banks[b][:, 0:128]
        P.op("pe", lambda e, ps=ps, blk=blk: e.matmul(ps, lhsT=khT[:, 128 * blk:128 * blk + 128], rhs=ident_bf[:], start=True, stop=True),
             r=["khT", "ident_bf"], w=[("bank", b)])
        P.op("act", lambda e, ps=ps, blk=blk: e.activation(out=khat_v[:, blk, :], in_=ps, func=AF.Copy), r=[("bank", b)], w=["khat"])
    Sb = P.sb("Sb", [128, 65, 128], BF16)
    Sf = [P.sb(f"Sf{i}", [128, 128], F32) for i in range(2)]
    P.op("pool", lambda e: e.memset(Sb[:, 0, :], 0.0), w=["Sb0"])
    P.op("pool", lambda e: e.memset(Sf[0][:], 0.0), w=["Sf0"])
    for blk in range(32):
        bb_ = [P.bank(), P.bank()]

        def mms(e, blk=blk, bb_=bb_):
            ins = None
            for par in range(2):
                for i in range(2):
                    ins = e.matmul(P.banks[bb_[par]][64 * i:64 * i + 64, 0:128],
                                   lhsT=khat_v[64 * par:64 * par + 64, blk, 64 * i:64 * i + 64],
                                   rhs=gvb[64 * par:64 * par + 64, blk, i, :], start=True, stop=True)
            return ins
        P.op("pe", mms, r=["khat", "C16"], w=[("bank", bb_[0]), ("bank", bb_[1])])
        for par in range(2):
            j = 2 * blk + par
            so, sok = Sf[j % 2], f"Sf{j % 2}"
            sn, snk = Sf[(j + 1) % 2], f"Sf{(j + 1) % 2}"
            P.op("dve", lambda e, so=so, sn=sn, par=par, j=j, bb_=bb_: e.scalar_tensor_tensor(
                out=sn[:], in0=so[:], scalar=dcol[:, j:j + 1], in1=P.banks[bb_[par]][:, 0:128], op0=ALU.mult, op1=ALU.add),
                r=[sok, "dcol", ("bank", bb_[par]), snk], w=[snk])
            P.op("act", lambda e, sn=sn, j=j: e.activation(out=Sb[:, j + 1, :], in_=sn[:], func=AF.Copy), r=[snk], w=["Sb"])
    items = [(blk, i) for blk in range(32) for i in range(2)]
    stA, stB = {}, {}
    cur = {}

    def stage_a(n):
        blk, i = items[n]
        b = P.bank()
        ps_a = P.banks[b][:, 0:128]
        P.op("pe", lambda e: e.matmul(ps_a, lhsT=kt[64 * i:64 * i + 64, 128 * blk:128 * blk + 128],
                                      rhs=qt[64 * i:64 * i + 64, 128 * blk:128 * blk + 128], start=True, stop=True),
             r=["kt", "qt"], w=[("bank", b)])
        am, amk = P.rbuf("am", [128, 128], BF16, n=3)
        P.op("dve", lambda e: e.tensor_tensor(out=am[:], in0=ps_a, in1=gmask[:], op=ALU.mult), r=[("bank", b), "gmask"], w=[amk])
        stA[n] = (am, amk)

    def stage_b(n):
        blk, i = items[n]
        am, amk = stA.pop(n)
        b2_ = P.bank()
        ps_o = P.banks[b2_][:, 0:128]

        def mmo2(e):
            e.matmul(ps_o, lhsT=am[:], rhs=gvb[:, blk, i, :], start=True, stop=False)
            ins = None
            for par in range(2):
                t0 = 128 * blk + 64 * par
                ins = e.matmul(ps_o[64 * par:64 * par + 64, :], lhsT=qt[64 * i:64 * i + 64, t0:t0 + 64],
                               rhs=Sb[64 * i:64 * i + 64, 2 * blk + par, :], start=False, stop=True)
            return ins
        P.op("pe", mmo2, r=[amk, "C16", "qt", "Sb", "Sb0"], w=[("bank", b2_)])
        junk, jk = P.rbuf("junk", [128, 128], F32)
        st, stk = P.rbuf("gst", [128, 2], F32, n=3)
        P.op("act", lambda e: e.activation(out=junk[:], in_=ps_o, func=AF.Square, accum_out=st[:, 0:1]), r=[("bank", b2_)], w=[jk, stk])
        P.op("dve", lambda e: e.tensor_scalar(out=st[:, 1:2], in0=st[:, 0:1], scalar1=1.0 / 128, scalar2=EPS, op0=ALU.mult, op1=ALU.add), r=[stk], w=[stk])
        P.op("act", lambda e: e.activation(out=st[:, 1:2], in_=st[:, 1:2], func=AF.Sqrt), r=[stk], w=[stk])
        P.op("dve", lambda e: e.reciprocal(out=st[:, 1:2], in_=st[:, 1:2]), r=[stk], w=[stk])
        tq, tqk = P.rbuf("tq", [128, 128], F32, n=3)
        P.op("dve", lambda e: e.scalar_tensor_tensor(out=tq[:], in0=ps_o, scalar=st[:, 1:2], in1=gnb[:], op0=ALU.mult, op1=ALU.mult),
             r=[("bank", b2_), stk, "gn_sb"], w=[tqk])
        stB[n] = (tq, tqk)

    def stage_c(n):
        blk, i = items[n]
        tq, tqk = stB.pop(n)
        if blk % 4 == 0 and i == 0:
            cur["ggc"], cur["ggk"] = P.rbuf("ggc", [128, 2, 512], F32)
            for i_ in range(2):
                if selm:
                    hh_, cc0 = (128 * blk) // NT, (128 * blk) % NT
                    gt, gtk = P.rbuf("ggt", [128, 512], F32)
                    P.sel_load(cur["ggc"][:, i_, :], gt[:],
                               [(lambda base: base, pg[hh_][2568 + i_ * 128:2568 + i_ * 128 + 128, cc0:cc0 + 512])],
                               [(lambda base: base, pg[hh_][2568 + (2 + i_) * 128:2568 + (2 + i_) * 128 + 128, cc0:cc0 + 512])],
                               sel, cur["ggk"], gtk)
                else:
                    gr0 = RB["gg"] + (2 * v + i_) * 128
                    P.dma(cur["ggc"][:, i_, :], projT[gr0:gr0 + 128, 128 * blk:128 * blk + 512], w=[cur["ggk"]])
            cur["yo"], cur["yok"] = P.rbuf("yo", [128, 2, 512], BF16 if io.get("out_bf16") else F32)
        ggc, ggk, yo, yok = cur["ggc"], cur["ggk"], cur["yo"], cur["yok"]
        bt = P.bank()
        pst = P.banks[bt][:, 0:128]
        P.op("pe", lambda e: e.matmul(pst, lhsT=tq[:], rhs=IDENT[:], start=True, stop=True), r=[tqk, "ident"], w=[("bank", bt)])
        sg, sgk = P.rbuf("sg", [128, 128], F32)
        c0 = (blk % 4) * 128
        P.op("act", lambda e: e.activation(out=sg[:], in_=ggc[:, i, c0:c0 + 128], func=AF.Silu), r=[ggk], w=[sgk])
        P.op("dve", lambda e: e.tensor_tensor(out=yo[:, i, c0:c0 + 128], in0=pst, in1=sg[:], op=ALU.mult), r=[("bank", bt), sgk], w=[yok])
        if blk % 4 == 3 and i == 1:
            for i_ in range(2):
                r0 = (256 + i_ * 128) if (selm or own_out) else (512 + (2 * v + i_) * 128)
                P.dma(mixT[r0:r0 + 128, 128 * (blk - 3):128 * (blk + 1)], yo[:, i_, :], r=[yok], q="pool")
    NI = len(items)
    for step in range(NI + 2):
        if step < NI:
            stage_a(step)
        if 0 <= step - 1 < NI:
            stage_b(step - 1)
        if 0 <= step - 2 < NI:
            stage_c(step - 2)


S5P = ("lamr", "lami", "logdt", "br", "bi", "cr", "ci")


def phase_mod(P, io):
    c = P.load("c_col", io["ccol"], [128, 8])
    csil = P.sb("c_silf", [128, 8], F32)
    P.op("act", lambda e: e.activation(out=csil[:], in_=c[:], func=AF.Silu), r=["c_col"], w=["c_silf"])
    for t, (wk, bk) in enumerate((("mod_w", "mod_b"), ("mw1", "mb1"), ("mw2", "mb2"), ("mw3", "mb3"))):
        W = io[wk]
        bc = P.load(f"bc{t}", io[bk], [128, 24])
        modc = P.sb(f"modc{t}", [128, 24], F32)
        Wv = W.rearrange("(kt p) m -> p kt m", p=128)
        for c0 in range(0, 3 * D, 512):
            st, stk = P.rbuf("mst", [128, 8, 512], F32, n=3)
            P.dma(st[:], Wv[:, :, c0:c0 + 512], w=[stk])
            b = P.bank()
            ps = P.banks[b]

            def mm(e, st=st, ps=ps):
                ins = None
                for m in range(4):
                    for kt in range(8):
                        ins = e.matmul(ps[:, m:m + 1], lhsT=st[:, kt, 128 * m:128 * m + 128], rhs=csil[:, kt:kt + 1], start=(kt == 0), stop=(kt == 7))
                return ins
            P.op("pe", mm, r=[stk, "c_silf"], w=[("bank", b)])
            col = c0 // 128
            P.op("dve", lambda e, ps=ps, col=col, modc=modc, bc=bc: e.tensor_tensor(out=modc[:, col:col + 4], in0=ps[:, 0:4], in1=bc[:, col:col + 4], op=ALU.add),
                 r=[("bank", b), f"bc{t}"], w=[f"modc{t}"])
        P.dma(io["mods"][t], modc[:], r=[f"modc{t}"], q="pool")


def build_fused_dup():
    P = K()
    I = {}

    def inp(name, shape):
        I[name] = P.inp(name, shape)
        return I[name]
    xT = inp("xT", [D, S])
    inp("ccol", [128, 8])
    inp("sel", [128, 2])
    for n, sh in (("mod_w", [D, 3 * D]), ("mod_b", [128, 24]), ("norm_w", [128, 8]), ("w_in", [D, IN_COLS]),
                  ("fbB", [128, 8]), ("w2", [16, 256]), ("b2c", [128, 2]), ("gnorm", [128, 128]),
                  ("w_o", [D, D]), ("nw1", [128, 8]), ("mw1", [D, 3 * D]), ("mb1", [128, 24]),
                  ("Wg", [D, DFF]), ("Wu", [D, DFF]), ("Wd", [DFF, D]),
                  ("nw2", [128, 8]), ("mw2", [D, 3 * D]), ("mb2", [128, 24]), ("w_in2", [D, D]),
                  ("dskip", [128, 8]), ("w_glu", [D, D]), ("w_o2", [D, D]),
                  ("nw3", [128, 8]), ("mw3", [D, 3 * D]), ("mb3", [128, 24]), ("router", [D, NE]),
                  ("Eg", [NE, D, DFF]), ("Eu", [NE, D, DFF]), ("Ed", [NE, DFF, D]), ("fnorm", [128, 8])):
        inp(n, sh)
    for gh in range(2):
        for n in S5P:
            inp(f"{n}{gh}", [128, 16] if n in ("lamr", "lami", "logdt") else [128, 16, 16])
    outT = P.out("outT", [D, NT])
    projT_s = P.dram("projT_s", [IN_COLS, S])
    modc0_s = P.dram("modc0_s", [128, 24])
    mix_s = P.dram("mix_s", [D, S])
    x2T_s = P.dram("x2T_s", [D, S])
    uT_s = P.dram("uT_s", [D, S])
    modc2_s = P.dram("modc2_s", [128, 24])
    s5y_s = P.dram("s5y_s", [D, S])
    hs = [slice(v * NT, (v + 1) * NT) for v in range(2)]
    mods = P.dram("mods_s", [4, 128, 24])
    P.begin_phase("m_")
    phase_mod(P, dict(ccol=I["ccol"], mods=mods, **{k: I[k] for k in ("mod_w", "mod_b", "mw1", "mb1", "mw2", "mb2", "mw3", "mb3")}))
    for v in range(2):
        P.begin_phase(f"a{v}_")
        phase_L1(P, dict(xT=xT[:, hs[v]], ccol=I["ccol"], mod_w=I["mod_w"], mod_b=I["mod_b"], norm_w=I["norm_w"], w_in=I["w_in"],
                         projT=projT_s[:, hs[v]], modc=modc0_s, pre0=mods[0]))
    for v in range(2):
        P.begin_phase(f"b{v}_")
        phase_L2(P, dict(projT=projT_s, fbB=I["fbB"], w2=I["w2"], b2c=I["b2c"], gnorm=I["gnorm"], mixT=mix_s), v)
    for v in range(2):
        P.begin_phase(f"c{v}_")
        io = {k: I[k] for k in ("w_o", "ccol", "nw1", "mw1", "mb1", "Wg", "Wu", "Wd", "nw2", "mw2", "mb2", "w_in2")}
        io.update(x0T=xT[:, hs[v]], mixT=mix_s[:, hs[v]], modc0=modc0_s, x2T=x2T_s[:, hs[v]], uT=uT_s[:, hs[v]], modc2=modc2_s,
                  pre1=mods[1], pre2=mods[2])
        phase_L3(P, io)
    for v in range(2):
        P.begin_phase(f"d{v}_")
        io = {n: I[f"{n}{v}"] for n in S5P}
        io.update(uT=uT_s[512 * v:512 * v + 512, :], s5yT=s5y_s[512 * v:512 * v + 512, :])
        phase_L4(P, io)
    P.begin_phase("e_")
    io = {k: I[k] for k in ("dskip", "w_glu", "w_o2", "ccol", "nw3", "mw3", "mb3", "router", "Eg", "Eu", "Ed", "fnorm", "sel")}
    io.update(x2T=x2T_s, s5yT=s5y_s, uT=uT_s, modc2=modc2_s, outT=outT, pre3=mods[3])
    phase_L5(P, io)
    return P.build()


class Gath:
    def __init__(self, P, name, rows, cols, bounds, dtype=F32):
        self.P = P
        self.own = P.dram(name + "_own", [rows, cols], dtype)
        self.bounds = bounds
        self.dsts = [P.dram(f"{name}_g{k}", [2 * (b1 - b0), cols], dtype) for k, (b0, b1) in enumerate(bounds)]

    def gather(self, only=None, r=()):
        for k, (b0, b1) in enumerate(self.bounds):
            if only is None or k in only:
                self.P.allgather_pair(self.own[b0:b1, :], self.dsts[k], r=r, w=[("gath", id(self), k)])

    def keys(self):
        return [("gath", id(self), k) for k in range(len(self.bounds))]

    def __getitem__(self, h):
        return _GathRank(self, h)


class _GathRank:
    def __init__(self, g, h):
        self.g, self.h = g, h

    def __getitem__(self, key):
        rs, cs = key
        r0, r1 = rs.start, rs.stop
        for k, (b0, b1) in enumerate(self.g.bounds):
            if b0 <= r0 and r1 <= b1:
                n = b1 - b0
                return self.g.dsts[k][self.h * n + (r0 - b0):self.h * n + (r1 - b0), cs]
        raise AssertionError(f"rows {r0}:{r1} straddle gather chunks")


OWN_ROWBASE = dict(fq=0, fk=256, fv=512, ff=768, gq=772, gk=900, gv=1028, gg=1284, glr=1540)
OWN_COLS = 1556


def own_cols(hh):
    r = np.arange
    return np.concatenate([r(256 * hh, 256 * hh + 256), 512 + r(256 * hh, 256 * hh + 256), 1024 + r(256 * hh, 256 * hh + 256),
                           1536 + r(4 * hh, 4 * hh + 4), 1544 + r(128 * hh, 128 * hh + 128), 1800 + r(128 * hh, 128 * hh + 128),
                           2056 + r(256 * hh, 256 * hh + 256), 2568 + r(256 * hh, 256 * hh + 256), r(3080, 3096)])


PROJ_BOUNDS = [(0, 256), (256, 512), (512, 768), (768, 1024), (1024, 1280), (1280, 1536), (1536, 1672), (1672, 1800),
               (1800, 1928), (1928, 2056), (2056, 2312), (2312, 2568), (2568, 2824), (2824, 3080), (3080, 3096)]


def build_fused():
    P = K()
    I = {}

    def inp(name, shape):
        I[name] = P.inp(name, shape)
        return I[name]
    xT = inp("xT", [D, NT])
    xT_full = inp("xT_full", [D, S])
    inp("ccol", [128, 8])
    inp("sel", [128, 2])
    for n, sh in (("mod_w", [D, 3 * D]), ("mod_b", [128, 24]), ("norm_w", [128, 8]), ("w_in", [D, OWN_COLS]),
                  ("fbB", [128, 8]), ("w2", [16, 256]), ("b2c", [128, 2]), ("gnorm", [128, 128]),
                  ("w_o", [D, D]), ("nw1", [128, 8]), ("mw1", [D, 3 * D]), ("mb1", [128, 24]),
                  ("Wg", [D, DFF]), ("Wu", [D, DFF]), ("Wd", [DFF, D]),
                  ("nw2", [128, 8]), ("mw2", [D, 3 * D]), ("mb2", [128, 24]), ("w_in2", [D, D]),
                  ("dskip", [128, 8]), ("w_glu", [D, D]), ("w_o2", [D, D]),
                  ("nw3", [128, 8]), ("mw3", [D, 3 * D]), ("mb3", [128, 24]), ("router", [D, NE]),
                  ("Eg", [NE, D, DFF]), ("Eu", [NE, D, DFF]), ("Ed", [NE, DFF, D]), ("fnorm", [128, 8])):
        inp(n, sh)
    for n in S5P:
        inp(n, [128, 16] if n in ("lamr", "lami", "logdt") else [128, 16, 16])
    outT = P.out("outT", [D, NT])
    mods = P.dram("mods_s", [4, 128, 24])
    modc0_s = P.dram("modc0_s", [128, 24])
    modc2_s = P.dram("modc2_s", [128, 24])
    proj_own = P.dram("proj_own", [OWN_COLS, S])
    mix_g = Gath(P, "mix", 512, S, [(0, 256), (256, 512)], BF16)
    u_g = Gath(P, "ub", D, NT, [(0, 512), (512, 1024)], BF16)
    s5y_g = Gath(P, "s5y", 512, S, [(128 * k, 128 * k + 128) for k in range(4)])
    mix_own, s5y_own = mix_g.own, s5y_g.own
    uT_own = P.dram("uT_own", [D, NT])
    x2T_own = P.dram("x2T_own", [D, NT])

    P.begin_phase("m_")
    phase_mod(P, dict(ccol=I["ccol"], mods=mods, **{k: I[k] for k in ("mod_w", "mod_b", "mw1", "mb1", "mw2", "mb2", "mw3", "mb3")}))
    P.begin_phase("a_")
    phase_L1(P, dict(xT=xT_full, ccol=I["ccol"], mod_w=I["mod_w"], mod_b=I["mod_b"], norm_w=I["norm_w"], w_in=I["w_in"],
                     projT=proj_own, modc=modc0_s, pre0=mods[0]))
    P.begin_phase("b_")
    phase_L2(P, dict(projT=proj_own, rowbase=OWN_ROWBASE, own_out=True, fbB=I["fbB"], w2=I["w2"], b2c=I["b2c"], gnorm=I["gnorm"],
                     mixT=mix_own, out_bf16=True), 0)
    P.begin_phase("c_")
    mix_g.gather()
    io = {k: I[k] for k in ("w_o", "ccol", "nw1", "mw1", "mb1", "Wg", "Wu", "Wd", "nw2", "mw2", "mb2", "w_in2", "sel")}
    io.update(x0T=xT, mix_g=mix_g, mix_bf16=True, modc0=modc0_s, x2T=x2T_own, uT=uT_own, uT_bf=u_g.own, modc2=modc2_s,
              pre1=mods[1], pre2=mods[2])
    phase_L3(P, io)
    P.begin_phase("d_")
    u_g.gather()
    io = {n: I[n] for n in S5P}
    io.update(u_g=u_g, u_bf16=True, sel=I["sel"], s5yT=s5y_own, gath=s5y_g)
    phase_L4(P, io)
    P.begin_phase("e_")
    io = {k: I[k] for k in ("dskip", "w_glu", "w_o2", "ccol", "nw3", "mw3", "mb3", "router", "Eg", "Eu", "Ed", "fnorm", "sel")}
    io.update(x2T=x2T_own, uT=uT_own, s5y_g=s5y_g, modc2=modc2_s, outT=outT, pre3=mods[3])
    phase_L5(P, io)
    return P.build()


def kernel(**inputs):
    inp = {k: np.asarray(v, dtype=np.float32) for k, v in inputs.items()}
    x = inp["x"]
    c = inp["c"]
    shared = {
        "mod_w": inp["e_mod_mix_w"][0], "mod_b": fm_cols(inp["e_mod_mix_b"][0]), "norm_w": fm_cols(inp["e_norm_mix"][0]),
        "gnorm": np.ascontiguousarray(np.broadcast_to(inp["e_gla_norm"][0][None, :], (128, 128))),
        "w_o": inp["e_w_o"][0], "nw1": fm_cols(inp["e_norm_ffn"][0]), "mw1": inp["e_mod_ffn_w"][0], "mb1": fm_cols(inp["e_mod_ffn_b"][0]),
        "Wg": inp["e_ffn_gate"][0], "Wu": inp["e_ffn_up"][0], "Wd": inp["e_ffn_down"][0],
        "nw2": fm_cols(inp["o_norm_mix"][0]), "mw2": inp["o_mod_mix_w"][0], "mb2": fm_cols(inp["o_mod_mix_b"][0]),
        "w_in2": inp["o_w_in"][0], "dskip": fm_cols(inp["o_d_skip"][0]), "w_glu": inp["o_w_glu"][0], "w_o2": inp["o_w_o"][0],
        "nw3": fm_cols(inp["o_norm_ffn"][0]), "mw3": inp["o_mod_ffn_w"][0], "mb3": fm_cols(inp["o_mod_ffn_b"][0]),
        "router": inp["o_router"][0], "Eg": inp["o_exp_gate"][0], "Eu": inp["o_exp_up"][0], "Ed": inp["o_exp_down"][0],
        "fnorm": fm_cols(inp["final_norm"]),
    }
    s5 = []
    for gh in range(2):
        s5.append({
            "lamr": s5_pair_layout(inp["o_lam_re"][0], gh), "lami": s5_pair_layout(inp["o_lam_im"][0], gh),
            "logdt": s5_pair_layout(np.broadcast_to(inp["o_log_dt"][0][:, None], (64, 64)), gh),
            "br": s5_pair_layout(inp["o_b_re"][0], gh), "bi": s5_pair_layout(inp["o_b_im"][0], gh),
            "cr": s5_pair_layout(inp["o_c_re"][0].transpose(0, 2, 1), gh), "ci": s5_pair_layout(inp["o_c_im"][0].transpose(0, 2, 1), gh)})
    onehot = np.eye(2, dtype=np.float32)
    xT_full = [np.ascontiguousarray(x[b].T) for b in range(NB)]
    percore = []
    for hh in range(2):
        order = [hh, 1 - hh]
        fb = inp["e_fox_fb"][0].reshape(2, 4)[order].reshape(8)
        w2 = inp["e_gla_w2"][0].reshape(16, 2, 128)[:, order, :].reshape(16, 256)
        b2 = inp["e_gla_b2"][0].reshape(2, 128)[order]
        percore.append({"w_in": np.ascontiguousarray(inp["e_w_in"][0][:, own_cols(hh)]),
                        "fbB": np.ascontiguousarray(np.broadcast_to(fb[None, :], (128, 8))),
                        "w2": np.ascontiguousarray(w2), "b2c": np.ascontiguousarray(b2.T)})
    maps = []
    for core in range(NCORE):
        b, hh = core // 2, core % 2
        m = dict(shared)
        m.update(s5[hh])
        m.update(percore[hh])
        m["xT"] = np.ascontiguousarray(x[b, hh * NT:(hh + 1) * NT].T)
        m["xT_full"] = xT_full[b]
        m["ccol"] = fm_cols(c[b])
        m["sel"] = np.ascontiguousarray(np.broadcast_to(onehot[hh][None, :], (128, 2)))
        maps.append(m)
    res = run_bass_kernel_spmd(build_fused(), maps, core_ids=list(range(NCORE))).results
    out = np.empty((NB, S, D), dtype=np.float32)
    for core in range(NCORE):
        b, hh = core // 2, core % 2
        out[b, hh * NT:(hh + 1) * NT] = res[core]["outT"].T
    return out
```
